# Optimizing a Trainium2 kernel written in Bass

```python
import math
import jax, jax.numpy as jnp
from jax import lax
import numpy as np

D_MODEL = 2048
BATCH = 4
SEQ = 2048
DEPTH = 2
DEC_BATCH = 128
DEC_SEQ = 1
PAST_LEN = 16384
PAGE_SIZE = 128

N_EVEN = (DEPTH + 1) // 2
N_ODD = DEPTH // 2

RWKV_WIDTH = D_MODEL // 2
RWKV_HEAD = 64
RWKV_HEADS = RWKV_WIDTH // RWKV_HEAD
LORA_W = 64
LORA_A = 64
LORA_G = 128
RWKV_PROJ = 3 * RWKV_WIDTH + LORA_W + LORA_A + LORA_G
GN_EPS_RWKV = 64e-5
S5_WIDTH = D_MODEL - RWKV_WIDTH
S5_GROUP = 16
S5_GROUPS = S5_WIDTH // S5_GROUP
S5_STATE = 64
IN_A = RWKV_PROJ + S5_WIDTH
RET_QK = 256
RET_HEADS = D_MODEL // RET_QK
RET_V = 2 * RET_QK
RET_VW = RET_HEADS * RET_V
IN_C = 2 * D_MODEL + 2 * RET_VW
RET_CHUNK = 128
D_FF = -(-8 * D_MODEL // 768) * 256
RMS_EPS = 1e-6

kernel_name = 'rwkv7_s5_retnet_hybrid_step'


def rmsnorm(x, g):
    xf = x.astype(jnp.float32)
    return xf * lax.rsqrt(jnp.mean(xf * xf, -1, keepdims=True) + RMS_EPS) * g.astype(jnp.float32)


def swiglu(h, w_gate, w_up, w_down):
    return (jax.nn.silu(h @ w_gate) * (h @ w_up)) @ w_down


def rotary(x, pos):
    half = x.shape[-1] // 2
    freq = 1.0 / (10000.0 ** jnp.linspace(0.0, 1.0, half, dtype=jnp.float32))
    ang = pos[:, None] * freq[None, :]
    cos = jnp.cos(ang)[None, :, None, :]
    sin = jnp.sin(ang)[None, :, None, :]
    x1, x2 = x[..., :half], x[..., half:]
    return jnp.concatenate([x1 * cos - x2 * sin, x2 * cos + x1 * sin], -1)


def rwkv7_mix(p, shift_prev, S0, mu, w0, w2, a0, a2, g2, k_k, k_a, r_k, ln_w, ln_b):
    Bsz, L, _ = p.shape
    f32 = jnp.float32
    W = RWKV_WIDTH
    prev = jnp.concatenate([shift_prev.astype(f32)[:, None, :], p[:, :-1]], axis=1)
    pm = p + (prev - p) * mu
    r, k, v = pm[..., :W], pm[..., W:2 * W], pm[..., 2 * W:3 * W]
    o = 3 * W
    xw = pm[..., o:o + LORA_W]
    o += LORA_W
    xa = pm[..., o:o + LORA_A]
    o += LORA_A
    xg = pm[..., o:o + LORA_G]
    w = -jax.nn.softplus(-(w0 + jnp.tanh(xw) @ w2)) - 0.5
    decay = jnp.exp(-jnp.exp(w))
    a = jax.nn.sigmoid(a0 + xa @ a2)
    g = jax.nn.sigmoid(xg) @ g2
    hs = (Bsz, L, RWKV_HEADS, RWKV_HEAD)
    kk = (k * k_k).reshape(hs)
    kk = kk / jnp.maximum(jnp.sqrt(jnp.sum(kk * kk, -1, keepdims=True)), 1e-12)
    k = (k * (1.0 + (a - 1.0) * k_a)).reshape(hs)
    r = r.reshape(hs)
    v = v.reshape(hs)
    decay = decay.reshape(hs)
    a = a.reshape(hs)
    tm = lambda t: jnp.moveaxis(t, 1, 0)

    def step(S, inp):
        r_t, w_t, k_t, v_t, kk_t, a_t = inp
        sa = jnp.einsum('bhij,bhj->bhi', S, kk_t)
        S = (S * w_t[:, :, None, :] - sa[..., None] * (kk_t * a_t)[:, :, None, :]
             + v_t[..., None] * k_t[:, :, None, :])
        return S, jnp.einsum('bhij,bhj->bhi', S, r_t)

    S_T, ys = lax.scan(step, S0.astype(f32), (tm(r), tm(decay), tm(k), tm(v), tm(kk), tm(a)))
    y = jnp.moveaxis(ys, 0, 1)
    mean = jnp.mean(y, -1, keepdims=True)
    var = jnp.mean(jnp.square(y - mean), -1, keepdims=True)
    hn = (RWKV_HEADS, RWKV_HEAD)
    yn = (y - mean) * lax.rsqrt(var + GN_EPS_RWKV) * ln_w.reshape(hn) + ln_b.reshape(hn)
    bonus = jnp.sum(r * k * r_k.reshape(hn), -1, keepdims=True) * v
    out = (yn + bonus).reshape(Bsz, L, W) * g
    return out, p[:, -1], S_T


def s5_mix(u, h0_re, h0_im, a_re, a_im, b_re, b_im, c_re, c_im, d, log_dt, w_glu, b_glu):
    Bsz, L, _ = u.shape
    f32 = jnp.float32
    ug = u.reshape(Bsz, L, S5_GROUPS, S5_GROUP)
    dt = jnp.exp(log_dt.astype(f32))[:, None]
    lr, li = a_re.astype(f32), a_im.astype(f32)
    mag = jnp.exp(lr * dt)
    ab_re, ab_im = mag * jnp.cos(li * dt), mag * jnp.sin(li * dt)
    den = lr * lr + li * li
    f_re = ((ab_re - 1.0) * lr + ab_im * li) / den
    f_im = (ab_im * lr - (ab_re - 1.0) * li) / den
    bb_re = f_re[..., None] * b_re - f_im[..., None] * b_im
    bb_im = f_re[..., None] * b_im + f_im[..., None] * b_re
    bu_re = jnp.einsum('gnp,blgp->lbgn', bb_re, ug)
    bu_im = jnp.einsum('gnp,blgp->lbgn', bb_im, ug)
    h0r, h0i = h0_re.astype(f32), h0_im.astype(f32)
    bu_re = bu_re.at[0].add(ab_re * h0r - ab_im * h0i)
    bu_im = bu_im.at[0].add(ab_re * h0i + ab_im * h0r)
    elem_a_re = jnp.broadcast_to(ab_re, (L, 1) + ab_re.shape)
    elem_a_im = jnp.broadcast_to(ab_im, (L, 1) + ab_im.shape)

    def combine(e1, e2):
        a1r, a1i, b1r, b1i = e1
        a2r, a2i, b2r, b2i = e2
        return (a2r * a1r - a2i * a1i, a2r * a1i + a2i * a1r,
                a2r * b1r - a2i * b1i + b2r, a2r * b1i + a2i * b1r + b2i)

    _, _, h_re, h_im = lax.associative_scan(combine, (elem_a_re, elem_a_im, bu_re, bu_im), axis=0)
    y = (jnp.einsum('gpn,lbgn->blgp', c_re, h_re) - jnp.einsum('gpn,lbgn->blgp', c_im, h_im))
    y = y.reshape(Bsz, L, S5_WIDTH) + d * u
    y = jax.nn.gelu(y)
    y = y * jax.nn.sigmoid(y @ w_glu + b_glu)
    return y, h_re[-1], h_im[-1]


def retention_mix(p, S0, pos, gn, w_out):
    Bsz, L, _ = p.shape
    H, DK, DV = RET_HEADS, RET_QK, RET_V
    q = rotary(p[..., :D_MODEL].reshape(Bsz, L, H, DK), pos)
    k = rotary(p[..., D_MODEL:2 * D_MODEL].reshape(Bsz, L, H, DK), pos) * (DK ** -0.5)
    v = p[..., 2 * D_MODEL:2 * D_MODEL + RET_VW].reshape(Bsz, L, H, DV)
    g = p[..., 2 * D_MODEL + RET_VW:]
    C = math.gcd(L, RET_CHUNK)
    nC = L // C
    log_g = jnp.log(1.0 - jnp.exp2(-5.0 - jnp.arange(H, dtype=jnp.float32)))
    idx = jnp.arange(C, dtype=jnp.float32)
    dist = idx[:, None] - idx[None, :]
    intra = jnp.where(dist >= 0, jnp.exp(log_g[:, None, None] * jnp.maximum(dist, 0.0)), 0.0)
    q_scale = jnp.exp(log_g[None, :] * (idx[:, None] + 1.0))
    k_scale = jnp.exp(log_g[None, :] * (C - 1.0 - idx[:, None]))
    chunk_decay = jnp.exp(log_g * C)
    ch = lambda t: jnp.moveaxis(t.reshape(Bsz, nC, C, H, t.shape[-1]), 1, 0)

    def chunk_step(S, inp):
        qc, kc, vc = inp
        att = jnp.einsum('bihd,bjhd->bhij', qc, kc) * intra
        o = (jnp.einsum('bhij,bjhe->bihe', att, vc)
             + jnp.einsum('bihd,bhde->bihe', qc * q_scale[None, :, :, None], S))
        S = (S * chunk_decay[None, :, None, None]
             + jnp.einsum('bjhd,bjhe->bhde', kc * k_scale[None, :, :, None], vc))
        return S, o

    S_T, o = lax.scan(chunk_step, S0.astype(jnp.float32), (ch(q), ch(k), ch(v)))
    o = jnp.moveaxis(o, 0, 1).reshape(Bsz, L, H, DV)
    o = o * lax.rsqrt(jnp.mean(o * o, -1, keepdims=True) + RMS_EPS) * gn.reshape(H, DV)
    out = (jax.nn.silu(g) * o.reshape(Bsz, L, RET_VW)) @ w_out
    return out, S_T


def run_trunk(x, pos, st_rwkv, st_shift, st_s5_re, st_s5_im, st_ret, params):
    (norm_mix, norm_ffn, norm_final, w_in_a, mu_shift, rwkv_w0, rwkv_w2, rwkv_a0, rwkv_a2, rwkv_g2,
     rwkv_k_k, rwkv_k_a, rwkv_r_k, rwkv_ln_w, rwkv_ln_b, s5_a_re, s5_a_im, s5_b_re, s5_b_im,
     s5_c_re, s5_c_im, s5_d, s5_log_dt, s5_w_glu, s5_b_glu, w_out_a, w_in_c, ret_gn, w_out_c,
     ffn_w_gate, ffn_w_up, ffn_w_down) = params
    n_rwkv, n_shift, n_re, n_im, n_ret = [], [], [], [], []
    for i in range(DEPTH):
        j = i // 2
        h = rmsnorm(x, norm_mix[i])
        if i % 2 == 0:
            proj = h @ w_in_a[j]
            o_rwkv, sh, S = rwkv7_mix(proj[..., :RWKV_PROJ], st_shift[j], st_rwkv[j], mu_shift[j],
                                      rwkv_w0[j], rwkv_w2[j], rwkv_a0[j], rwkv_a2[j], rwkv_g2[j],
                                      rwkv_k_k[j], rwkv_k_a[j], rwkv_r_k[j], rwkv_ln_w[j], rwkv_ln_b[j])
            o_s5, hr, hi = s5_mix(proj[..., RWKV_PROJ:], st_s5_re[j], st_s5_im[j], s5_a_re[j], s5_a_im[j],
                                  s5_b_re[j], s5_b_im[j], s5_c_re[j], s5_c_im[j], s5_d[j],
                                  s5_log_dt[j], s5_w_glu[j], s5_b_glu[j])
            mix = jnp.concatenate([o_rwkv, o_s5], -1) @ w_out_a[j]
            n_rwkv.append(S.astype(st_rwkv.dtype))
            n_shift.append(sh.astype(st_shift.dtype))
            n_re.append(hr.astype(st_s5_re.dtype))
            n_im.append(hi.astype(st_s5_im.dtype))
        else:
            proj = h @ w_in_c[j]
            mix, S = retention_mix(proj, st_ret[j], pos, ret_gn[j], w_out_c[j])
            n_ret.append(S.astype(st_ret.dtype))
        x = x + mix.astype(x.dtype)
        h = rmsnorm(x, norm_ffn[i])
        x = x + swiglu(h, ffn_w_gate[i], ffn_w_up[i], ffn_w_down[i]).astype(x.dtype)
    y = rmsnorm(x, norm_final).astype(x.dtype)
    return (y, jnp.stack(n_rwkv), jnp.stack(n_shift), jnp.stack(n_re), jnp.stack(n_im), jnp.stack(n_ret))


def setup_inputs(seed: int = 0) -> dict:
    key = jax.random.key(seed)
    keys = jax.random.split(key, 64)
    kit = iter([keys[i] for i in range(64)])
    nrm = lambda shape, scale: scale * jax.random.normal(next(kit), shape, jnp.float32)
    D, W, G, N, P = D_MODEL, RWKV_WIDTH, S5_GROUPS, S5_STATE, S5_GROUP
    x_prompt = nrm((BATCH, SEQ, D), 1.0)
    x_sample = nrm((DEC_BATCH, DEC_SEQ, D), 1.0)
    state_rwkv = nrm((N_EVEN, DEC_BATCH, RWKV_HEADS, RWKV_HEAD, RWKV_HEAD), 0.5)
    state_shift = nrm((N_EVEN, DEC_BATCH, RWKV_PROJ), 1.0)
    state_s5_re = nrm((N_EVEN, DEC_BATCH, G, N), 0.3)
    state_s5_im = nrm((N_EVEN, DEC_BATCH, G, N), 0.3)
    state_ret = nrm((N_ODD, DEC_BATCH, RET_HEADS, RET_QK, RET_V), 0.5)
    norm_mix = 1.0 + nrm((DEPTH, D), 0.01)
    norm_ffn = 1.0 + nrm((DEPTH, D), 0.01)
    norm_final = 1.0 + nrm((D,), 0.01)
    w_in_a = nrm((N_EVEN, D, IN_A), D ** -0.5)
    mu_shift = jax.random.uniform(next(kit), (N_EVEN, RWKV_PROJ), jnp.float32)
    rwkv_w0 = jnp.linspace(-6.0, -1.0, W, dtype=jnp.float32)[None, :] + nrm((N_EVEN, W), 0.1)
    rwkv_w2 = nrm((N_EVEN, LORA_W, W), 0.1 * LORA_W ** -0.5)
    rwkv_a0 = nrm((N_EVEN, W), 0.1)
    rwkv_a2 = nrm((N_EVEN, LORA_A, W), 0.1 * LORA_A ** -0.5)
    rwkv_g2 = nrm((N_EVEN, LORA_G, W), LORA_G ** -0.5)
    rwkv_k_k = 0.85 + nrm((N_EVEN, W), 0.02)
    rwkv_k_a = 1.0 + nrm((N_EVEN, W), 0.02)
    rwkv_r_k = nrm((N_EVEN, W), 0.1)
    rwkv_ln_w = 1.0 + nrm((N_EVEN, W), 0.01)
    rwkv_ln_b = nrm((N_EVEN, W), 0.01)
    s5_a_re = -0.5 + nrm((N_EVEN, G, N), 0.01)
    s5_a_im = math.pi * jnp.arange(N, dtype=jnp.float32) + nrm((N_EVEN, G, N), 0.01)
    s5_b_re = nrm((N_EVEN, G, N, P), (2 * P) ** -0.5)
    s5_b_im = nrm((N_EVEN, G, N, P), (2 * P) ** -0.5)
    s5_c_re = nrm((N_EVEN, G, P, N), N ** -0.5)
    s5_c_im = nrm((N_EVEN, G, P, N), N ** -0.5)
    s5_d = nrm((N_EVEN, S5_WIDTH), 1.0)
    s5_log_dt = jax.random.uniform(next(kit), (N_EVEN, G), jnp.float32, math.log(1e-3), math.log(1e-1))
    s5_w_glu = nrm((N_EVEN, S5_WIDTH, S5_WIDTH), S5_WIDTH ** -0.5)
    s5_b_glu = nrm((N_EVEN, S5_WIDTH), 0.01)
    w_out_a = nrm((N_EVEN, D, D), D ** -0.5)
    w_in_c = nrm((N_ODD, D, IN_C), D ** -0.5)
    ret_gn = 1.0 + nrm((N_ODD, RET_VW), 0.01)
    w_out_c = nrm((N_ODD, RET_VW, D), RET_VW ** -0.5)
    ffn_w_gate = nrm((DEPTH, D, D_FF), D ** -0.5)
    ffn_w_up = nrm((DEPTH, D, D_FF), D ** -0.5)
    ffn_w_down = nrm((DEPTH, D_FF, D), D_FF ** -0.5)
    return {'x_prompt': x_prompt, 'x_sample': x_sample, 'state_rwkv': state_rwkv,
            'state_shift': state_shift, 'state_s5_re': state_s5_re, 'state_s5_im': state_s5_im,
            'state_ret': state_ret, 'norm_mix': norm_mix, 'norm_ffn': norm_ffn, 'norm_final': norm_final,
            'w_in_a': w_in_a, 'mu_shift': mu_shift, 'rwkv_w0': rwkv_w0, 'rwkv_w2': rwkv_w2,
            'rwkv_a0': rwkv_a0, 'rwkv_a2': rwkv_a2, 'rwkv_g2': rwkv_g2, 'rwkv_k_k': rwkv_k_k,
            'rwkv_k_a': rwkv_k_a, 'rwkv_r_k': rwkv_r_k, 'rwkv_ln_w': rwkv_ln_w, 'rwkv_ln_b': rwkv_ln_b,
            's5_a_re': s5_a_re, 's5_a_im': s5_a_im, 's5_b_re': s5_b_re, 's5_b_im': s5_b_im,
            's5_c_re': s5_c_re, 's5_c_im': s5_c_im, 's5_d': s5_d, 's5_log_dt': s5_log_dt,
            's5_w_glu': s5_w_glu, 's5_b_glu': s5_b_glu, 'w_out_a': w_out_a, 'w_in_c': w_in_c,
            'ret_gn': ret_gn, 'w_out_c': w_out_c, 'ffn_w_gate': ffn_w_gate, 'ffn_w_up': ffn_w_up,
            'ffn_w_down': ffn_w_down}


def reference(x_prompt, x_sample, state_rwkv, state_shift, state_s5_re, state_s5_im, state_ret,
              norm_mix, norm_ffn, norm_final, w_in_a, mu_shift, rwkv_w0, rwkv_w2, rwkv_a0, rwkv_a2,
              rwkv_g2, rwkv_k_k, rwkv_k_a, rwkv_r_k, rwkv_ln_w, rwkv_ln_b, s5_a_re, s5_a_im, s5_b_re,
              s5_b_im, s5_c_re, s5_c_im, s5_d, s5_log_dt, s5_w_glu, s5_b_glu, w_out_a, w_in_c, ret_gn,
              w_out_c, ffn_w_gate, ffn_w_up, ffn_w_down):
    params = (norm_mix, norm_ffn, norm_final, w_in_a, mu_shift, rwkv_w0, rwkv_w2, rwkv_a0, rwkv_a2,
              rwkv_g2, rwkv_k_k, rwkv_k_a, rwkv_r_k, rwkv_ln_w, rwkv_ln_b, s5_a_re, s5_a_im, s5_b_re,
              s5_b_im, s5_c_re, s5_c_im, s5_d, s5_log_dt, s5_w_glu, s5_b_glu, w_out_a, w_in_c, ret_gn,
              w_out_c, ffn_w_gate, ffn_w_up, ffn_w_down)
    bp = x_prompt.shape[0]
    zeros = lambda s: jnp.zeros((s.shape[0], bp) + s.shape[2:], s.dtype)
    pos_p = jnp.arange(x_prompt.shape[1], dtype=jnp.float32)
    pos_s = PAST_LEN + jnp.arange(x_sample.shape[1], dtype=jnp.float32)
    y_prompt, p_rwkv, p_shift, p_s5_re, p_s5_im, p_ret = run_trunk(
        x_prompt, pos_p, zeros(state_rwkv), zeros(state_shift), zeros(state_s5_re),
        zeros(state_s5_im), zeros(state_ret), params)
    y_sample, s_rwkv, s_shift, s_s5_re, s_s5_im, s_ret = run_trunk(
        x_sample, pos_s, state_rwkv, state_shift, state_s5_re, state_s5_im, state_ret, params)
    return (y_prompt, y_sample, p_rwkv, p_shift, p_s5_re, p_s5_im, p_ret,
            s_rwkv, s_shift, s_s5_re, s_s5_im, s_ret)
```

```python
import math
import numpy as np
import concourse.bass as bass
import concourse.mybir as mybir
from concourse.alu_op_type import AluOpType as ALU
from concourse.bass_utils import run_bass_kernel_spmd

F32 = mybir.dt.float32
BF16 = mybir.dt.bfloat16
I32 = mybir.dt.int32
AF = mybir.ActivationFunctionType
AX = mybir.AxisListType

D = 2048
W = 1024
HR = 16
PROJ = 3328
INA = 4352
DFF = 5632
RH = 8
DK = 256
DV = 512
INC = 12288
PAST = 16384
EPS = 1e-6
GN_EPS = 64e-5

ENGS = ("tensor", "vector", "scalar", "gpsimd", "sync")
CHUNKED = True
USE_SCRATCH = True


class Prog:
    def __init__(self, nc, stack):
        self.nc = nc
        self.ops = {e: [] for e in ENGS}
        self.seq = {e: 0 for e in ENGS}
        self.seen = {e: {} for e in ENGS}
        self.lastw = {}
        self.readers = {}
        self.esem = {e: stack.enter_context(nc.semaphore("es_" + e)) for e in ENGS}
        self.ndsem = 24
        self.dsem = [stack.enter_context(nc.semaphore("ds%d" % i)) for i in range(self.ndsem)]
        self.dcnt = [0] * self.ndsem
        self.drr = 0
        self.pending_dma = []

    def _need(self, eng, ev, waits, force=False):
        key, sem, val, src = ev
        if self.seen[eng].get(key, 0) >= val:
            return
        self.seen[eng][key] = val
        waits.append((sem, val))

    def op(self, eng, fn, r=(), w=(), dma=False, rg=None):
        waits = []
        if eng == "tensor":
            pfrg = self.__dict__.setdefault("pfrg", {})
            for k in w:
                last = pfrg.get(k)
                if last is not None and last[0] != rg:
                    self._need(eng, last[1], waits)
        for k in r:
            ev = self.lastw.get(k)
            if ev is not None:
                if ev[3] == eng and not dma and eng == "tensor":
                    pass
                else:
                    self._need(eng, ev, waits)
        for k in r:
            if isinstance(k, tuple) and k[0] in ("pf", "pb"):
                for ev2 in self.readers.get(k, ()):
                    if ev2[3] != eng:
                        self._need(eng, ev2, waits)
        for k in w:
            ev = self.lastw.get(k)
            if ev is not None and (dma or ev[3] != eng):
                self._need(eng, ev, waits)
            for ev2 in self.readers.get(k, ()):
                if dma or ev2[3] != eng:
                    self._need(eng, ev2, waits)
        if dma:
            i = self.drr
            self.drr = (self.drr + 1) % self.ndsem
            if self.dcnt[i] > 0:
                self._need(eng, (("D", i), self.dsem[i], self.dcnt[i], None), waits)
            self.dcnt[i] += 16
            ev = (("D", i), self.dsem[i], self.dcnt[i], None)
            inc = (self.dsem[i], 16)
            self.pending_dma.append(ev)
        else:
            self.seq[eng] += 1
            ev = (("E", eng), self.esem[eng], self.seq[eng], eng)
            inc = (self.esem[eng], 1)
            self.seen[eng][("E", eng)] = max(self.seen[eng].get(("E", eng), 0), 0)
        for k in r:
            self.readers.setdefault(k, []).append(ev)
        for k in w:
            self.lastw[k] = ev
            self.readers[k] = []
            if eng == "tensor":
                self.pfrg[k] = (rg, ev)
        self.nrec = getattr(self, "nrec", 0) + 1
        import os
        if self.nrec > int(os.environ.get("KLIMIT", "100000000")):
            fn = lambda e: e.nop()
        self.ops[eng].append((waits, fn, inc))
        return ev

    def pe(self, fn, r=(), w=(), rg=None):
        return self.op("tensor", fn, r, w, rg=rg)

    def vec(self, fn, r=(), w=()):
        return self.op("vector", fn, r, w)

    def act(self, fn, r=(), w=()):
        return self.op("scalar", fn, r, w)

    def pool(self, fn, r=(), w=()):
        return self.op("gpsimd", fn, r, w)

    def dma(self, q, out, in_, r=(), w=(), **kw):
        return self.op(q, lambda e: e.dma_start(out=out, in_=in_, **kw), r, w, dma=True)

    def barrier(self):
        evs = [(("E", e), self.esem[e], self.seq[e], e) for e in ENGS if self.seq[e] > 0]
        evs += [(("D", i), self.dsem[i], self.dcnt[i], None) for i in range(self.ndsem) if self.dcnt[i] > 0]
        for e in ENGS:
            waits = []
            for ev in evs:
                if ev[3] == e:
                    continue
                self._need(e, ev, waits)
            if waits:
                self.ops[e].append((waits, None, None))
        self.lastw = {}
        self.readers = {}
        self.pfrg = {}

    def replay(self, eng, e):
        for waits, fn, inc in self.ops[eng]:
            for sem, val in waits:
                e.wait_ge(sem, val)
            if fn is not None:
                ins = fn(e)
                ins.then_inc(inc[0], inc[1])


def TT(out, in0, in1, op):
    return lambda e: e.tensor_tensor(out=out, in0=in0, in1=in1, op=op)


def TS(out, in0, s1, op0, s2=None, op1=None):
    if op1 is None:
        return lambda e: e.tensor_scalar(out=out, in0=in0, scalar1=s1, scalar2=None, op0=op0)
    return lambda e: e.tensor_scalar(out=out, in0=in0, scalar1=s1, scalar2=s2, op0=op0, op1=op1)


def STT(out, in0, scalar, in1, op0, op1):
    return lambda e: e.scalar_tensor_tensor(out=out, in0=in0, scalar=scalar, in1=in1, op0=op0, op1=op1)


def ACTF(out, in_, func, bias=None, scale=None, accum_out=None):
    kw = {}
    if bias is not None:
        kw["bias"] = bias
    if scale is not None:
        kw["scale"] = scale
    if accum_out is not None:
        kw["accum_out"] = accum_out
    return lambda e: e.activation(out=out, in_=in_, func=func, **kw)


def ACP(out, in_):
    return lambda e: e.activation(out=out, in_=in_, func=AF.Copy)


def CP(out, in_):
    return lambda e: e.tensor_copy(out=out, in_=in_)


def MM(out, lhsT, rhs, start=True, stop=True, sgc=False):
    if sgc:
        return lambda e: e.matmul(out, lhsT=lhsT, rhs=rhs, start=start, stop=stop, skip_group_check=True)
    return lambda e: e.matmul(out, lhsT=lhsT, rhs=rhs, start=start, stop=stop)


def TR(out, in_, ident):
    return lambda e: e.transpose(out, in_, ident)


def MS(ap, val):
    return lambda e: e.memset(ap, val)


def fm(v):
    v = np.asarray(v, np.float32).reshape(-1, 128)
    return np.ascontiguousarray(v.T)


def tile_w(w, gw):
    K, N = w.shape
    kc = K // 128
    g = N // gw
    t = w.reshape(kc, 128, g, gw).transpose(2, 1, 0, 3)
    return np.ascontiguousarray(t).reshape(g, 128, kc * gw)


def tile_rows(w, rk):
    K, N = w.shape
    g = K // (128 * rk)
    t = w.reshape(g, rk, 128, N).transpose(0, 2, 1, 3)
    return np.ascontiguousarray(t).reshape(g, 128, rk * N)


def make_consts():
    c = {}
    c["ident"] = np.eye(128, dtype=np.float32)
    bo = np.zeros((128, 128), np.float32)
    bo[:64, :64] = 1.0
    bo[64:, 64:] = 1.0
    c["bones"] = bo
    c["iota"] = np.broadcast_to(np.arange(512, dtype=np.float32)[None, :], (128, 512)).copy()
    log_g = np.log(1.0 - np.exp2(-5.0 - np.arange(RH, dtype=np.float64)))
    idx = np.arange(128, dtype=np.float64)
    dist = idx[None, :] - idx[:, None]
    intra = np.where(dist >= 0, np.exp(log_g[:, None, None] * np.maximum(dist, 0.0)), 0.0)
    c["intraT"] = np.ascontiguousarray(intra.transpose(1, 0, 2)).astype(np.float32).reshape(128, RH * 128)
    c["qs"] = np.exp(log_g[None, :] * (idx[:, None] + 1.0)).astype(np.float32)
    c["ks"] = np.exp(log_g[None, :] * (127.0 - idx[:, None])).astype(np.float32)
    pp = np.arange(128)
    c["m0"] = ((pp // 32) % 2 == 0).astype(np.float32).reshape(128, 1)
    c["m1"] = ((pp // 32) % 2 == 1).astype(np.float32).reshape(128, 1)
    r64 = (pp % 64)[:, None]
    t64 = np.arange(64)[None, :]
    c["nsu"] = -(t64 > r64).astype(np.float32)
    c["su"] = (t64 > r64).astype(np.float32)
    c["nsl"] = -(t64 < r64).astype(np.float32)
    c["iu"] = (t64 >= r64).astype(np.float32)
    c["i64"] = (t64 == r64).astype(np.float32)
    c["rmask"] = np.broadcast_to((np.arange(128) % 64 != 0).astype(np.float32)[None, :], (128, 128)).copy()
    c["cd"] = np.exp(log_g * 128.0)
    c["g1"] = np.exp(log_g)
    return c


def rot_tables(pos):
    half = 128
    freq = (1.0 / (10000.0 ** np.linspace(0.0, 1.0, half, dtype=np.float32))).astype(np.float32)
    ang = (np.asarray(pos, np.float32)[None, :] * freq[:, None]).astype(np.float32)
    cs = np.cos(ang).astype(np.float32)
    sn = np.sin(ang).astype(np.float32)
    return np.ascontiguousarray(np.stack([cs, sn, cs / 16.0, sn / 16.0], axis=1).astype(np.float32))


CONST_LAYOUT = [("ident", 128), ("bones", 128), ("iota", 512), ("intraT", RH * 128), ("qs", RH), ("ks", RH), ("m0", 1), ("m1", 1), ("nsu", 64), ("su", 64), ("nsl", 64), ("iu", 64), ("i64", 64), ("rmask", 128)]
VEC_LAYOUT = [("nm0", 16), ("nf0", 16), ("nm1", 16), ("nf1", 16), ("mu", 26), ("w0", 8), ("a0", 8), ("kk", 8),
              ("ka", 8), ("rk", 8), ("lnw", 8), ("lnb", 8), ("s5d", 8), ("bglu", 8)]


def _offsets(layout):
    o = {}
    p = 0
    for n, k in layout:
        o[n] = (p, k)
        p += k
    return o, p


COFF, NCONST = _offsets(CONST_LAYOUT)
VOFF, NVEC = _offsets(VEC_LAYOUT)


class Builder:
    def __init__(self, seq, nsub, dbg=(), stages=None, with_sample=True):
        from contextlib import ExitStack
        self.SEQ = seq
        self.NSUB = nsub
        self.T = nsub * 128
        self.NTILE = seq // self.T
        self.dbg = set(dbg)
        self.stages = stages
        self.with_sample = with_sample
        self.stack = ExitStack()
        self.nc = bass.Bass("TRN2", target_bir_lowering=False)
        self.P = Prog(self.nc, self.stack)
        self.dram_in = {}
        self.dram_out = {}
        self.wslot = 0
        self.gbank = 0
        self.scr = {}
        self.scr_done = {}
        self.dbg_shapes = {}

    def din(self, name, shape, dt=F32):
        t = self.nc.dram_tensor(name, list(shape), dt, kind="ExternalInput").ap()
        self.dram_in[name] = t
        return t

    def dout(self, name, shape, dt=F32):
        t = self.nc.dram_tensor(name, list(shape), dt, kind="ExternalOutput").ap()
        self.dram_out[name] = t
        return t

    def sb(self, name, shape, dt=F32, stack=None):
        self._uid = getattr(self, "_uid", 0) + 1
        return (stack or self.stack).enter_context(self.nc.sbuf_tensor("sb%d_%s" % (self._uid, name), list(shape), dt))

    def on(self, st):
        return self.stages is None or st in self.stages

    def dump(self, name, ap, shape, r, dt=F32):
        if name not in self.dbg:
            return
        o = self.dout("dbg_" + name, shape, dt)
        self.P.dma("sync", o, ap, r=r, w=["dbg_" + name])

    def wload(self, src_ap, kc, gw, part=None):
        s = self.wslot
        self.wslot = (self.wslot + 1) % 4
        key = "wb%d" % s
        view = self.wbuf[:, s, 0:kc * gw]
        name = src_ap.tensor.name
        gidx = src_ap.offset // (128 * 4096)
        if not USE_SCRATCH:
            self.P.dma("gpsimd", view, src_ap, w=[key])
            return view.rearrange("p (k g) -> p k g", g=gw), key
        if name not in self.scr:
            ng = 1
            for d in src_ap.tensor.shape:
                ng *= d
            ng //= 128 * 4096
            self.scr[name] = self.nc.dram_tensor("scr_" + name, [ng, 128, 4096], BF16, kind="Internal").ap()
            self.scr_done[name] = set()
        sk = ("scr", name, gidx)
        if gidx not in self.scr_done[name]:
            self.scr_done[name].add(gidx)
            self.P.dma("gpsimd", view, src_ap, w=[key])
            self.P.dma("sync", self.scr[name][gidx], view, r=[key], w=[sk])
        else:
            self.P.dma("sync", view, self.scr[name][gidx], r=[sk], w=[key])
        return view.rearrange("p (k g) -> p k g", g=gw), key

    def bank(self, lo=0, hi=4):
        b = lo + (self.gbank % (hi - lo))
        self.gbank += 1
        return b

    def declare(self):
        SEQ = self.SEQ
        self.xp = self.din("xp", [SEQ, D])
        self.consts_d = self.din("consts", [128, NCONST])
        self.vecs_d = self.din("vecs", [128, NVEC])
        self.rot_d = self.din("rot", [128, 4, SEQ + 1])
        self.nfin_d = self.din("nfin", [1, D])
        self.retgn_d = self.din("retgn", [RH, DV])
        self.w2a2_d = self.din("w2a2", [128, W])
        self.g2_d = self.din("g2", [128, W])
        self.s5a_d = self.din("s5a", [128, 3, 32])
        self.s5b_d = self.din("s5b", [128, 2, 32 * 16])
        self.s5c_d = self.din("s5c", [128, 2, 32 * 64])
        if self.on("rwkv") or self.on("s5"):
            self.wa_d = self.din("wa", [17, 128, 16 * 256])
            self.wglu_d = self.din("wglu", [2, 128, 8 * 512])
            self.woa_d = self.din("woa", [8, 128, 16 * 256])
        self.wg_d, self.wu_d, self.wd_d = {}, {}, {}
        for i in range(2):
            if self.on("ffn%d" % i):
                self.wg_d[i] = self.din("wg%d" % i, [22, 128, 16 * 256])
                self.wu_d[i] = self.din("wu%d" % i, [22, 128, 16 * 256])
                self.wd_d[i] = self.din("wd%d" % i, [22, 128, 2 * D])
        if self.on("ret"):
            self.wc_d = self.din("wc", [RH, 6, 128, 16 * 256])
            self.woc_d = self.din("woc", [RH, 2, 128, 2 * D])
        if self.with_sample:
            self.xs_d = self.din("xs", [16, D])
            self.smp = {"st_rwkv": self.din("st_rwkv", [16, HR, 64, 64]), "st_shift": self.din("st_shift", [16, PROJ]),
                        "st_s5": self.din("st_s5", [2, 16, 4096]), "st_ret": self.din("st_ret", [16, RH, DK, DV]),
                        "s_rwkv": self.dout("s_rwkv", [16, HR, 64, 64]), "s_shift": self.dout("s_shift", [16, PROJ]),
                        "s_s5": self.dout("s_s5", [2, 16, 4096]), "s_ret": self.dout("s_ret", [16, RH, DK, DV])}
            self.ys = self.dout("ys", [16, D])
        self.yp = self.dout("yp", [SEQ, D])
        self.o_prwkv = self.dout("p_rwkv", [HR, 64, 64])
        self.o_pshift = self.dout("p_shift", [26, 128])
        self.o_ps5 = self.dout("p_s5", [2, 32, 128])
        self.o_pret = self.dout("p_ret", [RH, 2, 128, DV])

    def alloc(self):
        T, NSUB = self.T, self.NSUB
        nc = self.nc
        self.x = self.sb("x_tm", [128, NSUB, D])
        self.hT = self.sb("hT", [128, 16, T], BF16)
        self.wbuf = self.sb("wbuf", [128, 4, 4096], BF16)
        self.cst = self.sb("cst", [128, NCONST])
        self.vecs = self.sb("vecs", [128, NVEC])
        self.identb = self.sb("identb", [128, 128], BF16)
        self.nfin = self.sb("nfin_bc", [128, D])
        self.w2a2 = self.sb("w2a2", [128, W], BF16)
        self.g2 = self.sb("g2", [128, W], BF16)
        self.rot = self.sb("rot", [128, 4, T])
        self.junk = self.sb("junk", [128, D], BF16)
        self.stat = self.sb("stat", [128, 16])
        self.omm = self.sb("omm", [128, 26])
        self.psf = self.stack.enter_context(nc.psum_tensor("psf", [128, 6, 512], F32))
        self.psb = self.stack.enter_context(nc.psum_tensor("psb", [128, 2, 1024], BF16))

    def cv(self, name):
        o, k = COFF[name]
        return self.cst[:, o:o + k]

    def vv(self, name, c=None):
        o, k = VOFF[name]
        if c is None:
            return self.vecs[:, o:o + k]
        return self.vecs[:, o + c:o + c + 1]

    def setup(self):
        P = self.P
        P.dma("sync", self.cst[:], self.consts_d[:, :], w=["cst"])
        P.dma("sync", self.vecs[:], self.vecs_d[:, :], w=["vecs"])
        P.dma("sync", self.nfin[:], self.nfin_d[0, :].partition_broadcast(128), w=["nfin"])
        P.dma("gpsimd", self.w2a2[:], self.w2a2_d[:, :], w=["w2a2"])
        P.dma("gpsimd", self.g2[:], self.g2_d[:, :], w=["g2"])
        P.vec(CP(self.identb[:], self.cv("ident")), r=["cst"], w=["identb"])
        mo, mk = VOFF["mu"]
        P.vec(TS(self.omm[:], self.vecs[:, mo:mo + mk], -1.0, ALU.mult, 1.0, ALU.add), r=["vecs"], w=["omm"])

    def norm_to_hT(self, gname, ntok=128, nsub=None, x=None, hT=None):
        P = self.P
        nsub = self.NSUB if nsub is None else nsub
        x = self.x if x is None else x
        hT = self.hT if hT is None else hT
        for s in range(nsub):
            xs = x[0:ntok, s, :]
            ss = self.stat[0:ntok, 0:1]
            P.act(ACTF(self.junk[0:ntok, :], xs, AF.Square, accum_out=ss), r=[("x", s)], w=["junk", "stat0"])
            P.act(ACTF(self.stat[0:ntok, 1:2], ss, AF.Ln, scale=1.0 / D, bias=self.epsc[0:ntok, 0:1]), r=["stat0", "epsc"], w=["stat1"])
            P.act(ACTF(self.stat[0:ntok, 2:3], self.stat[0:ntok, 1:2], AF.Exp, scale=-0.5), r=["stat1"], w=["stat2"])
            P.vec(TS(self.junk[0:ntok, :], xs, self.stat[0:ntok, 2:3], ALU.mult), r=[("x", s), "stat2"], w=["junk"])
            for half in range(2):
                pb = ("pb", half)
                for k8 in range(8):
                    kc = half * 8 + k8
                    P.pe(TR(self.psb[:, half, k8 * 128:k8 * 128 + ntok], self.junk[0:ntok, kc * 128:(kc + 1) * 128],
                            self.identb[0:ntok, 0:ntok]), r=["junk", "identb"], w=[pb])
                for k8 in range(8):
                    kc = half * 8 + k8
                    src = self.psb[:, half, k8 * 128:k8 * 128 + ntok]
                    dst = hT[:, kc, s * 128:s * 128 + ntok]
                    g = self.vv(gname, kc)
                    if False:
                        P.act(ACTF(dst, src, AF.Identity, scale=g), r=[pb, "vecs"], w=[("hT", kc)])
                    else:
                        P.vec(TS(dst, src, g, ALU.mult), r=[pb, "vecs"], w=[("hT", kc)])

    def tm_accum(self, lhs_fn, kcn, w_fn, w_keys, lhs_keys, ntok=128, nsub=None, x=None, xkey="x", banks=(0, 4)):
        P = self.P
        nsub = self.NSUB if nsub is None else nsub
        x = self.x if x is None else x
        for s in range(nsub):
            for cb in range(4):
                b = self.bank(*banks)
                pk = ("pf", b)
                for kc in range(kcn):
                    P.pe(MM(self.psf[0:ntok, b, :], lhs_fn(kc, s), w_fn(kc, cb), start=(kc == 0), stop=(kc == kcn - 1)),
                         r=list(lhs_keys) + list(w_keys), w=[pk])
                xs = x[0:ntok, s, cb * 512:(cb + 1) * 512]
                P.vec(TT(xs, xs, self.psf[0:ntok, b, :], ALU.add), r=[pk, (xkey, s)], w=[(xkey, s)])

    def ffn(self, li, ntok=128, nsub=None, x=None, hT=None, stack=None):
        P = self.P
        nsub = self.NSUB if nsub is None else nsub
        T = nsub * ntok if ntok == 128 else ntok
        hT = self.hT if hT is None else hT
        actv = self.ffn_act
        sil = self.ffn_sil
        for g in range(22):
            wg, kg = self.wload(self.wg_d[li][g], 16, 256)
            wu, ku = self.wload(self.wu_d[li][g], 16, 256)
            wd, kd = self.wload(self.wd_d[li][g], 2, D)
            for m in range(2):
                bg = self.bank(0, 6)
                bu = self.bank(0, 6)
                for kc in range(16):
                    P.pe(MM(self.psf[:, bg, 0:T], wg[:, kc, m * 128:(m + 1) * 128], hT[:, kc, 0:T], kc == 0, kc == 15),
                         r=[kg, ("hT", kc)], w=[("pf", bg)])
                for kc in range(16):
                    P.pe(MM(self.psf[:, bu, 0:T], wu[:, kc, m * 128:(m + 1) * 128], hT[:, kc, 0:T], kc == 0, kc == 15),
                         r=[ku, ("hT", kc)], w=[("pf", bu)])
                P.act(ACTF(sil[:, 0:T], self.psf[:, bg, 0:T], AF.Silu), r=[("pf", bg)], w=["sil"])
                P.vec(TT(actv[:, m, 0:T], sil[:, 0:T], self.psf[:, bu, 0:T], ALU.mult), r=["sil", ("pf", bu)], w=[("actv", m)])
            self.tm_accum(lambda kc, s: actv[:, kc, s * 128:s * 128 + ntok], 2,
                          lambda kc, cb: wd[:, kc, cb * 512:(cb + 1) * 512], [kd], [("actv", 0), ("actv", 1)],
                          ntok=ntok, nsub=nsub, x=x, banks=(0, 6))

    def outproj_a(self, oFM, ntok=128, nsub=None, x=None):
        for cb2 in range(8):
            wo, ko = self.wload(self.woa_d[cb2], 16, 256)
            P = self.P
            nsub_ = self.NSUB if nsub is None else nsub
            x_ = self.x if x is None else x
            for s in range(nsub_):
                b = self.bank(0, 6)
                pk = ("pf", b)
                for kc in range(16):
                    P.pe(MM(self.psf[0:ntok, b, 0:256], oFM[:, kc, s * 128:s * 128 + ntok], wo[:, kc, :], kc == 0, kc == 15),
                         r=[("oFM", kc), ko], w=[pk])
                xs = x_[0:ntok, s, cb2 * 256:(cb2 + 1) * 256]
                P.vec(TT(xs, xs, self.psf[0:ntok, b, 0:256], ALU.add), r=[pk, ("x", s)], w=[("x", s)])

    def final_norm(self, out_ap_fn, ntok=128, nsub=None, x=None):
        P = self.P
        nsub = self.NSUB if nsub is None else nsub
        x = self.x if x is None else x
        for s in range(nsub):
            xs = x[0:ntok, s, :]
            ss = self.stat[0:ntok, 4:5]
            P.act(ACTF(self.junk[0:ntok, :], xs, AF.Square, accum_out=ss), r=[("x", s)], w=["junk", "stat4"])
            P.act(ACTF(self.stat[0:ntok, 5:6], ss, AF.Ln, scale=1.0 / D, bias=self.epsc[0:ntok, 0:1]), r=["stat4", "epsc"], w=["stat5"])
            P.act(ACTF(self.stat[0:ntok, 6:7], self.stat[0:ntok, 5:6], AF.Exp, scale=-0.5), r=["stat5"], w=["stat6"])
            P.vec(STT(xs, xs, self.stat[0:ntok, 6:7], self.nfin[0:ntok, :], ALU.mult, ALU.mult), r=[("x", s), "stat6", "nfin"], w=[("x", s)])
            P.dma("gpsimd", out_ap_fn(s), xs, r=[("x", s)], w=["yout"])

    def layer1(self, first_tile, last_tile):
        from contextlib import ExitStack
        P = self.P
        T, NSUB = self.T, self.NSUB
        CC = make_consts()
        P.barrier()
        with ExitStack() as st:
            Qp = self.sb("r_Qp", [128, 2, T], F32, st)
            Kp = self.sb("r_Kp", [128, 2, T], F32, st)
            TA = self.sb("r_TA", [128, T], F32, st)
            TB = self.sb("r_TB", [128, T], F32, st)
            Qr2 = [self.sb("r_Qr%d" % i, [128, 2, T], BF16, st) for i in range(2)]
            Kr2 = [self.sb("r_Kr%d" % i, [128, 2, T], BF16, st) for i in range(2)]
            KT2 = [self.sb("r_KT%d" % i, [128, NSUB, 256], BF16, st) for i in range(2)]
            V2 = [self.sb("r_V%d" % i, [128, NSUB, 512], BF16, st) for i in range(2)]
            GS2 = [self.sb("r_GS%d" % i, [128, NSUB, 512], BF16, st) for i in range(2)]
            Sf = self.sb("r_Sf", [128, 2, 512], F32, st)
            Sb = self.sb("r_Sb", [128, 2, 512], BF16, st)
            ATT = self.sb("r_ATT", [128, 128], BF16, st)
            CR = self.sb("r_CR", [128, 512], F32, st)
            O = self.sb("r_O", [128, 512], F32, st)
            TMP = self.sb("r_TMP", [128, 512], F32, st)
            GA = self.sb("r_GA", [128, 512], BF16, st)
            GAT = self.sb("r_GAT", [128, 4, T], BF16, st)
            gnb = self.sb("r_gnb", [128, 512], F32, st)
            cos, sin, cos16, sin16 = (self.rot[:, i, :] for i in range(4))

            def proj(h):
                i = h % 2
                Qr, Kr, KT, V, GS = Qr2[i], Kr2[i], KT2[i], V2[i], GS2[i]
                for which, dst, (c_, s_), rdst in ((0, Qp, (cos, sin), Qr), (1, Kp, (cos16, sin16), Kr)):
                    wq, kq = self.wload(self.wc_d[h, which], 16, 256)
                    nm = "QK"[which]
                    for dkc in range(2):
                        b = self.bank(0, 3)
                        for kc in range(16):
                            P.pe(MM(self.psf[:, b, 0:T], wq[:, kc, dkc * 128:(dkc + 1) * 128], self.hT[:, kc, 0:T], kc == 0, kc == 15),
                                 r=[kq, ("hT", kc)], w=[("pf", b)])
                        P.act(ACP(dst[:, dkc, :], self.psf[:, b, 0:T]), r=[("pf", b)], w=[(nm + "p", dkc)])
                        yield
                    P.vec(TT(TA[:], dst[:, 0, :], c_, ALU.mult), r=[(nm + "p", 0), "rot"], w=["TA"])
                    P.vec(TT(TB[:], dst[:, 1, :], s_, ALU.mult), r=[(nm + "p", 1), "rot"], w=["TB"])
                    P.vec(TT(rdst[:, 0, :], TA[:], TB[:], ALU.subtract), r=["TA", "TB"], w=[(nm + "r", i, 0)])
                    P.vec(TT(TA[:], dst[:, 1, :], c_, ALU.mult), r=[(nm + "p", 1), "rot"], w=["TA"])
                    P.vec(TT(TB[:], dst[:, 0, :], s_, ALU.mult), r=[(nm + "p", 0), "rot"], w=["TB"])
                    P.vec(TT(rdst[:, 1, :], TA[:], TB[:], ALU.add), r=["TA", "TB"], w=[(nm + "r", i, 1)])
                    yield
                for s in range(NSUB):
                    for dkc in range(2):
                        slot = (s * 2 + dkc) % 8
                        pb = ("pb", 1)
                        P.pe(TR(self.psb[:, 1, slot * 128:(slot + 1) * 128], Kr[:, dkc, s * 128:(s + 1) * 128], self.identb[:]),
                             r=[("Kr", i, dkc), "identb"], w=[pb])
                    o_, k_ = COFF["ks"]
                    for dkc in range(2):
                        slot = (s * 2 + dkc) % 8
                        P.vec(TS(KT[:, s, dkc * 128:(dkc + 1) * 128], self.psb[:, 1, slot * 128:(slot + 1) * 128],
                                 self.cst[:, o_ + h:o_ + h + 1], ALU.mult), r=[("pb", 1), "cst"], w=[("KT", i, s)])
                    yield
                for which, dst, nm in ((2, V, "V"), (4, GS, "GS")):
                    for vg in range(2):
                        wv, kv = self.wload(self.wc_d[h, which + vg], 16, 256)
                        for s in range(NSUB):
                            b = self.bank(0, 3)
                            for kc in range(16):
                                P.pe(MM(self.psf[:, b, 0:256], self.hT[:, kc, s * 128:(s + 1) * 128], wv[:, kc, :], kc == 0, kc == 15),
                                     r=[kv, ("hT", kc)], w=[("pf", b)])
                            if nm == "V":
                                P.act(ACP(dst[:, s, vg * 256:(vg + 1) * 256], self.psf[:, b, 0:256]), r=[("pf", b)], w=[(nm, i, s)])
                            else:
                                P.act(ACTF(dst[:, s, vg * 256:(vg + 1) * 256], self.psf[:, b, 0:256], AF.Silu), r=[("pf", b)], w=[(nm, i, s)])
                            yield

            def drain(g, n):
                if g is None:
                    return
                for _ in range(n):
                    try:
                        next(g)
                    except StopIteration:
                        return

            for _ in proj(0):
                pass
            for h in range(RH):
                i = h % 2
                Qr, Kr, KT, V, GS = Qr2[i], Kr2[i], KT2[i], V2[i], GS2[i]
                nxt = proj(h + 1) if h + 1 < RH else None
                P.dma("sync", gnb[:], self.retgn_d[h, :].partition_broadcast(128), w=["gnb"])
                if first_tile:
                    P.vec(MS(Sf[:], 0.0), w=["Sf0", "Sf1"])
                else:
                    P.dma("sync", Sf[:], self.o_pret[h].rearrange("a p e -> p a e"), w=["Sf0", "Sf1"], r=[("scr", h)])
                P.act(ACP(Sb[:], Sf[:]), r=["Sf0", "Sf1"], w=["Sb"])
                qo, _ = COFF["qs"]
                io, _ = COFF["intraT"]
                for s in range(NSUB):
                    sl = slice(s * 128, (s + 1) * 128)
                    b = 3
                    for dkc in range(2):
                        P.pe(MM(self.psf[:, b, 0:128], Kr[:, dkc, sl], Qr[:, dkc, sl], dkc == 0, dkc == 1),
                             r=[("Kr", i, dkc), ("Qr", i, dkc)], w=[("pf", b)])
                    bc = 4
                    for dkc in range(2):
                        P.pe(MM(self.psf[:, bc, :], Qr[:, dkc, sl], Sb[:, dkc, :], dkc == 0, dkc == 1),
                             r=[("Qr", i, dkc), "Sb"], w=[("pf", bc)])
                    P.vec(TT(ATT[:], self.psf[:, b, 0:128], self.cst[:, io + h * 128:io + (h + 1) * 128], ALU.mult),
                          r=[("pf", b), "cst"], w=["ATT"])
                    P.vec(TS(CR[:], self.psf[:, bc, :], self.cst[:, qo + h:qo + h + 1], ALU.mult), r=[("pf", bc), "cst"], w=["CR"])
                    for dkc in range(2):
                        bs = 3 + dkc if False else (5 if dkc == 0 else 3)
                        P.pe(MM(self.psf[:, bs, :], KT[:, s, dkc * 128:(dkc + 1) * 128], V[:, s, :]), r=[("KT", i, s), ("V", i, s)], w=[("pf", bs)])
                        if dkc == 0:
                            drain(nxt, 1)
                    bi = 4
                    P.pe(MM(self.psf[:, bi, :], ATT[:], V[:, s, :]), r=["ATT", ("V", i, s)], w=[("pf", bi)])
                    drain(nxt, 1)
                    for dkc in range(2):
                        bs = 5 if dkc == 0 else 3
                        P.vec(STT(Sf[:, dkc, :], Sf[:, dkc, :], float(CC["cd"][h]), self.psf[:, bs, :], ALU.mult, ALU.add),
                              r=[("pf", bs), "Sf%d" % dkc], w=["Sf%d" % dkc])
                    P.act(ACP(Sb[:], Sf[:]), r=["Sf0", "Sf1"], w=["Sb"])
                    P.vec(TT(O[:], self.psf[:, bi, :], CR[:], ALU.add), r=[("pf", bi), "CR"], w=["O"])
                    drain(nxt, 1)
                    P.act(ACTF(self.junk[:, 0:512], O[:], AF.Square, accum_out=self.stat[:, 8:9]), r=["O"], w=["junk", "stat8"])
                    P.act(ACTF(self.stat[:, 9:10], self.stat[:, 8:9], AF.Ln, scale=1.0 / DV, bias=self.epsc[:, 0:1]), r=["stat8", "epsc"], w=["stat9"])
                    P.act(ACTF(self.stat[:, 10:11], self.stat[:, 9:10], AF.Exp, scale=-0.5), r=["stat9"], w=["stat10"])
                    P.vec(STT(TMP[:], O[:], self.stat[:, 10:11], gnb[:], ALU.mult, ALU.mult), r=["O", "stat10", "gnb"], w=["TMP"])
                    P.vec(TT(GA[:], TMP[:], GS[:, s, :], ALU.mult), r=["TMP", ("GS", i, s)], w=["GA"])
                    drain(nxt, 1)
                    for ec in range(4):
                        P.pe(TR(self.psb[:, 0, ec * 128:(ec + 1) * 128], GA[:, ec * 128:(ec + 1) * 128], self.identb[:]),
                             r=["GA", "identb"], w=[("pb", 0)])
                    for ec in range(4):
                        P.act(ACP(GAT[:, ec, sl], self.psb[:, 0, ec * 128:(ec + 1) * 128]), r=[("pb", 0)], w=[("GAT", ec)])
                    drain(nxt, 2)
                P.dma("gpsimd", self.o_pret[h].rearrange("a p e -> p a e"), Sf[:], r=["Sf0", "Sf1"], w=[("scr", h)])
                drain(nxt, 100)
                wo0, k0 = self.wload(self.woc_d[h, 0], 2, D)
                wo1, k1 = self.wload(self.woc_d[h, 1], 2, D)
                self.tm_accum(lambda kc, s: GAT[:, kc, s * 128:(s + 1) * 128], 4,
                              lambda kc, cb: (wo0 if kc < 2 else wo1)[:, kc % 2, cb * 512:(cb + 1) * 512],
                              [k0, k1], [("GAT", e) for e in range(4)])
            P.barrier()

    def bc(self, name, c0, n, ntok):
        o, _ = VOFF[name]
        return self.vecs[:, o + c0:o + c0 + n].rearrange("p (a b) -> p a b", b=1).to_broadcast([128, n, ntok])

    def rwkv_alloc(self, st, chunked=False):
        B = {}
        if chunked:
            B["KR"] = self.sb("c_KR", [128, 8, 2, 2, 64], BF16, st)
            for nm in ("BT", "KTl", "BH", "KH", "BHT", "KHT"):
                B[nm] = self.sb("c_" + nm, [128, 8, 128], BF16, st)
            B["GC"] = self.sb("c_GC", [128, 8, 2], F32, st)
        for nm in ("R", "K", "V", "A", "KK", "BB", "KM", "T1", "T2"):
            B[nm] = self.sb("w_" + nm, [128, 8, 128], F32, st)
        B["P4"] = self.sb("w_P4", [128, 2, 129], F32, st)
        B["PA"] = self.sb("w_PA", [128, 2, 128], F32, st)
        B["PB"] = self.sb("w_PB", [128, 2, 128], F32, st)
        B["XL"] = self.sb("w_XL", [128, 2, 128], F32, st)
        B["TXW"] = self.sb("w_TXW", [128, 128], BF16, st)
        B["SXG"] = self.sb("w_SXG", [128, 128], BF16, st)
        B["Vb"] = self.sb("w_Vb", [128, 8, 128], BF16, st)
        B["VT"] = self.sb("w_VT", [128, 8, 128], BF16, st)
        if not chunked:
            B["S1"] = self.sb("w_S1", [128, 512], F32, st)
            B["S2"] = self.sb("w_S2", [128, 512], F32, st)
            B["S3"] = self.sb("w_S3", [128, 512], F32, st)
            B["S4"] = self.sb("w_S4", [128, 512], F32, st)
            B["S5"] = self.sb("w_S5", [128, 512], BF16, st)
        B["YB"] = self.sb("w_YB", [128, 8, 128], BF16, st)
        B["ST"] = self.sb("w_ST", [128, 64], F32, st)
        return B

    def rwkv_subtile(self, B, s, oFM, ntok=128, prev=None, hT=None, Tstates=None, step_post=None, chunked=False):
        P = self.P
        hT = self.hT if hT is None else hT
        R, K, V, A, KK, BB, KM, T1, T2 = (B[n] for n in ("R", "K", "V", "A", "KK", "BB", "KM", "T1", "T2"))
        P4, PA, PB, XL = B["P4"], B["PA"], B["PB"], B["XL"]
        nt = ntok
        tsl = slice(s * 128, s * 128 + nt)
        for gi in range(13):
            wg, kg = self.wload(self.wa_d[gi], 16, 256)
            b = self.bank()
            for m in range(2):
                for kc in range(16):
                    P.pe(MM(self.psf[:, b, m * 128:m * 128 + nt], wg[:, kc, m * 128:(m + 1) * 128], hT[:, kc, tsl], kc == 0, kc == 15),
                         r=[kg, ("hT", kc)], w=[("pf", b)])
            c0 = 2 * gi
            if prev is None:
                P.vec(CP(P4[:, :, 0:1], self.carry[:, c0:c0 + 2].rearrange("p (a b) -> p a b", b=1)), r=["carry"], w=["bP4"])
                P.act(ACP(P4[:, :, 1:1 + nt], self.psf[:, b, 0:256].rearrange("p (a b) -> p a b", b=128)[:, :, 0:nt]), r=[("pf", b)], w=["bP4"])
                pprev = P4[:, :, 0:nt]
                pcur = P4[:, :, 1:1 + nt]
                P.vec(CP(self.carry[:, c0:c0 + 2].rearrange("p (a b) -> p a b", b=1), P4[:, :, nt:nt + 1]), r=["bP4"], w=["carry"])
                pk = ["bP4"]
            else:
                P.act(ACP(P4[:, :, 0:nt], self.psf[:, b, 0:256].rearrange("p (a b) -> p a b", b=128)[:, :, 0:nt]), r=[("pf", b)], w=["bP4"])
                pprev = prev[:, c0:c0 + 2, 0:nt]
                pcur = P4[:, :, 0:nt]
                P.vec(CP(self.pnew[:, c0:c0 + 2, 0:nt], pcur), r=["bP4"], w=["pnew"])
                pk = ["bP4", "prev"]
            P.vec(TT(PA[:, :, 0:nt], pprev, self.bc("mu", c0, 2, nt), ALU.mult), r=pk + ["vecs"], w=["bPA"])
            P.vec(TT(PB[:, :, 0:nt], pcur, self.omm[:, c0:c0 + 2].rearrange("p (a b) -> p a b", b=1).to_broadcast([128, 2, nt]), ALU.mult),
                  r=pk + ["omm"], w=["bPB"])
            if gi < 4:
                dst, dk = R[:, c0:c0 + 2, 0:nt], "bR"
            elif gi < 8:
                dst, dk = K[:, c0 - 8:c0 - 6, 0:nt], "bK"
            elif gi < 12:
                dst, dk = V[:, c0 - 16:c0 - 14, 0:nt], "bV"
            else:
                dst, dk = XL[:, :, 0:nt], "bXL"
            P.vec(TT(dst, PA[:, :, 0:nt], PB[:, :, 0:nt], ALU.add), r=["bPA", "bPB"], w=[dk])
        TXW, SXG = B["TXW"], B["SXG"]
        P.act(ACTF(TXW[0:64, 0:nt], XL[0:64, 0, 0:nt], AF.Tanh), r=["bXL"], w=["bTXW"])
        P.vec(CP(TXW[64:128, 0:nt], XL[64:128, 0, 0:nt]), r=["bXL"], w=["bTXW"])
        P.act(ACTF(SXG[:, 0:nt], XL[:, 1, 0:nt], AF.Sigmoid), r=["bXL"], w=["bSXG"])
        WD = A
        for what in ("w", "a", "g"):
            for half in range(2):
                b = self.bank()
                for q in range(4):
                    hp = half * 4 + q
                    cs = slice(hp * 128, (hp + 1) * 128)
                    if what == "w":
                        P.pe(MM(self.psf[:, b, q * 128:q * 128 + nt], self.w2a2[0:64, cs], TXW[0:64, 0:nt]), r=["w2a2", "bTXW"], w=[("pf", b)])
                    elif what == "a":
                        P.pe(MM(self.psf[:, b, q * 128:q * 128 + nt], self.w2a2[64:128, cs], TXW[64:128, 0:nt]), r=["w2a2", "bTXW"], w=[("pf", b)])
                    else:
                        P.pe(MM(self.psf[:, b, q * 128:q * 128 + nt], self.g2[:, cs], SXG[:, 0:nt]), r=["g2", "bSXG"], w=[("pf", b)])
                for q in range(4):
                    hp = half * 4 + q
                    src = self.psf[:, b, q * 128:q * 128 + nt]
                    if what == "w":
                        P.act(ACTF(T2[:, hp, 0:nt], src, AF.Sigmoid, bias=self.vv("w0", hp)), r=[("pf", b), "vecs"], w=["bT2"])
                    elif what == "a":
                        P.act(ACTF(A[:, hp, 0:nt], src, AF.Sigmoid, bias=self.vv("a0", hp)), r=[("pf", b), "vecs"], w=["bA"])
                    else:
                        P.act(ACP(T1[:, hp, 0:nt], src), r=[("pf", b)], w=["bT1"])
        G = T1
        P.vec(TT(KK[:, :, 0:nt], K[:, :, 0:nt], self.bc("kk", 0, 8, nt), ALU.mult), r=["bK", "vecs"], w=["bKK"])
        P.act(ACTF(BB[:, :, 0:nt], KK[:, :, 0:nt], AF.Square), r=["bKK"], w=["bBB"])
        for half in range(2):
            b = self.bank()
            hs = slice(half * 4, half * 4 + 4)
            P.pe(MM(self.psf[:, b, 0:4 * nt], self.cv("bones"), BB[:, hs, 0:nt]), r=["cst", "bBB"], w=[("pf", b)])
            P.act(ACTF(KM[:, hs, 0:nt], self.psf[:, b, 0:4 * nt].rearrange("p (a b) -> p a b", b=nt), AF.Sqrt), r=[("pf", b)], w=["bKM"])
        P.vec(TS(KM[:, :, 0:nt], KM[:, :, 0:nt], 1e-12, ALU.max), r=["bKM"], w=["bKM"])
        P.vec(lambda e: e.reciprocal(out=KM[:, :, 0:nt], in_=KM[:, :, 0:nt]), r=["bKM"], w=["bKM"])
        P.vec(TT(KK[:, :, 0:nt], KK[:, :, 0:nt], KM[:, :, 0:nt], ALU.mult), r=["bKK", "bKM"], w=["bKK"])
        P.vec(TT(BB[:, :, 0:nt], KK[:, :, 0:nt], A[:, :, 0:nt], ALU.mult), r=["bKK", "bA"], w=["bBB"])
        P.vec(STT(KM[:, :, 0:nt], A[:, :, 0:nt], -1.0, self.bc("ka", 0, 8, nt), ALU.add, ALU.mult), r=["bA", "vecs"], w=["bKM"])
        P.vec(STT(KM[:, :, 0:nt], KM[:, :, 0:nt], 1.0, K[:, :, 0:nt], ALU.add, ALU.mult), r=["bKM", "bK"], w=["bKM"])
        if chunked:
            P.vec(TS(A[:], T2[:], -math.exp(-0.5), ALU.mult), r=["bT2", "bA"], w=["bA"])
        else:
            P.act(ACTF(WD[:, :, 0:nt], T2[:, :, 0:nt], AF.Exp, scale=-math.exp(-0.5)), r=["bT2", "bA"], w=["bA"])
        BON = K
        P.vec(TT(T2[:, :, 0:nt], R[:, :, 0:nt], KM[:, :, 0:nt], ALU.mult), r=["bR", "bKM"], w=["bT2"])
        P.vec(TT(T2[:, :, 0:nt], T2[:, :, 0:nt], self.bc("rk", 0, 8, nt), ALU.mult), r=["bT2", "vecs"], w=["bT2"])
        for half in range(2):
            b = self.bank()
            hs = slice(half * 4, half * 4 + 4)
            P.pe(MM(self.psf[:, b, 0:4 * nt], self.cv("bones"), T2[:, hs, 0:nt]), r=["cst", "bT2"], w=[("pf", b)])
            P.vec(TT(BON[:, hs, 0:nt], self.psf[:, b, 0:4 * nt].rearrange("p (a b) -> p a b", b=nt), V[:, hs, 0:nt], ALU.mult),
                  r=[("pf", b), "bV", "bKM"], w=["bK"])
        Vb, VT = B["Vb"], B["VT"]
        P.act(ACP(Vb[:, :, 0:nt], V[:, :, 0:nt]), r=["bV"], w=["bVb"])
        for hp in range(8):
            pb = ("pb", 0)
            P.pe(TR(self.psb[0:nt, 0, hp * 128:(hp + 1) * 128], Vb[:, hp, 0:nt], self.identb[:]), r=["bVb", "identb"], w=[pb])
        P.vec(CP(VT[0:nt, :, :], self.psb[0:nt, 0, :].rearrange("p (a b) -> p a b", b=128)), r=[("pb", 0) for hp in range(8)], w=["bVT"])
        v3 = lambda ap: ap.rearrange("p (a b) -> p a b", b=64)
        if chunked:
            self.rwkv_chunk_core(B)
        else:
            S1, S2, S3, S4, S5 = (B[n] for n in ("S1", "S2", "S3", "S4", "S5"))
        for t in range(0 if chunked else nt):
            if Tstates is None:
                Tst, tk = self.Tst, "Tst"
            else:
                Tst, tk = Tstates(t)
            col = lambda X: X[:, :, t:t + 1].to_broadcast([128, 8, 64])
            P.vec(TT(v3(S1[:]), Tst[:], col(KK), ALU.mult), r=[tk, "bKK"], w=["bS1"])
            P.pe(MM(self.psf[:, 4, :], self.cv("bones"), S1[:]), r=["cst", "bS1"], w=[("pf", 4)])
            P.pool(TT(v3(S3[:]), Tst[:], col(WD), ALU.mult), r=[tk, "bA"], w=["bS3"])
            P.pe(MM(self.psf[0:64, 5, :], self.identb[0:nt, t:t + 1].to_broadcast([nt, 64]), VT[0:nt, :, 0:64]), r=["identb", "bVT"], w=[("pf", 5)])
            P.pe(MM(self.psf[64:128, 5, :], self.identb[0:nt, t:t + 1].to_broadcast([nt, 64]), VT[0:nt, :, 64:128]), r=["identb", "bVT"], w=[("pf", 5)])
            P.vec(TT(v3(S4[:]), v3(self.psf[:, 5, :]), col(KM), ALU.mult), r=[("pf", 5), "bKM"], w=["bS4"])
            P.vec(TT(v3(S2[:]), v3(self.psf[:, 4, :]), col(BB), ALU.mult), r=[("pf", 4), "bBB"], w=["bS2"])
            P.vec(TT(S3[:], S3[:], S2[:], ALU.subtract), r=["bS3", "bS2"], w=["bS3"])
            P.vec(TT(Tst[:], v3(S3[:]), v3(S4[:]), ALU.add), r=["bS3", "bS4"], w=[tk])
            P.pool(TT(v3(S5[:]), Tst[:], col(R), ALU.mult), r=[tk, "bR"], w=["bS5"])
            o_, _ = COFF["iota"]
            P.pe(MM(self.psf[0:nt, 2, :], self.csel[0:64, 127 - t:127 - t + nt], S5[0:64, :], t == 0, t == nt - 1), r=["csel", "bS5"], w=[("pf", 2)])
            P.pe(MM(self.psf[0:nt, 3, :], self.csel[64:128, 127 - t:127 - t + nt], S5[64:128, :], t == 0, t == nt - 1), r=["csel", "bS5"], w=[("pf", 3)])
            if step_post is not None:
                step_post(t, Tst, tk)
        YN = KK
        YB, ST = B["YB"], B["ST"]
        yv = YN[0:nt, :, :].rearrange("p a (c d) -> p a c d", d=64)
        if chunked:
            P.act(ACP(YN[:, 0:4, :], self.psf[:, 4, :].rearrange("p (a b) -> p a b", b=128)), r=[("pf", 4)], w=["bKK"])
            P.vec(CP(YN[:, 4:8, :], self.psf[:, 5, :].rearrange("p (a b) -> p a b", b=128)), r=[("pf", 5)], w=["bKK"])
        else:
            P.act(ACP(yv[:, :, 0, :], v3(self.psf[0:nt, 2, :])), r=[("pf", 2)], w=["bKK"])
            P.vec(CP(yv[:, :, 1, :], v3(self.psf[0:nt, 3, :])), r=[("pf", 3)], w=["bKK"])
        y16 = YN[0:nt, :, :].rearrange("p a (c d) -> p (a c) d", d=64)
        q16 = BB[0:nt, :, :].rearrange("p a (c d) -> p (a c) d", d=64)
        P.vec(lambda e: e.tensor_reduce(out=ST[0:nt, 0:16], in_=y16, axis=AX.X, op=ALU.add), r=["bKK"], w=["bST0"])
        P.vec(TS(ST[0:nt, 0:16], ST[0:nt, 0:16], -1.0 / 64, ALU.mult), r=["bST0"], w=["bST0"])
        P.vec(TT(y16, y16, ST[0:nt, 0:16].rearrange("p (a b) -> p a b", b=1).to_broadcast([nt, 16, 64]), ALU.add), r=["bKK", "bST0"], w=["bKK"])
        P.act(ACTF(q16, y16, AF.Square), r=["bKK"], w=["bBB"])
        P.vec(lambda e: e.tensor_reduce(out=ST[0:nt, 16:32], in_=q16, axis=AX.X, op=ALU.add), r=["bBB"], w=["bST1"])
        P.act(ACTF(ST[0:nt, 32:48], ST[0:nt, 16:32], AF.Ln, scale=1.0 / 64, bias=self.epsc[0:nt, 1:2]), r=["bST1", "epsc"], w=["bST2"])
        P.act(ACTF(ST[0:nt, 48:64], ST[0:nt, 32:48], AF.Exp, scale=-0.5), r=["bST2"], w=["bST3"])
        P.vec(TT(YB[0:nt, :, :].rearrange("p a (c d) -> p (a c) d", d=64), y16,
                 ST[0:nt, 48:64].rearrange("p (a b) -> p a b", b=1).to_broadcast([nt, 16, 64]), ALU.mult), r=["bKK", "bST3"], w=["bYB"])
        for hp in range(8):
            pb = ("pb", 1)
            P.pe(TR(self.psb[:, 1, hp * 128:hp * 128 + nt], YB[0:nt, hp, :], self.identb[0:nt, 0:nt]), r=["bYB", "identb"], w=[pb])
            P.vec(TS(T2[:, hp, 0:nt], self.psb[:, 1, hp * 128:hp * 128 + nt], self.vv("lnw", hp), ALU.mult, self.vv("lnb", hp), ALU.add),
                  r=[pb, "vecs"], w=["bT2"])
        P.vec(TT(T2[:, :, 0:nt], T2[:, :, 0:nt], BON[:, :, 0:nt], ALU.add), r=["bT2", "bK"], w=["bT2"])
        P.vec(TT(oFM[:, 0:8, tsl], T2[:, :, 0:nt], G[:, :, 0:nt], ALU.mult), r=["bT2", "bT1"], w=[("oFM", k) for k in range(8)])

    def sincos(self, S, C, X, Rt, Ft, shape_key, n, pool=False):
        P = self.P
        MAGIC = 12582912.0
        (P.pool if pool else P.vec)(TS(Rt, X, MAGIC, ALU.add), r=[shape_key + "X"], w=[shape_key + "R"])
        P.vec(STT(Ft, Rt, -MAGIC, X, ALU.add, ALU.subtract), r=[shape_key + "R", shape_key + "X"], w=[shape_key + "F"])
        P.act(ACTF(S, Ft, AF.Sin, scale=-2.0 * math.pi), r=[shape_key + "F"], w=[shape_key + "S"])
        P.vec(STT(Rt, Ft, -1.0, Ft, ALU.mult, ALU.max), r=[shape_key + "F"], w=[shape_key + "R"])
        P.act(ACTF(C, Rt, AF.Sin, scale=-2.0 * math.pi, bias=self.epsc[0:n, 2:3]), r=[shape_key + "R", "epsc"], w=[shape_key + "C"])

    def s5_setup(self):
        from contextlib import ExitStack
        P = self.P
        T = self.T
        self.LB = self.sb("s5_LB", [128, 2, 8, 128], BF16)
        self.LC = self.sb("s5_LC", [128, 2, 32, 64], BF16)
        self.LBz = self.sb("s5_LBz", [128, 2, 8, 2, 128], BF16)
        self.s5p = self.sb("s5_p", [128, 10, 32])
        self.G0 = self.sb("s5_G0", [128, 2, 32])
        self.GL = self.sb("s5_GL", [128, 2, 32])
        self.Hs5 = self.sb("s5_H", [128, 2, 32])
        self.Hs5T = self.sb("s5_HT", [32, 2, 128])
        with ExitStack() as st:
            a = self.sb("s5s_a", [128, 3, 32], F32, st)
            b = self.sb("s5s_b", [128, 2, 32, 16], F32, st)
            c = self.sb("s5s_c", [128, 2, 32 * 64], F32, st)
            t = self.sb("s5s_t", [128, 24, 32], F32, st)
            bb = self.sb("s5s_bb", [128, 2, 32, 16], F32, st)
            tb = self.sb("s5s_tb", [128, 2, 32, 16], F32, st)
            ZB = self.sb("s5s_ZB", [128, 2, 32, 2, 16], BF16, st)
            P.dma("sync", a[:], self.s5a_d[:, :, :], w=["s_a"])
            P.dma("sync", b[:].rearrange("p a g q -> p a (g q)"), self.s5b_d[:, :, :], w=["s_b"])
            P.dma("sync", c[:], self.s5c_d[:, :, :], w=["s_c"])
            P.vec(CP(self.LC[:].rearrange("p a g q -> p a (g q)"), c[:]), r=["s_c"], w=["LC"])
            lr, li = a[:, 0, :], a[:, 1, :]
            dt = t[:, 0, :]
            P.act(ACTF(dt, a[:, 2, :], AF.Exp), r=["s_a"], w=["s_dt"])
            th = self.s5p[:, 0, :]
            P.vec(STT(th, li, 1.0 / (2.0 * math.pi), dt, ALU.mult, ALU.mult), r=["s_a", "s_dt"], w=["s5p0", "thX"])
            P.vec(TT(t[:, 1, :], lr, dt, ALU.mult), r=["s_a", "s_dt"], w=["s_t1"])
            rho = self.s5p[:, 1, :]
            P.act(ACTF(rho, t[:, 1, :], AF.Exp), r=["s_t1"], w=["s5p1"])
            sn, cs = t[:, 2, :], t[:, 3, :]
            self.sincos(sn, cs, th, t[:, 4, :], t[:, 5, :], "th", 128)
            abr, abi = self.s5p[:, 8, :], self.s5p[:, 9, :]
            P.vec(TT(abr, rho, cs, ALU.mult), r=["s5p1", "thC"], w=["s_abr"])
            P.vec(TT(abi, rho, sn, ALU.mult), r=["s5p1", "thS"], w=["s_abi"])
            den = t[:, 8, :]
            P.vec(TT(den, lr, lr, ALU.mult), r=["s_a"], w=["s_den"])
            P.vec(TT(t[:, 9, :], li, li, ALU.mult), r=["s_a"], w=["s_t9"])
            P.vec(TT(den, den, t[:, 9, :], ALU.add), r=["s_den", "s_t9"], w=["s_den"])
            P.vec(lambda e: e.reciprocal(out=den, in_=den), r=["s_den"], w=["s_den"])
            am1 = t[:, 10, :]
            P.vec(TS(am1, abr, -1.0, ALU.add), r=["s_abr"], w=["s_am1"])
            fre, fim = t[:, 11, :], t[:, 12, :]
            P.vec(TT(fre, am1, lr, ALU.mult), r=["s_am1", "s_a"], w=["s_fre"])
            P.vec(TT(t[:, 13, :], abi, li, ALU.mult), r=["s_abi", "s_a"], w=["s_t13"])
            P.vec(TT(fre, fre, t[:, 13, :], ALU.add), r=["s_fre", "s_t13"], w=["s_fre"])
            P.vec(TT(fre, fre, den, ALU.mult), r=["s_fre", "s_den"], w=["s_fre"])
            P.vec(TT(fim, abi, lr, ALU.mult), r=["s_abi", "s_a"], w=["s_fim"])
            P.vec(TT(t[:, 14, :], am1, li, ALU.mult), r=["s_am1", "s_a"], w=["s_t14"])
            P.vec(TT(fim, fim, t[:, 14, :], ALU.subtract), r=["s_fim", "s_t14"], w=["s_fim"])
            P.vec(TT(fim, fim, den, ALU.mult), r=["s_fim", "s_den"], w=["s_fim"])
            fb = lambda x: x.rearrange("p (g q) -> p g q", q=1).to_broadcast([128, 32, 16])
            P.vec(TT(bb[:, 0], b[:, 0], fb(fre), ALU.mult), r=["s_b", "s_fre"], w=["s_bb0"])
            P.vec(TT(tb[:, 0], b[:, 1], fb(fim), ALU.mult), r=["s_b", "s_fim"], w=["s_tb0"])
            P.vec(TT(bb[:, 0], bb[:, 0], tb[:, 0], ALU.subtract), r=["s_bb0", "s_tb0"], w=["s_bb0"])
            P.vec(TT(bb[:, 1], b[:, 1], fb(fre), ALU.mult), r=["s_b", "s_fre"], w=["s_bb1"])
            P.vec(TT(tb[:, 1], b[:, 0], fb(fim), ALU.mult), r=["s_b", "s_fim"], w=["s_tb1"])
            P.vec(TT(bb[:, 1], bb[:, 1], tb[:, 1], ALU.add), r=["s_bb1", "s_tb1"], w=["s_bb1"])
            P.vec(MS(ZB[:], 0.0), w=["s_ZB"])
            for ri in range(2):
                P.vec(CP(ZB[0:64, ri, :, 0, :], bb[0:64, ri]), r=["s_bb%d" % ri], w=["s_ZB"])
                P.vec(CP(ZB[64:128, ri, :, 1, :], bb[64:128, ri]), r=["s_bb%d" % ri], w=["s_ZB"])
            for ri in range(2):
                for ch in range(8):
                    pb = ("pb", ri)
                    P.pe(TR(self.psb[:, ri, ch * 128:(ch + 1) * 128], ZB[:, ri, 4 * ch:4 * ch + 4, :, :].rearrange("p g a q -> p (g a q)"),
                            self.identb[:]), r=["s_ZB", "identb"], w=[pb])
                P.vec(CP(self.LB[:, ri, :, :], self.psb[:, ri, :].rearrange("p (a b) -> p a b", b=128)),
                      r=[("pb", ri) for ch in range(8)], w=["LB"])
                for ql in range(2):
                    mo, _ = COFF["m%d" % ql]
                    P.vec(TS(self.LBz[:, ri, :, ql, :], self.LB[:, ri, :, :], self.cst[:, mo:mo + 1], ALU.mult), r=["LB", "cst"], w=["LBz"])
            for k, mult, base in ((2, float(T), 15), (4, float(T - 1), 18)):
                X = t[:, base, :]
                kk_ = "ec%d" % k
                P.vec(TS(X, th, mult, ALU.mult), r=["s5p0"], w=[kk_ + "X"])
                self.sincos(self.s5p[:, k + 1, :], self.s5p[:, k, :], X, t[:, base + 1, :], t[:, base + 2, :], kk_, 128)
            P.vec(MS(self.G0[:], 0.0), w=["G0"])
            P.barrier()

    def s5_tile(self, st, oFM, last_tile, sample=None):
        P = self.P
        T = self.T if sample is None else 16
        import os
        if int(os.environ.get("S5STOP", "99")) <= 1:
            return
        f = lambda nm: self.sb("v_" + nm, [128, T], F32, st)
        Ub = self.sb("v_Ub", [128, 8, T], BF16, st)
        DU = self.sb("v_DU", [128, 8, T], F32, st)
        YG = self.sb("v_YG", [128, 8, T], BF16, st)
        Y, Y2 = f("Y"), f("Y2")
        TNAMES = ("X", "R", "F", "SIN", "COS", "A1", "A2", "WR", "WI", "GR", "GI", "A3", "A4", "A5", "A6")
        TSET = [{n: f(n + str(i)) for n in TNAMES} for i in range(2)]
        HRS = [self.sb("v_HR%d" % i, [128, T], BF16, st) for i in range(2)]
        HIS = [self.sb("v_HI%d" % i, [128, T], BF16, st) for i in range(2)]
        for gi in range(13, 17):
            wg, kg = self.wload(self.wa_d[gi], 16, 256)
            for m in range(2):
                ch = 2 * (gi - 13) + m
                b = self.bank()
                for kc in range(16):
                    P.pe(MM(self.psf[:, b, 0:T], wg[:, kc, m * 128:(m + 1) * 128], self.hT[:, kc, 0:T], kc == 0, kc == 15),
                         r=[kg, ("hT", kc)], w=[("pf", b)])
                P.act(ACP(Ub[:, ch, :], self.psf[:, b, 0:T]), r=[("pf", b)], w=[("Ub", ch)])
                P.vec(TS(DU[:, ch, :], self.psf[:, b, 0:T], self.vv("s5d", ch), ALU.mult), r=[("pf", b), "vecs"], w=[("DU", ch)])
        io, _ = COFF["iota"]
        import os
        S5STOP = int(os.environ.get("S5STOP", "99"))
        if S5STOP <= 2:
            return
        if sample is not None:
            self.s5_sample_step(st, Ub, sample)
        for ch in range(8 if S5STOP > 5 else 1):
            by = 4 + (ch % 2)
            def gp_body(q, ch=ch, by=by):
                    if sample is not None:
                        gp = 4 * ch + q
                        hf, ql = q // 2, q % 2
                        ps = slice(64 * hf, 64 * hf + 64)
                        P.pe(MM(self.psf[ps, by, 0:T], self.LC[:, 0, gp, :], self.sHR[:, gp, :], ql == 0, False), r=["LC", "sHR"], w=[("pf", by)])
                        yield
                        P.pe(MM(self.psf[ps, by, 0:T], self.LC[:, 1, gp, :], self.sHI[:, gp, :], False, ql == 1), r=["LC", "sHI"], w=[("pf", by)])
                        yield
                        return
                    gp = 4 * ch + q
                    hf, ql = q // 2, q % 2
                    ps = slice(64 * hf, 64 * hf + 64)
                    pz = gp % 2
                    X, Rt, Ft, SIN, COS, A1, A2, WR, WI, GR, GI, A3, A4, A5, A6 = (TSET[pz][n] for n in TNAMES)
                    HR, HI = HRS[pz], HIS[pz]
                    K_ = lambda nm: nm + str(pz)
                    br, bi = self.bank(), self.bank()
                    P.pe(MM(self.psf[:, br, 0:T], self.LBz[ps, 0, ch, ql, :], Ub[ps, ch, :]), r=["LBz", ("Ub", ch)], w=[("pf", br)])
                    yield
                    P.pe(MM(self.psf[:, bi, 0:T], self.LBz[ps, 1, ch, ql, :], Ub[ps, ch, :]), r=["LBz", ("Ub", ch)], w=[("pf", bi)])
                    yield
                    P.vec(TS(X[:], self.cst[:, io:io + T], self.s5p[:, 0, gp:gp + 1], ALU.mult), r=["cst", "s5p0"], w=[K_("tb") + "X"])
                    yield
                    self.sincos(SIN[:], COS[:], X[:], Rt[:], Ft[:], K_("tb"), 128)
                    yield
                    Br, Bi = self.psf[:, br, 0:T], self.psf[:, bi, 0:T]
                    P.vec(TT(A1[:], Br, COS[:], ALU.mult), r=[("pf", br), K_("tb") + "C"], w=[K_("A1")])
                    yield
                    P.vec(TT(A2[:], Bi, SIN[:], ALU.mult), r=[("pf", bi), K_("tb") + "S"], w=[K_("A2")])
                    yield
                    P.vec(TT(WR[:], A1[:], A2[:], ALU.add), r=[K_("A1"), K_("A2")], w=[K_("WR")])
                    yield
                    P.vec(TT(A1[:], Bi, COS[:], ALU.mult), r=[("pf", bi), K_("tb") + "C"], w=[K_("A1")])
                    yield
                    P.vec(TT(A2[:], Br, SIN[:], ALU.mult), r=[("pf", br), K_("tb") + "S"], w=[K_("A2")])
                    yield
                    P.vec(TT(WI[:], A1[:], A2[:], ALU.subtract), r=[K_("A1"), K_("A2")], w=[K_("WI")])
                    yield
                    rho_bc = self.s5p[:, 1, gp:gp + 1].to_broadcast([128, T])
                    P.vec(lambda e, GR=GR, WR=WR, rho_bc=rho_bc, gp=gp: e.tensor_tensor_scan(out=GR[:], data0=rho_bc, data1=WR[:],
                          initial=self.G0[:, 0, gp:gp + 1], op0=ALU.mult, op1=ALU.add), r=[K_("WR"), "s5p1", "G0"], w=[K_("GR")])
                    P.vec(lambda e, GI=GI, WI=WI, rho_bc=rho_bc, gp=gp: e.tensor_tensor_scan(out=GI[:], data0=rho_bc, data1=WI[:],
                          initial=self.G0[:, 1, gp:gp + 1], op0=ALU.mult, op1=ALU.add), r=[K_("WI"), "s5p1", "G0"], w=[K_("GI")])
                    P.act(ACP(self.GL[:, 0, gp:gp + 1], GR[:, T - 1:T]), r=[K_("GR")], w=["GL"])
                    yield
                    P.act(ACP(self.GL[:, 1, gp:gp + 1], GI[:, T - 1:T]), r=[K_("GI")], w=["GL"])
                    yield
                    P.vec(TT(A3[:], GR[:], COS[:], ALU.mult), r=[K_("GR"), K_("tb") + "C"], w=[K_("A3")])
                    yield
                    P.vec(TT(A4[:], GI[:], SIN[:], ALU.mult), r=[K_("GI"), K_("tb") + "S"], w=[K_("A4")])
                    yield
                    P.vec(TT(A5[:], GR[:], SIN[:], ALU.mult), r=[K_("GR"), K_("tb") + "S"], w=[K_("A5")])
                    yield
                    P.vec(TT(A6[:], GI[:], COS[:], ALU.mult), r=[K_("GI"), K_("tb") + "C"], w=[K_("A6")])
                    yield
                    P.vec(TT(HR[:], A3[:], A4[:], ALU.subtract), r=[K_("A3"), K_("A4")], w=[K_("HR")])
                    yield
                    P.vec(STT(HI[:], A5[:], -1.0, A6[:], ALU.mult, ALU.subtract), r=[K_("A5"), K_("A6")], w=[K_("HI")])
                    yield
                    P.pe(MM(self.psf[ps, by, 0:T], self.LC[:, 0, gp, :], HR[:], ql == 0, False), r=["LC", K_("HR")], w=[("pf", by)])
                    yield
                    P.pe(MM(self.psf[ps, by, 0:T], self.LC[:, 1, gp, :], HI[:], False, ql == 1), r=["LC", K_("HI")], w=[("pf", by)])
                    yield
            if sample is not None:
                for q in range(4):
                    for _ in gp_body(q):
                        pass
            else:
                for pair in ((0, 1), (2, 3)):
                    alive = [gp_body(q) for q in pair]
                    while alive:
                        for g_ in list(alive):
                            try:
                                next(g_)
                            except StopIteration:
                                alive.remove(g_)
            P.vec(TT(Y[:], self.psf[:, by, 0:T], DU[:, ch, :], ALU.add), r=[("pf", by), ("DU", ch)], w=["Y"])
            P.act(ACTF(Y2[:], Y[:], AF.Square), r=["Y"], w=["Y2"])
            P.vec(STT(Y2[:], Y2[:], 0.044715, Y[:], ALU.mult, ALU.mult), r=["Y2", "Y"], w=["Y2"])
            P.vec(TT(Y2[:], Y2[:], Y[:], ALU.add), r=["Y2", "Y"], w=["Y2"])
            P.act(ACTF(Y2[:], Y2[:], AF.Sigmoid, scale=1.5957691216057308), r=["Y2"], w=["Y2"])
            P.vec(TT(YG[:, ch, :], Y[:], Y2[:], ALU.mult), r=["Y", "Y2"], w=[("YG", ch)])
        if S5STOP <= 6:
            return
        for gw in range(2):
            wgl, kgl = self.wload(self.wglu_d[gw], 8, 512)
            for m4 in range(4):
                m = 4 * gw + m4
                b = self.bank()
                for kc in range(8):
                    P.pe(MM(self.psf[:, b, 0:T], wgl[:, kc, m4 * 128:(m4 + 1) * 128], YG[:, kc, :], kc == 0, kc == 7),
                         r=[kgl, ("YG", kc)], w=[("pf", b)])
                P.act(ACTF(Y[:], self.psf[:, b, 0:T], AF.Sigmoid, bias=self.vv("bglu", m)), r=[("pf", b), "vecs"], w=["Y"])
                P.vec(TT(oFM[:, 8 + m, 0:T], YG[:, m, :], Y[:], ALU.mult), r=[("YG", m), "Y"], w=[("oFM", 8 + m)])
        if sample is None:
            self.s5_rot_state(2, self.G0, "G0")
            if last_tile:
                self.s5_state_out(self.o_ps5)

    def s5_rot_state(self, k, dst, dkey):
        P = self.P
        c, s = self.s5p[:, k, :], self.s5p[:, k + 1, :]
        t0, t1 = self.s5p[:, 6, :], self.s5p[:, 7, :]
        glr, gli = self.GL[:, 0, :], self.GL[:, 1, :]
        P.vec(TT(t0, c, glr, ALU.mult), r=["GL"], w=["s5t0"])
        P.vec(TT(t1, s, gli, ALU.mult), r=["GL"], w=["s5t1"])
        P.vec(TT(dst[:, 0, :], t0, t1, ALU.subtract), r=["s5t0", "s5t1"], w=[dkey])
        P.vec(TT(t0, s, glr, ALU.mult), r=["GL", dkey], w=["s5t0"])
        P.vec(TT(t1, c, gli, ALU.mult), r=["GL", dkey], w=["s5t1"])
        P.vec(TT(dst[:, 1, :], t0, t1, ALU.add), r=["s5t0", "s5t1"], w=[dkey])

    def s5_state_out(self, out_ap):
        P = self.P
        self.s5_rot_state(4, self.Hs5, "Hs5")
        for ri in range(2):
            b = self.bank()
            P.pe(TR(self.psf[0:32, b, 0:128], self.Hs5[:, ri, :], self.cv("ident")), r=["Hs5", "cst"], w=[("pf", b)])
            P.act(ACP(self.Hs5T[0:32, ri, :], self.psf[0:32, b, 0:128]), r=[("pf", b)], w=["Hs5T"])
            P.dma("gpsimd", out_ap[ri], self.Hs5T[0:32, ri, :], r=["Hs5T"], w=[("ps5o", ri)])

    def build(self):
        from contextlib import ExitStack
        P = self.P
        nc = self.nc
        T, NSUB = self.T, self.NSUB
        self.declare()
        self.alloc()
        self.carry = self.sb("carry", [128, 26])
        self.Tst = self.sb("Tst", [128, 8, 64])
        self.Tb = self.sb("Tb", [128, 8, 64], BF16)
        P.vec(MS(self.Tb[:], 0.0), w=["Tb"])
        self.csel = self.sb("csel", [128, 255], BF16)
        self.epsc = self.sb("epsc", [128, 4])
        self.ffn_act = self.sb("ffn_act", [128, 2, T], BF16)
        self.ffn_sil = self.sb("ffn_sil", [128, T])
        self.setup()
        P.vec(MS(self.carry[:], 0.0), w=["carry"])
        P.vec(MS(self.Tst[:], 0.0), w=["Tst"])
        P.vec(MS(self.csel[:], 0.0), w=["csel"])
        P.vec(MS(self.csel[:, 127:128], 1.0), w=["csel"])
        P.vec(MS(self.epsc[:, 0:1], EPS), w=["epsc"])
        P.vec(MS(self.epsc[:, 1:2], GN_EPS), w=["epsc"])
        P.vec(MS(self.epsc[:, 2:3], math.pi / 2.0), w=["epsc"])
        P.vec(MS(self.epsc[:, 3:4], 1.0), w=["epsc"])
        self.cself = self.sb("cself", [128, 31])
        P.vec(MS(self.cself[:], 0.0), w=["cself"])
        P.vec(MS(self.cself[:, 15:16], 1.0), w=["cself"])
        if self.on("s5"):
            self.s5_setup()
        P.barrier()
        for ti in range(self.NTILE):
            first, last = ti == 0, ti == self.NTILE - 1
            for s in range(NSUB):
                r0 = (ti * NSUB + s) * 128
                P.dma("sync", self.x[:, s, :], self.xp[r0:r0 + 128, :], w=[("x", s)])
            P.dma("sync", self.rot[:], self.rot_d[:, :, ti * T:(ti + 1) * T], w=["rot"])
            if self.on("rwkv") or self.on("s5"):
                self.norm_to_hT("nm0")
                P.barrier()
                with ExitStack() as st:
                    oFM = self.sb("oFM", [128, 16, T], BF16, st)
                    if not (self.on("rwkv") and self.on("s5")):
                        P.vec(MS(oFM[:], 0.0), w=[("oFM", k) for k in range(16)])
                    if self.on("rwkv"):
                        with ExitStack() as st2:
                            B = self.rwkv_alloc(st2, chunked=CHUNKED)
                            for s in range(NSUB):
                                self.rwkv_subtile(B, s, oFM, chunked=CHUNKED)
                            if last:
                                self.rwkv_state_out(B)
                            P.barrier()
                    if self.on("s5"):
                        with ExitStack() as st2:
                            self.s5_tile(st2, oFM, last)
                            P.barrier()
                    self.outproj_a(oFM)
                    P.barrier()
            if self.on("norm"):
                self.norm_to_hT("nf0")
                self.dump("hT", self.hT[:].rearrange("p a b -> p (a b)"), [128, 16 * T], [("hT", k) for k in range(16)], BF16)
            if self.on("ffn0"):
                self.norm_to_hT("nf0")
                self.ffn(0)
            if self.on("ret"):
                self.norm_to_hT("nm1")
                self.layer1(first, last)
            if self.on("ffn1"):
                self.norm_to_hT("nf1")
                self.ffn(1)
            if self.on("final"):
                self.final_norm(lambda s: self.yp[(ti * NSUB + s) * 128:(ti * NSUB + s + 1) * 128, :])
            else:
                for s in range(NSUB):
                    r0 = (ti * NSUB + s) * 128
                    P.dma("gpsimd", self.yp[r0:r0 + 128, :], self.x[:, s, :], r=[("x", s)], w=["yout"])
            P.barrier()
        if self.with_sample:
            self.sample_phase()
        P.barrier()
        with nc.Block() as block:
            @block.tensor
            def _(e):
                P.replay("tensor", e)

            @block.vector
            def _(e):
                P.replay("vector", e)

            @block.scalar
            def _(e):
                P.replay("scalar", e)

            @block.gpsimd
            def _(e):
                P.replay("gpsimd", e)

            @block.sync
            def _(e):
                P.replay("sync", e)
        return nc

    def sample_phase(self):
        from contextlib import ExitStack
        P = self.P
        smp = self.smp
        P.barrier()
        P.dma("sync", self.x[0:16, 0, :], self.xs_d[:, :], w=[("x", 0)])
        if self.on("rwkv") or self.on("s5"):
            self.norm_to_hT("nm0", ntok=16, nsub=1)
            P.barrier()
            with ExitStack() as st:
                oFM = self.sb("oFMs", [128, 16, 16], BF16, st)
                if not (self.on("rwkv") and self.on("s5")):
                    P.vec(MS(oFM[:], 0.0), w=[("oFM", k) for k in range(16)])
                if self.on("rwkv"):
                    with ExitStack() as st2:
                        self.rwkv_sample(st2, oFM, smp)
                        P.barrier()
                if self.on("s5"):
                    with ExitStack() as st2:
                        self.s5_tile(st2, oFM, False, sample=smp)
                        P.barrier()
                self.outproj_a(oFM, ntok=16, nsub=1)
                P.barrier()
        if self.on("ffn0"):
            self.norm_to_hT("nf0", ntok=16, nsub=1)
            self.ffn(0, ntok=16, nsub=1)
        if self.on("ret"):
            self.norm_to_hT("nm1", ntok=16, nsub=1)
            self.layer1_sample(smp)
        if self.on("ffn1"):
            self.norm_to_hT("nf1", ntok=16, nsub=1)
            self.ffn(1, ntok=16, nsub=1)
        if self.on("final"):
            self.final_norm(lambda s: self.ys[:, :], ntok=16, nsub=1)
        else:
            P.dma("gpsimd", self.ys[:, :], self.x[0:16, 0, :], r=[("x", 0)], w=["ysout"])
        P.barrier()

    def rwkv_state_out(self, B):
        P = self.P
        S1 = B["R"][:].rearrange("p a b -> p (a b)")
        for hp in range(8):
            b = self.bank()
            P.pe(TR(self.psf[0:64, b, 0:128], self.Tst[:, hp, :], self.cv("ident")), r=["Tst", "cst"], w=[("pf", b)])
            P.act(ACP(S1[0:64, 0:128], self.psf[0:64, b, 0:128]), r=[("pf", b)], w=["bS1"])
            P.dma("gpsimd", self.o_prwkv[2 * hp:2 * hp + 2].rearrange("h i j -> i h j"),
                  S1[0:64, 0:128].rearrange("p (h j) -> p h j", j=64), r=["bS1"], w=[("prwkv", hp)])
        b = self.bank()
        P.pe(TR(self.psf[0:26, b, 0:128], self.carry[:, 0:26], self.cv("ident")), r=["carry", "cst"], w=[("pf", b)])
        P.act(ACP(S1[0:26, 128:256], self.psf[0:26, b, 0:128]), r=[("pf", b)], w=["bS1"])
        P.dma("gpsimd", self.o_pshift[:, :], S1[0:26, 128:256], r=["bS1"], w=["pshift"])


def shared_maps(inp, seq):
    m = {}
    cc = make_consts()
    m["consts"] = np.ascontiguousarray(np.concatenate([cc[n].reshape(128, -1) for n, _ in CONST_LAYOUT], axis=1).astype(np.float32))
    vec = {"nm0": inp["norm_mix"][0], "nf0": inp["norm_ffn"][0], "nm1": inp["norm_mix"][1], "nf1": inp["norm_ffn"][1],
           "mu": inp["mu_shift"][0], "w0": inp["rwkv_w0"][0], "a0": inp["rwkv_a0"][0], "kk": inp["rwkv_k_k"][0],
           "ka": inp["rwkv_k_a"][0], "rk": inp["rwkv_r_k"][0], "lnw": inp["rwkv_ln_w"][0], "lnb": inp["rwkv_ln_b"][0],
           "s5d": inp["s5_d"][0], "bglu": inp["s5_b_glu"][0]}
    m["vecs"] = np.ascontiguousarray(np.concatenate([fm(vec[n]) for n, _ in VEC_LAYOUT], axis=1))
    m["rot"] = rot_tables(np.concatenate([np.arange(seq, dtype=np.float32), np.array([PAST], np.float32)]))
    m["nfin"] = np.ascontiguousarray(inp["norm_final"].reshape(1, D))
    m["retgn"] = np.ascontiguousarray(inp["ret_gn"][0].reshape(RH, DV))
    m["w2a2"] = np.ascontiguousarray(np.concatenate([inp["rwkv_w2"][0], inp["rwkv_a2"][0]], axis=0))
    m["g2"] = np.ascontiguousarray(inp["rwkv_g2"][0])
    lay = lambda a: a.reshape(32, 2, 64).transpose(1, 2, 0).reshape(128, 32)
    ld = np.repeat(inp["s5_log_dt"][0].reshape(32, 2).T[:, None, :], 64, axis=1).reshape(128, 32)
    m["s5a"] = np.ascontiguousarray(np.stack([lay(inp["s5_a_re"][0]), lay(inp["s5_a_im"][0]), ld], axis=1))
    layb = lambda b: b.reshape(32, 2, 64, 16).transpose(1, 2, 0, 3).reshape(128, 32 * 16)
    m["s5b"] = np.ascontiguousarray(np.stack([layb(inp["s5_b_re"][0]), layb(inp["s5_b_im"][0])], axis=1))

    def layc(c):
        c4 = c.reshape(32, 2, 16, 64)
        Z = np.zeros((2, 64, 32, 2, 2, 16), np.float32)
        for gl in range(2):
            for gp in range(32):
                Z[gl, :, gp, gp % 2, gl, :] = c4[gp, gl].T
        return Z.reshape(128, 32 * 64)
    m["s5c"] = np.ascontiguousarray(np.stack([layc(inp["s5_c_re"][0]), layc(inp["s5_c_im"][0])], axis=1))
    m["wa"] = tile_w(inp["w_in_a"][0], 256)
    m["wglu"] = tile_w(inp["s5_w_glu"][0], 512)
    m["woa"] = tile_w(inp["w_out_a"][0], 256)
    for i in range(2):
        m["wg%d" % i] = tile_w(inp["ffn_w_gate"][i], 256)
        m["wu%d" % i] = tile_w(inp["ffn_w_up"][i], 256)
        m["wd%d" % i] = tile_rows(inp["ffn_w_down"][i], 2)
    wc = inp["w_in_c"][0]
    hs = []
    for h in range(RH):
        cols = [wc[:, h * 256:(h + 1) * 256], wc[:, D + h * 256:D + (h + 1) * 256]]
        for base in (2 * D, 2 * D + RH * DV):
            for vg in range(2):
                cols.append(wc[:, base + h * 512 + vg * 256:base + h * 512 + (vg + 1) * 256])
        hs.append(np.stack([tile_w(np.ascontiguousarray(c), 256)[0] for c in cols], axis=0))
    m["wc"] = np.ascontiguousarray(np.stack(hs, axis=0))
    m["woc"] = tile_rows(inp["w_out_c"][0], 2).reshape(RH, 2, 128, 2 * D)
    return m


def run(inp, seq, nsub, cores, dbg=(), stages=None, trace=False, with_sample=True):
    bld = Builder(seq, nsub, dbg=dbg, stages=stages, with_sample=with_sample)
    nc = bld.build()
    sh = shared_maps(inp, seq)
    in_maps = []
    for c in cores:
        m = dict(sh)
        m["xp"] = np.ascontiguousarray(inp["x_prompt"][c % 4, :seq])
        rs = slice(16 * c, 16 * c + 16)
        m["xs"] = np.ascontiguousarray(inp["x_sample"][rs, 0, :])
        m["st_rwkv"] = np.ascontiguousarray(inp["state_rwkv"][0, rs])
        m["st_shift"] = np.ascontiguousarray(inp["state_shift"][0, rs])
        m["st_s5"] = np.ascontiguousarray(np.stack([inp["state_s5_re"][0, rs].reshape(16, 4096), inp["state_s5_im"][0, rs].reshape(16, 4096)]))
        m["st_ret"] = np.ascontiguousarray(inp["state_ret"][0, rs])
        in_maps.append({k: v for k, v in m.items() if k in bld.dram_in})
    res = run_bass_kernel_spmd(nc, in_maps, core_ids=list(range(len(cores))), trace=trace)
    return res, bld


def kernel(**inp):
    inp = {k: np.asarray(v) for k, v in inp.items()}
    seq = inp["x_prompt"].shape[1]
    res, bld = run(inp, seq, 2, list(range(8)))
    R = res.results
    B = 4
    y_prompt = np.stack([R[b]["yp"] for b in range(B)])
    p_rwkv = np.stack([R[b]["p_rwkv"] for b in range(B)])[None]
    p_shift = np.stack([R[b]["p_shift"].reshape(PROJ) for b in range(B)])[None]
    p_s5_re = np.stack([R[b]["p_s5"][0].reshape(64, 64) for b in range(B)])[None]
    p_s5_im = np.stack([R[b]["p_s5"][1].reshape(64, 64) for b in range(B)])[None]
    p_ret = np.stack([R[b]["p_ret"].reshape(RH, DK, DV) for b in range(B)])[None]
    cat = lambda k: np.concatenate([R[c][k] for c in range(8)], axis=0)
    y_sample = cat("ys")[:, None, :]
    s_rwkv = cat("s_rwkv")[None]
    s_shift = cat("s_shift")[None]
    s_s5_re = np.concatenate([R[c]["s_s5"][0] for c in range(8)], axis=0).reshape(1, 128, 64, 64)
    s_s5_im = np.concatenate([R[c]["s_s5"][1] for c in range(8)], axis=0).reshape(1, 128, 64, 64)
    s_ret = cat("s_ret")[None]
    return (y_prompt, y_sample, p_rwkv, p_shift, p_s5_re, p_s5_im, p_ret,
            s_rwkv, s_shift, s_s5_re, s_s5_im, s_ret)


def simulate_sync(P):
    val = {}
    pc = {e: 0 for e in ENGS}
    progress = True
    while progress:
        progress = False
        for e in ENGS:
            while pc[e] < len(P.ops[e]):
                waits, fn, inc = P.ops[e][pc[e]]
                if any(val.get(id(sem), 0) < v for sem, v in waits):
                    break
                if inc is not None:
                    val[id(inc[0])] = val.get(id(inc[0]), 0) + inc[1]
                pc[e] += 1
                progress = True
    stuck = {e: (pc[e], len(P.ops[e])) for e in ENGS if pc[e] < len(P.ops[e])}
    return stuck


def _s5_sample_step(self, st, Ub, smp):
    P = self.P
    H0 = self.sb("q_H0", [128, 2, 32, 16], F32, st)
    HN = self.sb("q_HN", [128, 2, 32, 16], F32, st)
    TA = self.sb("q_TA", [128, 32, 16], F32, st)
    TB = self.sb("q_TB", [128, 32, 16], F32, st)
    self.sHR = self.sb("q_sHR", [128, 32, 16], BF16, st)
    self.sHI = self.sb("q_sHI", [128, 32, 16], BF16, st)
    stg = self.sb("q_stg", [16, 2, 1024], F32, st)
    idf = self.cv("ident")
    n = 0
    for ri in range(2):
        for k in range(4):
            buf = stg[:, n % 2, :]
            sk = ("stg", n % 2)
            n += 1
            P.dma("sync", buf, smp["st_s5"][ri, :, k * 1024:(k + 1) * 1024], w=[sk])
            b = self.bank()
            for g8 in range(8):
                P.pe(TR(self.psf[:, b, g8 * 16:(g8 + 1) * 16], buf[:, g8 * 128:(g8 + 1) * 128], idf[0:16, 0:16]), r=[sk, "cst"], w=[("pf", b)])
            P.vec(CP(H0[:, ri, 8 * k:8 * k + 8, :], self.psf[:, b, 0:128].rearrange("p (a b) -> p a b", b=16)), r=[("pf", b)], w=[("H0", ri)])
    for gp in range(32):
        ch, q = gp // 4, gp % 4
        hf, ql = q // 2, q % 2
        ps = slice(64 * hf, 64 * hf + 64)
        for ri in range(2):
            P.pe(MM(self.psf[:, ri, gp * 16:(gp + 1) * 16], self.LBz[ps, ri, ch, ql, :], Ub[ps, ch, 0:16]), r=["LBz", ("Ub", ch)], w=[("pf", ri)])
    abc = lambda k: self.s5p[:, k, :].rearrange("p (g q) -> p g q", q=1).to_broadcast([128, 32, 16])
    pv = lambda ri: self.psf[:, ri, :].rearrange("p (a b) -> p a b", b=16)
    P.vec(TT(TA[:], H0[:, 0], abc(8), ALU.mult), r=[("H0", 0), "s5p"], w=["qTA"])
    P.vec(TT(TB[:], H0[:, 1], abc(9), ALU.mult), r=[("H0", 1), "s5p"], w=["qTB"])
    P.vec(TT(HN[:, 0], TA[:], TB[:], ALU.subtract), r=["qTA", "qTB"], w=[("HN", 0)])
    P.vec(TT(HN[:, 0], HN[:, 0], pv(0), ALU.add), r=[("HN", 0), ("pf", 0)], w=[("HN", 0)])
    P.vec(TT(TA[:], H0[:, 1], abc(8), ALU.mult), r=[("H0", 1), "s5p"], w=["qTA"])
    P.vec(TT(TB[:], H0[:, 0], abc(9), ALU.mult), r=[("H0", 0), "s5p"], w=["qTB"])
    P.vec(TT(HN[:, 1], TA[:], TB[:], ALU.add), r=["qTA", "qTB"], w=[("HN", 1)])
    P.vec(TT(HN[:, 1], HN[:, 1], pv(1), ALU.add), r=[("HN", 1), ("pf", 1)], w=[("HN", 1)])
    P.vec(CP(self.sHR[:], HN[:, 0]), r=[("HN", 0)], w=["sHR"])
    P.vec(TS(self.sHI[:], HN[:, 1], -1.0, ALU.mult), r=[("HN", 1)], w=["sHI"])
    for ri in range(2):
        for k in range(4):
            buf = stg[:, n % 2, :]
            sk = ("stg", n % 2)
            n += 1
            b = self.bank(2, 4)
            b2 = self.bank(2, 4)
            for g8 in range(8):
                bb = b if g8 < 4 else b2
                P.pe(TR(self.psf[0:16, bb, (g8 % 4) * 128:(g8 % 4 + 1) * 128], HN[:, ri, 8 * k + g8, :], idf), r=[("HN", ri), "cst"], w=[("pf", bb)])
            P.vec(CP(buf[:, 0:512], self.psf[0:16, b, :]), r=[("pf", b)], w=[sk])
            P.vec(CP(buf[:, 512:1024], self.psf[0:16, b2, :]), r=[("pf", b2)], w=[sk])
            P.dma("gpsimd", smp["s_s5"][ri, :, k * 1024:(k + 1) * 1024], buf, r=[sk], w=[("s5out", ri, k)])


def _rwkv_sample(self, st, oFM, smp):
    P = self.P
    B = self.rwkv_alloc(st)
    PREV = self.sb("q_PREV", [128, 26, 16], F32, st)
    self.pnew = self.sb("q_pnew", [128, 26, 16], F32, st)
    SH = self.sb("q_SH", [16, PROJ], F32, st)
    Sin = [self.sb("q_Sin%d" % i, [64, 16, 64], F32, st) for i in range(2)]
    Sout = [self.sb("q_Sout%d" % i, [64, 16, 64], F32, st) for i in range(2)]
    Tb = [self.sb("q_Tb%d" % i, [128, 8, 64], F32, st) for i in range(2)]
    idf = self.cv("ident")
    P.dma("sync", SH[:], smp["st_shift"][:, :], w=["SH"])
    for c0 in range(0, 26, 8):
        cn = min(8, 26 - c0)
        b = self.bank()
        for c in range(cn):
            P.pe(TR(self.psf[:, b, c * 16:(c + 1) * 16], SH[:, (c0 + c) * 128:(c0 + c + 1) * 128], idf[0:16, 0:16]), r=["SH", "cst"], w=[("pf", b)])
        P.vec(CP(PREV[:, c0:c0 + cn, :], self.psf[:, b, 0:cn * 16].rearrange("p (a b) -> p a b", b=16)), r=[("pf", b)], w=["prev"])

    def Tstates(t):
        i = t % 2
        P.dma("sync", Sin[i][:], smp["st_rwkv"][t].rearrange("h i j -> i h j"), w=[("Sin", i)])
        for half in range(2):
            b = self.bank(0, 2)
            for q in range(4):
                hp = half * 4 + q
                P.pe(TR(self.psf[:, b, q * 64:(q + 1) * 64], Sin[i][:, 2 * hp:2 * hp + 2, :].rearrange("p a b -> p (a b)"), idf[0:64, 0:64]),
                     r=[("Sin", i), "cst"], w=[("pf", b)])
            P.vec(CP(Tb[i][:, half * 4:half * 4 + 4, :], self.psf[:, b, 0:256].rearrange("p (a b) -> p a b", b=64)), r=[("pf", b)], w=[("Tb", i)])
        return Tb[i], ("Tb", i)

    def step_post(t, Tst, tk):
        i = t % 2
        for half in range(2):
            b = self.bank(0, 2)
            for q in range(4):
                hp = half * 4 + q
                P.pe(TR(self.psf[0:64, b, q * 128:(q + 1) * 128], Tst[:, hp, :], idf), r=[tk, "cst"], w=[("pf", b)])
            P.act(ACP(Sout[i][:, half * 8:half * 8 + 8, :].rearrange("p a b -> p (a b)"), self.psf[0:64, b, :]), r=[("pf", b)], w=[("Sout", i)])
        P.dma("gpsimd", smp["s_rwkv"][t].rearrange("h i j -> i h j"), Sout[i][:], r=[("Sout", i)], w=[("srwkv", t)])

    self.rwkv_subtile(B, 0, oFM, ntok=16, prev=PREV, Tstates=Tstates, step_post=step_post)
    for c0 in range(0, 26, 4):
        cn = min(4, 26 - c0)
        b = self.bank()
        for c in range(cn):
            P.pe(TR(self.psf[0:16, b, c * 128:(c + 1) * 128], self.pnew[:, c0 + c, :], idf), r=["pnew", "cst"], w=[("pf", b)])
        P.vec(CP(SH[:, c0 * 128:(c0 + cn) * 128], self.psf[0:16, b, 0:cn * 128]), r=[("pf", b)], w=["SH"])
    P.dma("gpsimd", smp["s_shift"][:, :], SH[:], r=["SH"], w=["sshift"])


def _layer1_sample(self, smp):
    from contextlib import ExitStack
    P = self.P
    CC = make_consts()
    NT = 16
    P.barrier()
    with ExitStack() as st:
        Qp = self.sb("z_Qp", [128, 2, NT], F32, st)
        Kp = self.sb("z_Kp", [128, 2, NT], F32, st)
        TA = self.sb("z_TA", [128, NT], F32, st)
        TB = self.sb("z_TB", [128, NT], F32, st)
        Qf = self.sb("z_Qf", [128, 2, NT], F32, st)
        Kf = self.sb("z_Kf", [128, 2, NT], F32, st)
        Kr = self.sb("z_Kr", [128, 2, NT], BF16, st)
        QK = self.sb("z_QK", [128, 2, NT], F32, st)
        QM = self.sb("z_QM", [128, 2, NT], F32, st)
        KT = self.sb("z_KT", [NT, 256], BF16, st)
        KTm = self.sb("z_KTm", [NT, 256], BF16, st)
        V = self.sb("z_V", [NT, 512], BF16, st)
        GS = self.sb("z_GS", [NT, 512], BF16, st)
        Sf = [self.sb("z_Sf%d" % i, [128, 2, 512], F32, st) for i in range(3)]
        ATTc = self.sb("z_ATT", [NT, 1], F32, st)
        O = self.sb("z_O", [NT, 512], F32, st)
        TMP = self.sb("z_TMP", [NT, 512], F32, st)
        GA = self.sb("z_GA", [NT, 512], BF16, st)
        GAT = self.sb("z_GAT", [128, 4, NT], BF16, st)
        gnb = self.sb("z_gnb", [NT, 512], F32, st)
        rs = self.sb("z_rot", [128, 4, NT], F32, st)
        r1 = self.sb("z_rot1", [128, 4, 1], F32, st)
        P.dma("sync", r1[:], self.rot_d[:, :, self.SEQ:self.SEQ + 1], w=["r1"], allow_slow_non_contiguous=True)
        P.vec(CP(rs[:], r1[:].to_broadcast([128, 4, NT])), r=["r1"], w=["rs"])
        cos, sin, cos16, sin16 = (rs[:, i, :] for i in range(4))
        nb = 0
        for h in range(RH):
            g1 = float(CC["g1"][h])
            P.dma("sync", gnb[:], self.retgn_d[h, :].partition_broadcast(NT), w=["gnb"])
            for which, dst, (c_, s_), rdst in ((0, Qp, (cos, sin), Qf), (1, Kp, (cos16, sin16), Kf)):
                wq, kq = self.wload(self.wc_d[h, which], 16, 256)
                nm = "QK"[which]
                for dkc in range(2):
                    b = self.bank()
                    for kc in range(16):
                        P.pe(MM(self.psf[:, b, 0:NT], wq[:, kc, dkc * 128:(dkc + 1) * 128], self.hT[:, kc, 0:NT], kc == 0, kc == 15),
                             r=[kq, ("hT", kc)], w=[("pf", b)])
                    P.act(ACP(dst[:, dkc, :], self.psf[:, b, 0:NT]), r=[("pf", b)], w=[(nm + "p", dkc)])
                P.vec(TT(TA[:], dst[:, 0, :], c_, ALU.mult), r=[(nm + "p", 0), "rs"], w=["TA"])
                P.vec(TT(TB[:], dst[:, 1, :], s_, ALU.mult), r=[(nm + "p", 1), "rs"], w=["TB"])
                P.vec(TT(rdst[:, 0, :], TA[:], TB[:], ALU.subtract), r=["TA", "TB"], w=[(nm + "r", 0)])
                P.vec(TT(TA[:], dst[:, 1, :], c_, ALU.mult), r=[(nm + "p", 1), "rs"], w=["TA"])
                P.vec(TT(TB[:], dst[:, 0, :], s_, ALU.mult), r=[(nm + "p", 0), "rs"], w=["TB"])
                P.vec(TT(rdst[:, 1, :], TA[:], TB[:], ALU.add), r=["TA", "TB"], w=[(nm + "r", 1)])
            P.vec(CP(Kr[:], Kf[:]), r=[("Kr", 0), ("Kr", 1)], w=["Krb"])
            P.vec(TT(QK[:], Qf[:], Kf[:], ALU.mult), r=[("Qr", 0), ("Qr", 1), ("Kr", 0), ("Kr", 1)], w=["QKp"])
            ba = self.bank()
            for dkc in range(2):
                P.pe(MM(self.psf[0:NT, ba, 0:2], QK[:, dkc, :], self.epsc[:, 2:4], dkc == 0, dkc == 1), r=["QKp", "epsc"], w=[("pf", ba)])
            P.vec(CP(ATTc[:], self.psf[0:NT, ba, 1:2]), r=[("pf", ba)], w=["ATTc"])
            for dkc in range(2):
                P.pe(TR(self.psb[0:NT, 1, dkc * 128:(dkc + 1) * 128], Kr[:, dkc, :], self.identb[:]), r=["Krb", "identb"], w=[("pb", 1)])
            P.vec(CP(KT[:], self.psb[0:NT, 1, 0:256]), r=[("pb", 1)], w=["KT"])
            for which, dst, nm in ((2, V, "V"), (4, GS, "GS")):
                for vg in range(2):
                    wv, kv = self.wload(self.wc_d[h, which + vg], 16, 256)
                    b = self.bank()
                    for kc in range(16):
                        P.pe(MM(self.psf[0:NT, b, 0:256], self.hT[:, kc, 0:NT], wv[:, kc, :], kc == 0, kc == 15), r=[kv, ("hT", kc)], w=[("pf", b)])
                    if nm == "V":
                        P.act(ACP(dst[:, vg * 256:(vg + 1) * 256], self.psf[0:NT, b, 0:256]), r=[("pf", b)], w=[nm])
                    else:
                        P.act(ACTF(dst[:, vg * 256:(vg + 1) * 256], self.psf[0:NT, b, 0:256], AF.Silu), r=[("pf", b)], w=[nm])
            bcx = 5
            for bb in range(NT):
                S = Sf[nb % 3]
                sk = ("Sf", nb % 3)
                nb += 1
                P.dma("sync", S[:], smp["st_ret"][bb, h].rearrange("(a p) e -> p a e", p=128), w=[sk])
                P.vec(TT(QM[:], Qf[:], self.cself[:, 15 - bb:31 - bb].rearrange("p (a b) -> p a b", a=1).to_broadcast([128, 2, NT]), ALU.mult),
                       r=[("Qr", 0), ("Qr", 1), "cself"], w=["QM"])
                for dkc in range(2):
                    P.pe(MM(self.psf[0:NT, bcx, :], QM[:, dkc, :], S[:, dkc, :], bb == 0 and dkc == 0, bb == NT - 1 and dkc == 1),
                         r=["QM", sk], w=[("pf", bcx)])
                io_, _ = COFF["ident"]
                P.vec(TS(KTm[:], KT[:], self.cst[0:NT, io_ + bb:io_ + bb + 1], ALU.mult), r=["KT", "cst"], w=["KTm"])
                for dkc in range(2):
                    bs = self.bank()
                    P.pe(MM(self.psf[:, bs, :], KTm[:, dkc * 128:(dkc + 1) * 128], V[:, :]), r=["KTm", "V"], w=[("pf", bs)])
                    P.vec(STT(S[:, dkc, :], S[:, dkc, :], g1, self.psf[:, bs, :], ALU.mult, ALU.add), r=[("pf", bs), sk], w=[sk])
                P.dma("gpsimd", smp["s_ret"][bb, h].rearrange("(a p) e -> p a e", p=128), S[:], r=[sk], w=[("sret", bb, h)])
            P.vec(TS(TMP[:], self.psf[0:NT, bcx, :], g1, ALU.mult), r=[("pf", bcx)], w=["TMP"])
            P.vec(STT(O[:], V[:, :], ATTc[:, 0:1], TMP[:], ALU.mult, ALU.add), r=["V", "ATTc", "TMP"], w=["O"])
            P.act(ACTF(self.junk[0:NT, 0:512], O[:], AF.Square, accum_out=self.stat[0:NT, 8:9]), r=["O"], w=["junk", "stat8"])
            P.act(ACTF(self.stat[0:NT, 9:10], self.stat[0:NT, 8:9], AF.Ln, scale=1.0 / DV, bias=self.epsc[0:NT, 0:1]), r=["stat8", "epsc"], w=["stat9"])
            P.act(ACTF(self.stat[0:NT, 10:11], self.stat[0:NT, 9:10], AF.Exp, scale=-0.5), r=["stat9"], w=["stat10"])
            P.vec(STT(TMP[:], O[:], self.stat[0:NT, 10:11], gnb[:], ALU.mult, ALU.mult), r=["O", "stat10", "gnb"], w=["TMP"])
            P.vec(TT(GA[:], TMP[:], GS[:, :], ALU.mult), r=["TMP", "GS"], w=["GA"])
            for ec in range(4):
                P.pe(TR(self.psb[:, 0, ec * 128:ec * 128 + NT], GA[:, ec * 128:(ec + 1) * 128], self.identb[0:NT, 0:NT]), r=["GA", "identb"], w=[("pb", 0)])
            for ec in range(4):
                P.vec(CP(GAT[:, ec, :], self.psb[:, 0, ec * 128:ec * 128 + NT]), r=[("pb", 0)], w=[("GAT", ec)])
            wo0, k0 = self.wload(self.woc_d[h, 0], 2, D)
            wo1, k1 = self.wload(self.woc_d[h, 1], 2, D)
            self.tm_accum(lambda kc, s: GAT[:, kc, 0:NT], 4,
                          lambda kc, cb: (wo0 if kc < 2 else wo1)[:, kc % 2, cb * 512:(cb + 1) * 512],
                          [k0, k1], [("GAT", e) for e in range(4)], ntok=NT, nsub=1)
        P.barrier()


Builder.s5_sample_step = _s5_sample_step
Builder.rwkv_sample = _rwkv_sample
Builder.layer1_sample = _layer1_sample


def _rwkv_chunk_core(self, B):
    P = self.P
    R, K, V, A, KK, BB, KM, T1, T2 = (B[n] for n in ("R", "K", "V", "A", "KK", "BB", "KM", "T1", "T2"))
    KR, BT, KTl, BH, KH, BHT, KHT, GC = (B[n] for n in ("KR", "BT", "KTl", "BH", "KH", "BHT", "KHT", "GC"))
    VT = B["VT"]
    LW, CUM, E1 = A, T2, V
    c4 = lambda X: X[:].rearrange("p a (c t) -> p a c t", t=64)
    cv = self.cv
    for hp in range(8):
        P.vec(lambda e, hp=hp: e.tensor_tensor_scan(out=CUM[:, hp, :], data0=cv("rmask"), data1=LW[:, hp, :], initial=0.0,
                                                    op0=ALU.mult, op1=ALU.add), r=["bA", "cst"], w=["bT2"])
    P.act(ACTF(E1[:], CUM[:], AF.Exp), r=["bT2", "bVb"], w=["bV"])
    P.vec(TT(KR[:, :, :, 1, :], c4(R), c4(E1), ALU.mult), r=["bR", "bV"], w=["KR"])
    P.vec(TT(E1[:], CUM[:], LW[:], ALU.subtract), r=["bT2", "bA", "KR"], w=["bV"])
    P.act(ACTF(E1[:], E1[:], AF.Exp), r=["bV"], w=["bV"])
    P.vec(TT(KR[:, :, :, 0, :], c4(KK), c4(E1), ALU.mult), r=["bKK", "bV"], w=["KR"])
    P.act(ACTF(E1[:], CUM[:], AF.Exp, scale=-1.0), r=["bT2", "KR"], w=["bV"])
    P.vec(TT(BT[:], BB[:], E1[:], ALU.mult), r=["bBB", "bV"], w=["BT"])
    P.vec(TT(KTl[:], KM[:], E1[:], ALU.mult), r=["bKM", "bV"], w=["KTl"])
    cumc = c4(CUM)[:, :, :, 63:64]
    P.vec(TT(c4(E1), cumc.to_broadcast([128, 8, 2, 64]), c4(CUM), ALU.subtract), r=["bT2", "BT", "KTl"], w=["bV"])
    P.act(ACTF(E1[:], E1[:], AF.Exp), r=["bV"], w=["bV"])
    P.vec(TT(BH[:], BB[:], E1[:], ALU.mult), r=["bBB", "bV"], w=["BH"])
    P.vec(TT(KH[:], KM[:], E1[:], ALU.mult), r=["bKM", "bV"], w=["KH"])
    P.act(ACTF(GC[:], cumc.rearrange("p a c o -> p a (c o)"), AF.Exp), r=["bT2"], w=["GC"])
    for src, dst, nm, bk in ((BH, BHT, "BH", 1), (KH, KHT, "KH", 0)):
        for hp in range(8):
            P.pe(TR(self.psb[:, bk, hp * 128:(hp + 1) * 128], src[:, hp, :], self.identb[:]), r=[nm, "identb"], w=[("pb", bk)])
        P.vec(CP(dst[:], self.psb[:, bk, :].rearrange("p (a b) -> p a b", b=128)), r=[("pb", bk)], w=[nm + "T"])
    P.barrier()
    import os
    CHSTOP = int(os.environ.get("CHSTOP", "99"))
    if CHSTOP <= 1:
        return
    bv = lambda X: X[:].bitcast(BF16).rearrange("p a (h t) -> p (a h) t", t=64)
    XA, XB = bv(R)[:, 0:16, :], bv(R)[:, 16:32, :]
    XtA, XtB = bv(KK)[:, 0:16, :], bv(KK)[:, 16:32, :]
    PA, PB = bv(BB)[:, 0:16, :], bv(BB)[:, 16:32, :]
    AkT, RbT = bv(KM)[:, 0:16, :], bv(KM)[:, 16:32, :]
    RkT, NW = bv(A)[:, 0:16, :], bv(A)[:, 16:32, :]
    Ub = bv(T2)[:, 0:16, :]
    Ttmp = V[:, 0:4, :].rearrange("p a (b c) -> p (a b) c", c=64)
    mb = lambda nm, n: cv(nm).rearrange("p (o t) -> p o t", o=1).to_broadcast([128, n, 64])
    for qd in range(4):
        b1, b2, b3 = (0, 1, 2) if qd % 2 == 0 else (3, 4, 5)
        for par in range(2):
            pj = slice(64 * par, 64 * par + 64)
            for hpp in range(2):
                hp = 2 * qd + hpp
                hh = hpp * 2 + par
                for c in range(2):
                    pc = slice(64 * c, 64 * c + 64)
                    krc = KR[pj, hp, c, :, :].rearrange("p a t -> p (a t)")
                    P.pe(MM(self.psf[pc, b1, hh * 128:(hh + 1) * 128], BT[pj, hp, pc], krc), r=["BT", "KR"], w=[("pf", b1)], rg=("j", par))
                    P.pe(MM(self.psf[pc, b2, hh * 128:(hh + 1) * 128], KTl[pj, hp, pc], krc), r=["KTl", "KR"], w=[("pf", b2)], rg=("j", par))
                    P.pe(MM(self.psf[pc, b3, hh * 64:(hh + 1) * 64], KR[pj, hp, c, 0, :], BT[pj, hp, pc]), r=["BT", "KR"], w=[("pf", b3)], rg=("j", par))
        hs = slice(4 * qd, 4 * qd + 4)
        if os.environ.get("CHM") == "1":
            continue
        m1 = self.psf[:, b1, :].rearrange("p (h a t) -> p h a t", a=2, t=64)
        m2 = self.psf[:, b2, :].rearrange("p (h a t) -> p h a t", a=2, t=64)
        m3 = self.psf[:, b3, 0:256].rearrange("p (h t) -> p h t", t=64)
        P.vec(TT(XA[:, hs, :], m1[:, :, 0, :], mb("nsu", 4), ALU.mult), r=[("pf", b1), "cst"], w=["XA"])
        P.vec(TT(RbT[:, hs, :], m1[:, :, 1, :], mb("iu", 4), ALU.mult), r=[("pf", b1), "cst"], w=["RbT"])
        P.vec(TT(AkT[:, hs, :], m2[:, :, 0, :], mb("su", 4), ALU.mult), r=[("pf", b2), "cst"], w=["AkT"])
        P.vec(TT(RkT[:, hs, :], m2[:, :, 1, :], mb("iu", 4), ALU.mult), r=[("pf", b2), "cst"], w=["RkT"])
        P.vec(TT(XtA[:, hs, :], m3, mb("nsl", 4), ALU.mult), r=[("pf", b3), "cst"], w=["XtA"])
    if CHSTOP <= 2:
        P.barrier()
        return
    P.pool(TT(PA, XA, mb("i64", 16), ALU.add), r=["XA", "cst"], w=["PA"])
    Xc, Xtc, Xn, Xtn = (XA, "XA"), (XtA, "XtA"), (XB, "XB"), (XtB, "XtB")
    Pc, Pn = (PA, "PA"), (PB, "PB")
    bview = lambda b: self.psf[:, b, :].rearrange("p (h t) -> p h t", t=64)
    for lvl in range(5):
        last = lvl == 4
        for c in range(2):
            pc = slice(64 * c, 64 * c + 64)
            for h in range(16):
                col = slice((h % 8) * 64, (h % 8) * 64 + 64)
                if not last:
                    P.pe(MM(self.psf[pc, 0 + h // 8, col], Xtc[0][pc, h, :], Xc[0][pc, h, :]), r=[Xtc[1], Xc[1]], w=[("pf", 0 + h // 8)], rg=("t", c))
                P.pe(MM(self.psf[pc, 2 + h // 8, col], Xc[0][pc, h, :], Xtc[0][pc, h, :]), r=[Xtc[1], Xc[1]], w=[("pf", 2 + h // 8)], rg=("t", c))
        for hb in range(2):
            hs = slice(8 * hb, 8 * hb + 8)
            if not last:
                P.act(ACP(Xn[0][:, hs, :], bview(0 + hb)), r=[("pf", 0 + hb)], w=[Xn[1]])
            P.vec(CP(Xtn[0][:, hs, :], bview(2 + hb)), r=[("pf", 2 + hb)], w=[Xtn[1]])
        for c in range(2):
            pc = slice(64 * c, 64 * c + 64)
            for h in range(16):
                col = slice((h % 8) * 64, (h % 8) * 64 + 64)
                P.pe(MM(self.psf[pc, 4 + h // 8, col], Xtn[0][pc, h, :], Pc[0][pc, h, :]), r=[Xtn[1], Pc[1]], w=[("pf", 4 + h // 8)], rg=("t", c))
        for hb in range(2):
            hs = slice(8 * hb, 8 * hb + 8)
            P.vec(TT(Pn[0][:, hs, :], bview(4 + hb), Pc[0][:, hs, :], ALU.add), r=[("pf", 4 + hb), Pc[1]], w=[Pn[1]])
        Xc, Xn = Xn, Xc
        Xtc, Xtn = Xtn, Xtc
        Pc, Pn = Pn, Pc
    MT = Pc
    if CHSTOP <= 3:
        P.barrier()
        return
    Tst, Tb = self.Tst, self.Tb
    for c in range(2):
        pc = slice(64 * c, 64 * c + 64)
        hcol = lambda h: slice((h % 8) * 64, (h % 8) * 64 + 64)
        first = {0: True, 1: True}
        for par in range(2):
            pj = slice(64 * par, 64 * par + 64)
            for hp in range(8):
                h = 2 * hp + par
                P.pe(MM(self.psf[pc, h // 8, hcol(h)], KR[pj, hp, c, 0, :], Tb[pj, hp, :], first[h // 8], False, sgc=True),
                     r=["KR", "Tb"], w=[("pf", h // 8)], rg=("j", par))
                first[h // 8] = False
        for h in range(16):
            hp, par = h // 2, h % 2
            P.pe(MM(self.psf[pc, h // 8, hcol(h)], AkT[pc, h, :], VT[pc, hp, 64 * par:64 * par + 64], False, True, sgc=True),
                 r=["AkT", "bVT"], w=[("pf", h // 8)], rg=("t", c))
        for hb in range(2):
            hs = slice(8 * hb, 8 * hb + 8)
            P.act(ACTF(NW[pc, hs, :], bview(hb)[pc], AF.Copy, scale=-1.0), r=[("pf", hb)], w=["NW"])
        for h in range(16):
            P.pe(MM(self.psf[pc, 2 + h // 8, hcol(h)], MT[0][pc, h, :], NW[pc, h, :]), r=[MT[1], "NW"], w=[("pf", 2 + h // 8)], rg=("t", c))
        for hb in range(2):
            hs = slice(8 * hb, 8 * hb + 8)
            P.vec(CP(Ub[pc, hs, :], bview(2 + hb)[pc]), r=[("pf", 2 + hb)], w=["Ub"])
        first = {0: True, 1: True}
        for par in range(2):
            pj = slice(64 * par, 64 * par + 64)
            for hp in range(8):
                h = 2 * hp + par
                P.pe(MM(self.psf[pc, 4 + h // 8, hcol(h)], KR[pj, hp, c, 1, :], Tb[pj, hp, :], first[h // 8], False, sgc=True),
                     r=["KR", "Tb"], w=[("pf", 4 + h // 8)], rg=("j", par))
                first[h // 8] = False
        for h in range(16):
            hp, par = h // 2, h % 2
            vt = VT[pc, hp, 64 * par:64 * par + 64]
            P.pe(MM(self.psf[pc, 4 + h // 8, hcol(h)], RbT[pc, h, :], Ub[pc, h, :], False, False, sgc=True), r=["RbT", "Ub"], w=[("pf", 4 + h // 8)], rg=("t", c))
            P.pe(MM(self.psf[pc, 4 + h // 8, hcol(h)], RkT[pc, h, :], vt, False, True, sgc=True), r=["RkT", "bVT"], w=[("pf", 4 + h // 8)], rg=("t", c))
        for h in range(16):
            hp, par = h // 2, h % 2
            pj = slice(64 * par, 64 * par + 64)
            vt = VT[pc, hp, 64 * par:64 * par + 64]
            P.pe(MM(self.psf[pj, 0, hp * 64:(hp + 1) * 64], BHT[pc, hp, pj], Ub[pc, h, :], True, False), r=["BHT", "Ub"], w=[("pf", 0)], rg=("t", c))
            P.pe(MM(self.psf[pj, 0, hp * 64:(hp + 1) * 64], KHT[pc, hp, pj], vt, False, True), r=["KHT", "bVT"], w=[("pf", 0)], rg=("t", c))
        gcb = GC[:, :, c:c + 1].to_broadcast([128, 8, 64])
        P.pool(TT(Ttmp, Tst[:], gcb, ALU.mult), r=["Tst", "GC"], w=["Ttmp"])
        P.vec(TT(Tst[:], Ttmp, bview(0), ALU.add), r=["Ttmp", ("pf", 0)], w=["Tst"])
        P.act(ACP(Tb[:], Tst[:]), r=["Tst"], w=["Tb"])
    P.barrier()


Builder.rwkv_chunk_core = _rwkv_chunk_core
```

```python
import math
import numpy as np
import concourse.bass as bass
import concourse.mybir as mybir
from concourse.alu_op_type import AluOpType as ALU
from concourse.bass_utils import run_bass_kernel_spmd

F32 = mybir.dt.float32
BF16 = mybir.dt.bfloat16
I32 = mybir.dt.int32
AF = mybir.ActivationFunctionType
AX = mybir.AxisListType

D = 2048
W = 1024
HR = 16
PROJ = 3328
INA = 4352
DFF = 5632
RH = 8
DK = 256
DV = 512
INC = 12288
PAST = 16384
EPS = 1e-6
GN_EPS = 64e-5

ENGS = ("tensor", "vector", "scalar", "gpsimd", "sync")
CHUNKED = True
USE_SCRATCH = True


class Prog:
    def __init__(self, nc, stack):
        self.nc = nc
        self.ops = {e: [] for e in ENGS}
        self.seq = {e: 0 for e in ENGS}
        self.seen = {e: {} for e in ENGS}
        self.lastw = {}
        self.readers = {}
        self.esem = {e: stack.enter_context(nc.semaphore("es_" + e)) for e in ENGS}
        self.ndsem = 24
        self.dsem = [stack.enter_context(nc.semaphore("ds%d" % i)) for i in range(self.ndsem)]
        self.dcnt = [0] * self.ndsem
        self.drr = 0
        self.pending_dma = []

    def _need(self, eng, ev, waits, force=False):
        key, sem, val, src = ev
        if self.seen[eng].get(key, 0) >= val:
            return
        self.seen[eng][key] = val
        waits.append((sem, val))

    def op(self, eng, fn, r=(), w=(), dma=False, rg=None):
        waits = []
        if eng == "tensor":
            pfrg = self.__dict__.setdefault("pfrg", {})
            for k in w:
                last = pfrg.get(k)
                if last is not None and last[0] != rg:
                    self._need(eng, last[1], waits)
        for k in r:
            ev = self.lastw.get(k)
            if ev is not None:
                if ev[3] == eng and not dma and eng == "tensor":
                    pass
                else:
                    self._need(eng, ev, waits)
        for k in r:
            if isinstance(k, tuple) and k[0] in ("pf", "pb"):
                for ev2 in self.readers.get(k, ()):
                    if ev2[3] != eng:
                        self._need(eng, ev2, waits)
        for k in w:
            ev = self.lastw.get(k)
            if ev is not None and (dma or ev[3] != eng):
                self._need(eng, ev, waits)
            for ev2 in self.readers.get(k, ()):
                if dma or ev2[3] != eng:
                    self._need(eng, ev2, waits)
        if dma:
            i = self.drr
            self.drr = (self.drr + 1) % self.ndsem
            if self.dcnt[i] > 0:
                self._need(eng, (("D", i), self.dsem[i], self.dcnt[i], None), waits)
            self.dcnt[i] += 16
            ev = (("D", i), self.dsem[i], self.dcnt[i], None)
            inc = (self.dsem[i], 16)
            self.pending_dma.append(ev)
        else:
            self.seq[eng] += 1
            ev = (("E", eng), self.esem[eng], self.seq[eng], eng)
            inc = (self.esem[eng], 1)
            self.seen[eng][("E", eng)] = max(self.seen[eng].get(("E", eng), 0), 0)
        for k in r:
            self.readers.setdefault(k, []).append(ev)
        for k in w:
            self.lastw[k] = ev
            self.readers[k] = []
            if eng == "tensor":
                self.pfrg[k] = (rg, ev)
        self.nrec = getattr(self, "nrec", 0) + 1
        import os
        if self.nrec > int(os.environ.get("KLIMIT", "100000000")):
            fn = lambda e: e.nop()
        self.ops[eng].append((waits, fn, inc))
        return ev

    def pe(self, fn, r=(), w=(), rg=None):
        return self.op("tensor", fn, r, w, rg=rg)

    def vec(self, fn, r=(), w=()):
        return self.op("vector", fn, r, w)

    def act(self, fn, r=(), w=()):
        return self.op("scalar", fn, r, w)

    def pool(self, fn, r=(), w=()):
        return self.op("gpsimd", fn, r, w)

    def dma(self, q, out, in_, r=(), w=(), **kw):
        return self.op(q, lambda e: e.dma_start(out=out, in_=in_, **kw), r, w, dma=True)

    def barrier(self):
        evs = [(("E", e), self.esem[e], self.seq[e], e) for e in ENGS if self.seq[e] > 0]
        evs += [(("D", i), self.dsem[i], self.dcnt[i], None) for i in range(self.ndsem) if self.dcnt[i] > 0]
        for e in ENGS:
            waits = []
            for ev in evs:
                if ev[3] == e:
                    continue
                self._need(e, ev, waits)
            if waits:
                self.ops[e].append((waits, None, None))
        self.lastw = {}
        self.readers = {}
        self.pfrg = {}

    def replay(self, eng, e):
        for waits, fn, inc in self.ops[eng]:
            for sem, val in waits:
                e.wait_ge(sem, val)
            if fn is not None:
                ins = fn(e)
                ins.then_inc(inc[0], inc[1])


def TT(out, in0, in1, op):
    return lambda e: e.tensor_tensor(out=out, in0=in0, in1=in1, op=op)


def TS(out, in0, s1, op0, s2=None, op1=None):
    if op1 is None:
        return lambda e: e.tensor_scalar(out=out, in0=in0, scalar1=s1, scalar2=None, op0=op0)
    return lambda e: e.tensor_scalar(out=out, in0=in0, scalar1=s1, scalar2=s2, op0=op0, op1=op1)


def STT(out, in0, scalar, in1, op0, op1):
    return lambda e: e.scalar_tensor_tensor(out=out, in0=in0, scalar=scalar, in1=in1, op0=op0, op1=op1)


def ACTF(out, in_, func, bias=None, scale=None, accum_out=None):
    kw = {}
    if bias is not None:
        kw["bias"] = bias
    if scale is not None:
        kw["scale"] = scale
    if accum_out is not None:
        kw["accum_out"] = accum_out
    return lambda e: e.activation(out=out, in_=in_, func=func, **kw)


def ACP(out, in_):
    return lambda e: e.activation(out=out, in_=in_, func=AF.Copy)


def CP(out, in_):
    return lambda e: e.tensor_copy(out=out, in_=in_)


def MM(out, lhsT, rhs, start=True, stop=True, sgc=False):
    if sgc:
        return lambda e: e.matmul(out, lhsT=lhsT, rhs=rhs, start=start, stop=stop, skip_group_check=True)
    return lambda e: e.matmul(out, lhsT=lhsT, rhs=rhs, start=start, stop=stop)


def TR(out, in_, ident):
    return lambda e: e.transpose(out, in_, ident)


def MS(ap, val):
    return lambda e: e.memset(ap, val)


def fm(v):
    v = np.asarray(v, np.float32).reshape(-1, 128)
    return np.ascontiguousarray(v.T)


def tile_w(w, gw):
    K, N = w.shape
    kc = K // 128
    g = N // gw
    t = w.reshape(kc, 128, g, gw).transpose(2, 1, 0, 3)
    return np.ascontiguousarray(t).reshape(g, 128, kc * gw)


def tile_rows(w, rk):
    K, N = w.shape
    g = K // (128 * rk)
    t = w.reshape(g, rk, 128, N).transpose(0, 2, 1, 3)
    return np.ascontiguousarray(t).reshape(g, 128, rk * N)


def make_consts():
    c = {}
    c["ident"] = np.eye(128, dtype=np.float32)
    bo = np.zeros((128, 128), np.float32)
    bo[:64, :64] = 1.0
    bo[64:, 64:] = 1.0
    c["bones"] = bo
    c["iota"] = np.broadcast_to(np.arange(512, dtype=np.float32)[None, :], (128, 512)).copy()
    log_g = np.log(1.0 - np.exp2(-5.0 - np.arange(RH, dtype=np.float64)))
    idx = np.arange(128, dtype=np.float64)
    dist = idx[None, :] - idx[:, None]
    intra = np.where(dist >= 0, np.exp(log_g[:, None, None] * np.maximum(dist, 0.0)), 0.0)
    c["intraT"] = np.ascontiguousarray(intra.transpose(1, 0, 2)).astype(np.float32).reshape(128, RH * 128)
    c["qs"] = np.exp(log_g[None, :] * (idx[:, None] + 1.0)).astype(np.float32)
    c["ks"] = np.exp(log_g[None, :] * (127.0 - idx[:, None])).astype(np.float32)
    pp = np.arange(128)
    c["m0"] = ((pp // 32) % 2 == 0).astype(np.float32).reshape(128, 1)
    c["m1"] = ((pp // 32) % 2 == 1).astype(np.float32).reshape(128, 1)
    r64 = (pp % 64)[:, None]
    t64 = np.arange(64)[None, :]
    c["nsu"] = -(t64 > r64).astype(np.float32)
    c["su"] = (t64 > r64).astype(np.float32)
    c["nsl"] = -(t64 < r64).astype(np.float32)
    c["iu"] = (t64 >= r64).astype(np.float32)
    c["i64"] = (t64 == r64).astype(np.float32)
    c["rmask"] = np.broadcast_to((np.arange(128) % 64 != 0).astype(np.float32)[None, :], (128, 128)).copy()
    c["cd"] = np.exp(log_g * 128.0)
    c["g1"] = np.exp(log_g)
    return c


def rot_tables(pos):
    half = 128
    freq = (1.0 / (10000.0 ** np.linspace(0.0, 1.0, half, dtype=np.float32))).astype(np.float32)
    ang = (np.asarray(pos, np.float32)[None, :] * freq[:, None]).astype(np.float32)
    cs = np.cos(ang).astype(np.float32)
    sn = np.sin(ang).astype(np.float32)
    return np.ascontiguousarray(np.stack([cs, sn, cs / 16.0, sn / 16.0], axis=1).astype(np.float32))


CONST_LAYOUT = [("ident", 128), ("bones", 128), ("iota", 512), ("intraT", RH * 128), ("qs", RH), ("ks", RH), ("m0", 1), ("m1", 1), ("nsu", 64), ("su", 64), ("nsl", 64), ("iu", 64), ("i64", 64), ("rmask", 128)]
VEC_LAYOUT = [("nm0", 16), ("nf0", 16), ("nm1", 16), ("nf1", 16), ("mu", 26), ("w0", 8), ("a0", 8), ("kk", 8),
              ("ka", 8), ("rk", 8), ("lnw", 8), ("lnb", 8), ("s5d", 8), ("bglu", 8)]


def _offsets(layout):
    o = {}
    p = 0
    for n, k in layout:
        o[n] = (p, k)
        p += k
    return o, p


COFF, NCONST = _offsets(CONST_LAYOUT)
VOFF, NVEC = _offsets(VEC_LAYOUT)


class Builder:
    def __init__(self, seq, nsub, dbg=(), stages=None, with_sample=True):
        from contextlib import ExitStack
        self.SEQ = seq
        self.NSUB = nsub
        self.T = nsub * 128
        self.NTILE = seq // self.T
        self.dbg = set(dbg)
        self.stages = stages
        self.with_sample = with_sample
        self.stack = ExitStack()
        self.nc = bass.Bass("TRN2", target_bir_lowering=False)
        self.P = Prog(self.nc, self.stack)
        self.dram_in = {}
        self.dram_out = {}
        self.wslot = 0
        self.gbank = 0
        self.scr = {}
        self.scr_done = {}
        self.dbg_shapes = {}

    def din(self, name, shape, dt=F32):
        t = self.nc.dram_tensor(name, list(shape), dt, kind="ExternalInput").ap()
        self.dram_in[name] = t
        return t

    def dout(self, name, shape, dt=F32):
        t = self.nc.dram_tensor(name, list(shape), dt, kind="ExternalOutput").ap()
        self.dram_out[name] = t
        return t

    def sb(self, name, shape, dt=F32, stack=None):
        self._uid = getattr(self, "_uid", 0) + 1
        return (stack or self.stack).enter_context(self.nc.sbuf_tensor("sb%d_%s" % (self._uid, name), list(shape), dt))

    def on(self, st):
        return self.stages is None or st in self.stages

    def dump(self, name, ap, shape, r, dt=F32):
        if name not in self.dbg:
            return
        o = self.dout("dbg_" + name, shape, dt)
        self.P.dma("sync", o, ap, r=r, w=["dbg_" + name])

    def wload(self, src_ap, kc, gw, part=None):
        s = self.wslot
        self.wslot = (self.wslot + 1) % 4
        key = "wb%d" % s
        view = self.wbuf[:, s, 0:kc * gw]
        name = src_ap.tensor.name
        gidx = src_ap.offset // (128 * 4096)
        if not USE_SCRATCH:
            self.P.dma("gpsimd", view, src_ap, w=[key])
            return view.rearrange("p (k g) -> p k g", g=gw), key
        if name not in self.scr:
            ng = 1
            for d in src_ap.tensor.shape:
                ng *= d
            ng //= 128 * 4096
            self.scr[name] = self.nc.dram_tensor("scr_" + name, [ng, 128, 4096], BF16, kind="Internal").ap()
            self.scr_done[name] = set()
        sk = ("scr", name, gidx)
        if gidx not in self.scr_done[name]:
            self.scr_done[name].add(gidx)
            self.P.dma("gpsimd", view, src_ap, w=[key])
            self.P.dma("sync", self.scr[name][gidx], view, r=[key], w=[sk])
        else:
            self.P.dma("sync", view, self.scr[name][gidx], r=[sk], w=[key])
        return view.rearrange("p (k g) -> p k g", g=gw), key

    def bank(self, lo=0, hi=4):
        b = lo + (self.gbank % (hi - lo))
        self.gbank += 1
        return b

    def declare(self):
        SEQ = self.SEQ
        self.xp = self.din("xp", [SEQ, D])
        self.consts_d = self.din("consts", [128, NCONST])
        self.vecs_d = self.din("vecs", [128, NVEC])
        self.rot_d = self.din("rot", [128, 4, SEQ + 1])
        self.nfin_d = self.din("nfin", [1, D])
        self.retgn_d = self.din("retgn", [RH, DV])
        self.w2a2_d = self.din("w2a2", [128, W])
        self.g2_d = self.din("g2", [128, W])
        self.s5a_d = self.din("s5a", [128, 3, 32])
        self.s5b_d = self.din("s5b", [128, 2, 32 * 16])
        self.s5c_d = self.din("s5c", [128, 2, 32 * 64])
        if self.on("rwkv") or self.on("s5"):
            self.wa_d = self.din("wa", [17, 128, 16 * 256])
            self.wglu_d = self.din("wglu", [2, 128, 8 * 512])
            self.woa_d = self.din("woa", [8, 128, 16 * 256])
        self.wg_d, self.wu_d, self.wd_d = {}, {}, {}
        for i in range(2):
            if self.on("ffn%d" % i):
                self.wg_d[i] = self.din("wg%d" % i, [22, 128, 16 * 256])
                self.wu_d[i] = self.din("wu%d" % i, [22, 128, 16 * 256])
                self.wd_d[i] = self.din("wd%d" % i, [22, 128, 2 * D])
        if self.on("ret"):
            self.wc_d = self.din("wc", [RH, 6, 128, 16 * 256])
            self.woc_d = self.din("woc", [RH, 2, 128, 2 * D])
        if self.with_sample:
            self.xs_d = self.din("xs", [16, D])
            self.smp = {"st_rwkv": self.din("st_rwkv", [16, HR, 64, 64]), "st_shift": self.din("st_shift", [16, PROJ]),
                        "st_s5": self.din("st_s5", [2, 16, 4096]), "st_ret": self.din("st_ret", [16, RH, DK, DV]),
                        "s_rwkv": self.dout("s_rwkv", [16, HR, 64, 64]), "s_shift": self.dout("s_shift", [16, PROJ]),
                        "s_s5": self.dout("s_s5", [2, 16, 4096]), "s_ret": self.dout("s_ret", [16, RH, DK, DV])}
            self.ys = self.dout("ys", [16, D])
        self.yp = self.dout("yp", [SEQ, D])
        self.o_prwkv = self.dout("p_rwkv", [HR, 64, 64])
        self.o_pshift = self.dout("p_shift", [26, 128])
        self.o_ps5 = self.dout("p_s5", [2, 32, 128])
        self.o_pret = self.dout("p_ret", [RH, 2, 128, DV])

    def alloc(self):
        T, NSUB = self.T, self.NSUB
        nc = self.nc
        self.x = self.sb("x_tm", [128, NSUB, D])
        self.hT = self.sb("hT", [128, 16, T], BF16)
        self.wbuf = self.sb("wbuf", [128, 4, 4096], BF16)
        self.cst = self.sb("cst", [128, NCONST])
        self.vecs = self.sb("vecs", [128, NVEC])
        self.identb = self.sb("identb", [128, 128], BF16)
        self.nfin = self.sb("nfin_bc", [128, D])
        self.w2a2 = self.sb("w2a2", [128, W], BF16)
        self.g2 = self.sb("g2", [128, W], BF16)
        self.rot = self.sb("rot", [128, 4, T])
        self.junk = self.sb("junk", [128, D], BF16)
        self.stat = self.sb("stat", [128, 16])
        self.omm = self.sb("omm", [128, 26])
        self.psf = self.stack.enter_context(nc.psum_tensor("psf", [128, 6, 512], F32))
        self.psb = self.stack.enter_context(nc.psum_tensor("psb", [128, 2, 1024], BF16))

    def cv(self, name):
        o, k = COFF[name]
        return self.cst[:, o:o + k]

    def vv(self, name, c=None):
        o, k = VOFF[name]
        if c is None:
            return self.vecs[:, o:o + k]
        return self.vecs[:, o + c:o + c + 1]

    def setup(self):
        P = self.P
        P.dma("sync", self.cst[:], self.consts_d[:, :], w=["cst"])
        P.dma("sync", self.vecs[:], self.vecs_d[:, :], w=["vecs"])
        P.dma("sync", self.nfin[:], self.nfin_d[0, :].partition_broadcast(128), w=["nfin"])
        P.dma("gpsimd", self.w2a2[:], self.w2a2_d[:, :], w=["w2a2"])
        P.dma("gpsimd", self.g2[:], self.g2_d[:, :], w=["g2"])
        P.vec(CP(self.identb[:], self.cv("ident")), r=["cst"], w=["identb"])
        mo, mk = VOFF["mu"]
        P.vec(TS(self.omm[:], self.vecs[:, mo:mo + mk], -1.0, ALU.mult, 1.0, ALU.add), r=["vecs"], w=["omm"])

    def norm_to_hT(self, gname, ntok=128, nsub=None, x=None, hT=None):
        P = self.P
        nsub = self.NSUB if nsub is None else nsub
        x = self.x if x is None else x
        hT = self.hT if hT is None else hT
        for s in range(nsub):
            xs = x[0:ntok, s, :]
            ss = self.stat[0:ntok, 0:1]
            P.act(ACTF(self.junk[0:ntok, :], xs, AF.Square, accum_out=ss), r=[("x", s)], w=["junk", "stat0"])
            P.act(ACTF(self.stat[0:ntok, 1:2], ss, AF.Ln, scale=1.0 / D, bias=self.epsc[0:ntok, 0:1]), r=["stat0", "epsc"], w=["stat1"])
            P.act(ACTF(self.stat[0:ntok, 2:3], self.stat[0:ntok, 1:2], AF.Exp, scale=-0.5), r=["stat1"], w=["stat2"])
            P.vec(TS(self.junk[0:ntok, :], xs, self.stat[0:ntok, 2:3], ALU.mult), r=[("x", s), "stat2"], w=["junk"])
            for half in range(2):
                pb = ("pb", half)
                for k8 in range(8):
                    kc = half * 8 + k8
                    P.pe(TR(self.psb[:, half, k8 * 128:k8 * 128 + ntok], self.junk[0:ntok, kc * 128:(kc + 1) * 128],
                            self.identb[0:ntok, 0:ntok]), r=["junk", "identb"], w=[pb])
                for k8 in range(8):
                    kc = half * 8 + k8
                    src = self.psb[:, half, k8 * 128:k8 * 128 + ntok]
                    dst = hT[:, kc, s * 128:s * 128 + ntok]
                    g = self.vv(gname, kc)
                    if False:
                        P.act(ACTF(dst, src, AF.Identity, scale=g), r=[pb, "vecs"], w=[("hT", kc)])
                    else:
                        P.vec(TS(dst, src, g, ALU.mult), r=[pb, "vecs"], w=[("hT", kc)])

    def tm_accum(self, lhs_fn, kcn, w_fn, w_keys, lhs_keys, ntok=128, nsub=None, x=None, xkey="x", banks=(0, 4)):
        P = self.P
        nsub = self.NSUB if nsub is None else nsub
        x = self.x if x is None else x
        for s in range(nsub):
            for cb in range(4):
                b = self.bank(*banks)
                pk = ("pf", b)
                for kc in range(kcn):
                    P.pe(MM(self.psf[0:ntok, b, :], lhs_fn(kc, s), w_fn(kc, cb), start=(kc == 0), stop=(kc == kcn - 1)),
                         r=list(lhs_keys) + list(w_keys), w=[pk])
                xs = x[0:ntok, s, cb * 512:(cb + 1) * 512]
                P.vec(TT(xs, xs, self.psf[0:ntok, b, :], ALU.add), r=[pk, (xkey, s)], w=[(xkey, s)])

    def ffn(self, li, ntok=128, nsub=None, x=None, hT=None, stack=None):
        P = self.P
        nsub = self.NSUB if nsub is None else nsub
        T = nsub * ntok if ntok == 128 else ntok
        hT = self.hT if hT is None else hT
        actv = self.ffn_act
        sil = self.ffn_sil
        for g in range(22):
            wg, kg = self.wload(self.wg_d[li][g], 16, 256)
            wu, ku = self.wload(self.wu_d[li][g], 16, 256)
            wd, kd = self.wload(self.wd_d[li][g], 2, D)
            for m in range(2):
                bg = self.bank(0, 6)
                bu = self.bank(0, 6)
                for kc in range(16):
                    P.pe(MM(self.psf[:, bg, 0:T], wg[:, kc, m * 128:(m + 1) * 128], hT[:, kc, 0:T], kc == 0, kc == 15),
                         r=[kg, ("hT", kc)], w=[("pf", bg)])
                for kc in range(16):
                    P.pe(MM(self.psf[:, bu, 0:T], wu[:, kc, m * 128:(m + 1) * 128], hT[:, kc, 0:T], kc == 0, kc == 15),
                         r=[ku, ("hT", kc)], w=[("pf", bu)])
                P.act(ACTF(sil[:, 0:T], self.psf[:, bg, 0:T], AF.Silu), r=[("pf", bg)], w=["sil"])
                P.vec(TT(actv[:, m, 0:T], sil[:, 0:T], self.psf[:, bu, 0:T], ALU.mult), r=["sil", ("pf", bu)], w=[("actv", m)])
            self.tm_accum(lambda kc, s: actv[:, kc, s * 128:s * 128 + ntok], 2,
                          lambda kc, cb: wd[:, kc, cb * 512:(cb + 1) * 512], [kd], [("actv", 0), ("actv", 1)],
                          ntok=ntok, nsub=nsub, x=x, banks=(0, 6))

    def outproj_a(self, oFM, ntok=128, nsub=None, x=None):
        for cb2 in range(8):
            wo, ko = self.wload(self.woa_d[cb2], 16, 256)
            P = self.P
            nsub_ = self.NSUB if nsub is None else nsub
            x_ = self.x if x is None else x
            for s in range(nsub_):
                b = self.bank(0, 6)
                pk = ("pf", b)
                for kc in range(16):
                    P.pe(MM(self.psf[0:ntok, b, 0:256], oFM[:, kc, s * 128:s * 128 + ntok], wo[:, kc, :], kc == 0, kc == 15),
                         r=[("oFM", kc), ko], w=[pk])
                xs = x_[0:ntok, s, cb2 * 256:(cb2 + 1) * 256]
                P.vec(TT(xs, xs, self.psf[0:ntok, b, 0:256], ALU.add), r=[pk, ("x", s)], w=[("x", s)])

    def final_norm(self, out_ap_fn, ntok=128, nsub=None, x=None):
        P = self.P
        nsub = self.NSUB if nsub is None else nsub
        x = self.x if x is None else x
        for s in range(nsub):
            xs = x[0:ntok, s, :]
            ss = self.stat[0:ntok, 4:5]
            P.act(ACTF(self.junk[0:ntok, :], xs, AF.Square, accum_out=ss), r=[("x", s)], w=["junk", "stat4"])
            P.act(ACTF(self.stat[0:ntok, 5:6], ss, AF.Ln, scale=1.0 / D, bias=self.epsc[0:ntok, 0:1]), r=["stat4", "epsc"], w=["stat5"])
            P.act(ACTF(self.stat[0:ntok, 6:7], self.stat[0:ntok, 5:6], AF.Exp, scale=-0.5), r=["stat5"], w=["stat6"])
            P.vec(STT(xs, xs, self.stat[0:ntok, 6:7], self.nfin[0:ntok, :], ALU.mult, ALU.mult), r=[("x", s), "stat6", "nfin"], w=[("x", s)])
            P.dma("gpsimd", out_ap_fn(s), xs, r=[("x", s)], w=["yout"])

    def layer1(self, first_tile, last_tile):
        from contextlib import ExitStack
        P = self.P
        T, NSUB = self.T, self.NSUB
        CC = make_consts()
        P.barrier()
        with ExitStack() as st:
            Qp = self.sb("r_Qp", [128, 2, T], F32, st)
            Kp = self.sb("r_Kp", [128, 2, T], F32, st)
            TA = self.sb("r_TA", [128, T], F32, st)
            TB = self.sb("r_TB", [128, T], F32, st)
            Qr2 = [self.sb("r_Qr%d" % i, [128, 2, T], BF16, st) for i in range(2)]
            Kr2 = [self.sb("r_Kr%d" % i, [128, 2, T], BF16, st) for i in range(2)]
            KT2 = [self.sb("r_KT%d" % i, [128, NSUB, 256], BF16, st) for i in range(2)]
            V2 = [self.sb("r_V%d" % i, [128, NSUB, 512], BF16, st) for i in range(2)]
            GS2 = [self.sb("r_GS%d" % i, [128, NSUB, 512], BF16, st) for i in range(2)]
            Sf = self.sb("r_Sf", [128, 2, 512], F32, st)
            Sb = self.sb("r_Sb", [128, 2, 512], BF16, st)
            ATT = self.sb("r_ATT", [128, 128], BF16, st)
            CR = self.sb("r_CR", [128, 512], F32, st)
            O = self.sb("r_O", [128, 512], F32, st)
            TMP = self.sb("r_TMP", [128, 512], F32, st)
            GA = self.sb("r_GA", [128, 512], BF16, st)
            GAT = self.sb("r_GAT", [128, 4, T], BF16, st)
            gnb = self.sb("r_gnb", [128, 512], F32, st)
            cos, sin, cos16, sin16 = (self.rot[:, i, :] for i in range(4))

            def proj(h):
                i = h % 2
                Qr, Kr, KT, V, GS = Qr2[i], Kr2[i], KT2[i], V2[i], GS2[i]
                for which, dst, (c_, s_), rdst in ((0, Qp, (cos, sin), Qr), (1, Kp, (cos16, sin16), Kr)):
                    wq, kq = self.wload(self.wc_d[h, which], 16, 256)
                    nm = "QK"[which]
                    for dkc in range(2):
                        b = self.bank(0, 3)
                        for kc in range(16):
                            P.pe(MM(self.psf[:, b, 0:T], wq[:, kc, dkc * 128:(dkc + 1) * 128], self.hT[:, kc, 0:T], kc == 0, kc == 15),
                                 r=[kq, ("hT", kc)], w=[("pf", b)])
                        P.act(ACP(dst[:, dkc, :], self.psf[:, b, 0:T]), r=[("pf", b)], w=[(nm + "p", dkc)])
                        yield
                    P.vec(TT(TA[:], dst[:, 0, :], c_, ALU.mult), r=[(nm + "p", 0), "rot"], w=["TA"])
                    P.vec(TT(TB[:], dst[:, 1, :], s_, ALU.mult), r=[(nm + "p", 1), "rot"], w=["TB"])
                    P.vec(TT(rdst[:, 0, :], TA[:], TB[:], ALU.subtract), r=["TA", "TB"], w=[(nm + "r", i, 0)])
                    P.vec(TT(TA[:], dst[:, 1, :], c_, ALU.mult), r=[(nm + "p", 1), "rot"], w=["TA"])
                    P.vec(TT(TB[:], dst[:, 0, :], s_, ALU.mult), r=[(nm + "p", 0), "rot"], w=["TB"])
                    P.vec(TT(rdst[:, 1, :], TA[:], TB[:], ALU.add), r=["TA", "TB"], w=[(nm + "r", i, 1)])
                    yield
                for s in range(NSUB):
                    for dkc in range(2):
                        slot = (s * 2 + dkc) % 8
                        pb = ("pb", 1)
                        P.pe(TR(self.psb[:, 1, slot * 128:(slot + 1) * 128], Kr[:, dkc, s * 128:(s + 1) * 128], self.identb[:]),
                             r=[("Kr", i, dkc), "identb"], w=[pb])
                    o_, k_ = COFF["ks"]
                    for dkc in range(2):
                        slot = (s * 2 + dkc) % 8
                        P.vec(TS(KT[:, s, dkc * 128:(dkc + 1) * 128], self.psb[:, 1, slot * 128:(slot + 1) * 128],
                                 self.cst[:, o_ + h:o_ + h + 1], ALU.mult), r=[("pb", 1), "cst"], w=[("KT", i, s)])
                    yield
                for which, dst, nm in ((2, V, "V"), (4, GS, "GS")):
                    for vg in range(2):
                        wv, kv = self.wload(self.wc_d[h, which + vg], 16, 256)
                        for s in range(NSUB):
                            b = self.bank(0, 3)
                            for kc in range(16):
                                P.pe(MM(self.psf[:, b, 0:256], self.hT[:, kc, s * 128:(s + 1) * 128], wv[:, kc, :], kc == 0, kc == 15),
                                     r=[kv, ("hT", kc)], w=[("pf", b)])
                            if nm == "V":
                                P.act(ACP(dst[:, s, vg * 256:(vg + 1) * 256], self.psf[:, b, 0:256]), r=[("pf", b)], w=[(nm, i, s)])
                            else:
                                P.act(ACTF(dst[:, s, vg * 256:(vg + 1) * 256], self.psf[:, b, 0:256], AF.Silu), r=[("pf", b)], w=[(nm, i, s)])
                            yield

            def drain(g, n):
                if g is None:
                    return
                for _ in range(n):
                    try:
                        next(g)
                    except StopIteration:
                        return

            for _ in proj(0):
                pass
            for h in range(RH):
                i = h % 2
                Qr, Kr, KT, V, GS = Qr2[i], Kr2[i], KT2[i], V2[i], GS2[i]
                nxt = proj(h + 1) if h + 1 < RH else None
                P.dma("sync", gnb[:], self.retgn_d[h, :].partition_broadcast(128), w=["gnb"])
                if first_tile:
                    P.vec(MS(Sf[:], 0.0), w=["Sf0", "Sf1"])
                else:
                    P.dma("sync", Sf[:], self.o_pret[h].rearrange("a p e -> p a e"), w=["Sf0", "Sf1"], r=[("scr", h)])
                P.act(ACP(Sb[:], Sf[:]), r=["Sf0", "Sf1"], w=["Sb"])
                qo, _ = COFF["qs"]
                io, _ = COFF["intraT"]
                for s in range(NSUB):
                    sl = slice(s * 128, (s + 1) * 128)
                    b = 3
                    for dkc in range(2):
                        P.pe(MM(self.psf[:, b, 0:128], Kr[:, dkc, sl], Qr[:, dkc, sl], dkc == 0, dkc == 1),
                             r=[("Kr", i, dkc), ("Qr", i, dkc)], w=[("pf", b)])
                    bc = 4
                    for dkc in range(2):
                        P.pe(MM(self.psf[:, bc, :], Qr[:, dkc, sl], Sb[:, dkc, :], dkc == 0, dkc == 1),
                             r=[("Qr", i, dkc), "Sb"], w=[("pf", bc)])
                    P.vec(TT(ATT[:], self.psf[:, b, 0:128], self.cst[:, io + h * 128:io + (h + 1) * 128], ALU.mult),
                          r=[("pf", b), "cst"], w=["ATT"])
                    P.vec(TS(CR[:], self.psf[:, bc, :], self.cst[:, qo + h:qo + h + 1], ALU.mult), r=[("pf", bc), "cst"], w=["CR"])
                    for dkc in range(2):
                        bs = 3 + dkc if False else (5 if dkc == 0 else 3)
                        P.pe(MM(self.psf[:, bs, :], KT[:, s, dkc * 128:(dkc + 1) * 128], V[:, s, :]), r=[("KT", i, s), ("V", i, s)], w=[("pf", bs)])
                        if dkc == 0:
                            drain(nxt, 1)
                    bi = 4
                    P.pe(MM(self.psf[:, bi, :], ATT[:], V[:, s, :]), r=["ATT", ("V", i, s)], w=[("pf", bi)])
                    drain(nxt, 1)
                    for dkc in range(2):
                        bs = 5 if dkc == 0 else 3
                        P.vec(STT(Sf[:, dkc, :], Sf[:, dkc, :], float(CC["cd"][h]), self.psf[:, bs, :], ALU.mult, ALU.add),
                              r=[("pf", bs), "Sf%d" % dkc], w=["Sf%d" % dkc])
                    P.act(ACP(Sb[:], Sf[:]), r=["Sf0", "Sf1"], w=["Sb"])
                    P.vec(TT(O[:], self.psf[:, bi, :], CR[:], ALU.add), r=[("pf", bi), "CR"], w=["O"])
                    drain(nxt, 1)
                    P.act(ACTF(self.junk[:, 0:512], O[:], AF.Square, accum_out=self.stat[:, 8:9]), r=["O"], w=["junk", "stat8"])
                    P.act(ACTF(self.stat[:, 9:10], self.stat[:, 8:9], AF.Ln, scale=1.0 / DV, bias=self.epsc[:, 0:1]), r=["stat8", "epsc"], w=["stat9"])
                    P.act(ACTF(self.stat[:, 10:11], self.stat[:, 9:10], AF.Exp, scale=-0.5), r=["stat9"], w=["stat10"])
                    P.vec(STT(TMP[:], O[:], self.stat[:, 10:11], gnb[:], ALU.mult, ALU.mult), r=["O", "stat10", "gnb"], w=["TMP"])
                    P.vec(TT(GA[:], TMP[:], GS[:, s, :], ALU.mult), r=["TMP", ("GS", i, s)], w=["GA"])
                    drain(nxt, 1)
                    for ec in range(4):
                        P.pe(TR(self.psb[:, 0, ec * 128:(ec + 1) * 128], GA[:, ec * 128:(ec + 1) * 128], self.identb[:]),
                             r=["GA", "identb"], w=[("pb", 0)])
                    for ec in range(4):
                        P.act(ACP(GAT[:, ec, sl], self.psb[:, 0, ec * 128:(ec + 1) * 128]), r=[("pb", 0)], w=[("GAT", ec)])
                    drain(nxt, 2)
                P.dma("gpsimd", self.o_pret[h].rearrange("a p e -> p a e"), Sf[:], r=["Sf0", "Sf1"], w=[("scr", h)])
                drain(nxt, 100)
                wo0, k0 = self.wload(self.woc_d[h, 0], 2, D)
                wo1, k1 = self.wload(self.woc_d[h, 1], 2, D)
                self.tm_accum(lambda kc, s: GAT[:, kc, s * 128:(s + 1) * 128], 4,
                              lambda kc, cb: (wo0 if kc < 2 else wo1)[:, kc % 2, cb * 512:(cb + 1) * 512],
                              [k0, k1], [("GAT", e) for e in range(4)], banks=(0, 6))
            P.barrier()

    def bc(self, name, c0, n, ntok):
        o, _ = VOFF[name]
        return self.vecs[:, o + c0:o + c0 + n].rearrange("p (a b) -> p a b", b=1).to_broadcast([128, n, ntok])

    def rwkv_alloc(self, st, chunked=False):
        B = {}
        if chunked:
            B["KR"] = self.sb("c_KR", [128, 8, 2, 2, 64], BF16, st)
            for nm in ("BT", "KTl", "BH", "KH", "BHT", "KHT"):
                B[nm] = self.sb("c_" + nm, [128, 8, 128], BF16, st)
            B["GC"] = self.sb("c_GC", [128, 8, 2], F32, st)
        for nm in ("R", "K", "V", "A", "KK", "BB", "KM", "T1", "T2"):
            B[nm] = self.sb("w_" + nm, [128, 8, 128], F32, st)
        B["P4"] = self.sb("w_P4", [128, 2, 129], F32, st)
        B["PA"] = self.sb("w_PA", [128, 2, 128], F32, st)
        B["PB"] = self.sb("w_PB", [128, 2, 128], F32, st)
        B["XL"] = self.sb("w_XL", [128, 2, 128], F32, st)
        B["TXW"] = self.sb("w_TXW", [128, 128], BF16, st)
        B["SXG"] = self.sb("w_SXG", [128, 128], BF16, st)
        B["Vb"] = self.sb("w_Vb", [128, 8, 128], BF16, st)
        B["VT"] = self.sb("w_VT", [128, 8, 128], BF16, st)
        if not chunked:
            B["S1"] = self.sb("w_S1", [128, 512], F32, st)
            B["S2"] = self.sb("w_S2", [128, 512], F32, st)
            B["S3"] = self.sb("w_S3", [128, 512], F32, st)
            B["S4"] = self.sb("w_S4", [128, 512], F32, st)
            B["S5"] = self.sb("w_S5", [128, 512], BF16, st)
        B["YB"] = self.sb("w_YB", [128, 8, 128], BF16, st)
        B["ST"] = self.sb("w_ST", [128, 64], F32, st)
        return B

    def rwkv_subtile(self, B, s, oFM, ntok=128, prev=None, hT=None, Tstates=None, step_post=None, chunked=False):
        P = self.P
        hT = self.hT if hT is None else hT
        R, K, V, A, KK, BB, KM, T1, T2 = (B[n] for n in ("R", "K", "V", "A", "KK", "BB", "KM", "T1", "T2"))
        P4, PA, PB, XL = B["P4"], B["PA"], B["PB"], B["XL"]
        nt = ntok
        tsl = slice(s * 128, s * 128 + nt)
        for gi in range(13):
            wg, kg = self.wload(self.wa_d[gi], 16, 256)
            b = self.bank(0, 6)
            for m in range(2):
                for kc in range(16):
                    P.pe(MM(self.psf[:, b, m * 128:m * 128 + nt], wg[:, kc, m * 128:(m + 1) * 128], hT[:, kc, tsl], kc == 0, kc == 15),
                         r=[kg, ("hT", kc)], w=[("pf", b)])
            c0 = 2 * gi
            if prev is None:
                P.vec(CP(P4[:, :, 0:1], self.carry[:, c0:c0 + 2].rearrange("p (a b) -> p a b", b=1)), r=["carry"], w=["bP4"])
                P.act(ACP(P4[:, :, 1:1 + nt], self.psf[:, b, 0:256].rearrange("p (a b) -> p a b", b=128)[:, :, 0:nt]), r=[("pf", b)], w=["bP4"])
                pprev = P4[:, :, 0:nt]
                pcur = P4[:, :, 1:1 + nt]
                P.vec(CP(self.carry[:, c0:c0 + 2].rearrange("p (a b) -> p a b", b=1), P4[:, :, nt:nt + 1]), r=["bP4"], w=["carry"])
                pk = ["bP4"]
            else:
                P.act(ACP(P4[:, :, 0:nt], self.psf[:, b, 0:256].rearrange("p (a b) -> p a b", b=128)[:, :, 0:nt]), r=[("pf", b)], w=["bP4"])
                pprev = prev[:, c0:c0 + 2, 0:nt]
                pcur = P4[:, :, 0:nt]
                P.vec(CP(self.pnew[:, c0:c0 + 2, 0:nt], pcur), r=["bP4"], w=["pnew"])
                pk = ["bP4", "prev"]
            P.vec(TT(PA[:, :, 0:nt], pprev, self.bc("mu", c0, 2, nt), ALU.mult), r=pk + ["vecs"], w=["bPA"])
            P.vec(TT(PB[:, :, 0:nt], pcur, self.omm[:, c0:c0 + 2].rearrange("p (a b) -> p a b", b=1).to_broadcast([128, 2, nt]), ALU.mult),
                  r=pk + ["omm"], w=["bPB"])
            if gi < 4:
                dst, dk = R[:, c0:c0 + 2, 0:nt], "bR"
            elif gi < 8:
                dst, dk = K[:, c0 - 8:c0 - 6, 0:nt], "bK"
            elif gi < 12:
                dst, dk = V[:, c0 - 16:c0 - 14, 0:nt], "bV"
            else:
                dst, dk = XL[:, :, 0:nt], "bXL"
            P.vec(TT(dst, PA[:, :, 0:nt], PB[:, :, 0:nt], ALU.add), r=["bPA", "bPB"], w=[dk])
        TXW, SXG = B["TXW"], B["SXG"]
        P.act(ACTF(TXW[0:64, 0:nt], XL[0:64, 0, 0:nt], AF.Tanh), r=["bXL"], w=["bTXW"])
        P.vec(CP(TXW[64:128, 0:nt], XL[64:128, 0, 0:nt]), r=["bXL"], w=["bTXW"])
        P.act(ACTF(SXG[:, 0:nt], XL[:, 1, 0:nt], AF.Sigmoid), r=["bXL"], w=["bSXG"])
        WD = A
        for what in ("w", "a", "g"):
            for half in range(2):
                b = self.bank(0, 6)
                for q in range(4):
                    hp = half * 4 + q
                    cs = slice(hp * 128, (hp + 1) * 128)
                    if what == "w":
                        P.pe(MM(self.psf[:, b, q * 128:q * 128 + nt], self.w2a2[0:64, cs], TXW[0:64, 0:nt]), r=["w2a2", "bTXW"], w=[("pf", b)])
                    elif what == "a":
                        P.pe(MM(self.psf[:, b, q * 128:q * 128 + nt], self.w2a2[64:128, cs], TXW[64:128, 0:nt]), r=["w2a2", "bTXW"], w=[("pf", b)])
                    else:
                        P.pe(MM(self.psf[:, b, q * 128:q * 128 + nt], self.g2[:, cs], SXG[:, 0:nt]), r=["g2", "bSXG"], w=[("pf", b)])
                for q in range(4):
                    hp = half * 4 + q
                    src = self.psf[:, b, q * 128:q * 128 + nt]
                    if what == "w":
                        P.act(ACTF(T2[:, hp, 0:nt], src, AF.Sigmoid, bias=self.vv("w0", hp)), r=[("pf", b), "vecs"], w=["bT2"])
                    elif what == "a":
                        P.act(ACTF(A[:, hp, 0:nt], src, AF.Sigmoid, bias=self.vv("a0", hp)), r=[("pf", b), "vecs"], w=["bA"])
                    else:
                        P.act(ACP(T1[:, hp, 0:nt], src), r=[("pf", b)], w=["bT1"])
        G = T1
        P.vec(TT(KK[:, :, 0:nt], K[:, :, 0:nt], self.bc("kk", 0, 8, nt), ALU.mult), r=["bK", "vecs"], w=["bKK"])
        P.act(ACTF(BB[:, :, 0:nt], KK[:, :, 0:nt], AF.Square), r=["bKK"], w=["bBB"])
        for half in range(2):
            b = self.bank()
            hs = slice(half * 4, half * 4 + 4)
            P.pe(MM(self.psf[:, b, 0:4 * nt], self.cv("bones"), BB[:, hs, 0:nt]), r=["cst", "bBB"], w=[("pf", b)])
            P.act(ACTF(KM[:, hs, 0:nt], self.psf[:, b, 0:4 * nt].rearrange("p (a b) -> p a b", b=nt), AF.Sqrt), r=[("pf", b)], w=["bKM"])
        P.vec(TS(KM[:, :, 0:nt], KM[:, :, 0:nt], 1e-12, ALU.max), r=["bKM"], w=["bKM"])
        P.vec(lambda e: e.reciprocal(out=KM[:, :, 0:nt], in_=KM[:, :, 0:nt]), r=["bKM"], w=["bKM"])
        P.vec(TT(KK[:, :, 0:nt], KK[:, :, 0:nt], KM[:, :, 0:nt], ALU.mult), r=["bKK", "bKM"], w=["bKK"])
        P.vec(TT(BB[:, :, 0:nt], KK[:, :, 0:nt], A[:, :, 0:nt], ALU.mult), r=["bKK", "bA"], w=["bBB"])
        P.vec(STT(KM[:, :, 0:nt], A[:, :, 0:nt], -1.0, self.bc("ka", 0, 8, nt), ALU.add, ALU.mult), r=["bA", "vecs"], w=["bKM"])
        P.vec(STT(KM[:, :, 0:nt], KM[:, :, 0:nt], 1.0, K[:, :, 0:nt], ALU.add, ALU.mult), r=["bKM", "bK"], w=["bKM"])
        if chunked:
            P.vec(TS(A[:], T2[:], -math.exp(-0.5), ALU.mult), r=["bT2", "bA"], w=["bA"])
        else:
            P.act(ACTF(WD[:, :, 0:nt], T2[:, :, 0:nt], AF.Exp, scale=-math.exp(-0.5)), r=["bT2", "bA"], w=["bA"])
        BON = K
        P.vec(TT(T2[:, :, 0:nt], R[:, :, 0:nt], KM[:, :, 0:nt], ALU.mult), r=["bR", "bKM"], w=["bT2"])
        P.vec(TT(T2[:, :, 0:nt], T2[:, :, 0:nt], self.bc("rk", 0, 8, nt), ALU.mult), r=["bT2", "vecs"], w=["bT2"])
        for half in range(2):
            b = self.bank()
            hs = slice(half * 4, half * 4 + 4)
            P.pe(MM(self.psf[:, b, 0:4 * nt], self.cv("bones"), T2[:, hs, 0:nt]), r=["cst", "bT2"], w=[("pf", b)])
            P.vec(TT(BON[:, hs, 0:nt], self.psf[:, b, 0:4 * nt].rearrange("p (a b) -> p a b", b=nt), V[:, hs, 0:nt], ALU.mult),
                  r=[("pf", b), "bV", "bKM"], w=["bK"])
        Vb, VT = B["Vb"], B["VT"]
        P.act(ACP(Vb[:, :, 0:nt], V[:, :, 0:nt]), r=["bV"], w=["bVb"])
        for hp in range(8):
            pb = ("pb", 0)
            P.pe(TR(self.psb[0:nt, 0, hp * 128:(hp + 1) * 128], Vb[:, hp, 0:nt], self.identb[:]), r=["bVb", "identb"], w=[pb])
        P.vec(CP(VT[0:nt, :, :], self.psb[0:nt, 0, :].rearrange("p (a b) -> p a b", b=128)), r=[("pb", 0) for hp in range(8)], w=["bVT"])
        v3 = lambda ap: ap.rearrange("p (a b) -> p a b", b=64)
        if chunked:
            self.rwkv_chunk_core(B)
        else:
            S1, S2, S3, S4, S5 = (B[n] for n in ("S1", "S2", "S3", "S4", "S5"))
        for t in range(0 if chunked else nt):
            if Tstates is None:
                Tst, tk = self.Tst, "Tst"
            else:
                Tst, tk = Tstates(t)
            col = lambda X: X[:, :, t:t + 1].to_broadcast([128, 8, 64])
            P.vec(TT(v3(S1[:]), Tst[:], col(KK), ALU.mult), r=[tk, "bKK"], w=["bS1"])
            P.pe(MM(self.psf[:, 4, :], self.cv("bones"), S1[:]), r=["cst", "bS1"], w=[("pf", 4)])
            P.pool(TT(v3(S3[:]), Tst[:], col(WD), ALU.mult), r=[tk, "bA"], w=["bS3"])
            P.pe(MM(self.psf[0:64, 5, :], self.identb[0:nt, t:t + 1].to_broadcast([nt, 64]), VT[0:nt, :, 0:64]), r=["identb", "bVT"], w=[("pf", 5)])
            P.pe(MM(self.psf[64:128, 5, :], self.identb[0:nt, t:t + 1].to_broadcast([nt, 64]), VT[0:nt, :, 64:128]), r=["identb", "bVT"], w=[("pf", 5)])
            P.vec(TT(v3(S4[:]), v3(self.psf[:, 5, :]), col(KM), ALU.mult), r=[("pf", 5), "bKM"], w=["bS4"])
            P.vec(TT(v3(S2[:]), v3(self.psf[:, 4, :]), col(BB), ALU.mult), r=[("pf", 4), "bBB"], w=["bS2"])
            P.vec(TT(S3[:], S3[:], S2[:], ALU.subtract), r=["bS3", "bS2"], w=["bS3"])
            P.vec(TT(Tst[:], v3(S3[:]), v3(S4[:]), ALU.add), r=["bS3", "bS4"], w=[tk])
            P.pool(TT(v3(S5[:]), Tst[:], col(R), ALU.mult), r=[tk, "bR"], w=["bS5"])
            o_, _ = COFF["iota"]
            P.pe(MM(self.psf[0:nt, 2, :], self.csel[0:64, 127 - t:127 - t + nt], S5[0:64, :], t == 0, t == nt - 1), r=["csel", "bS5"], w=[("pf", 2)])
            P.pe(MM(self.psf[0:nt, 3, :], self.csel[64:128, 127 - t:127 - t + nt], S5[64:128, :], t == 0, t == nt - 1), r=["csel", "bS5"], w=[("pf", 3)])
            if step_post is not None:
                step_post(t, Tst, tk)
        YN = KK
        YB, ST = B["YB"], B["ST"]
        yv = YN[0:nt, :, :].rearrange("p a (c d) -> p a c d", d=64)
        if chunked:
            P.act(ACP(YN[:, 0:4, :], self.psf[:, 4, :].rearrange("p (a b) -> p a b", b=128)), r=[("pf", 4)], w=["bKK"])
            P.vec(CP(YN[:, 4:8, :], self.psf[:, 5, :].rearrange("p (a b) -> p a b", b=128)), r=[("pf", 5)], w=["bKK"])
        else:
            P.act(ACP(yv[:, :, 0, :], v3(self.psf[0:nt, 2, :])), r=[("pf", 2)], w=["bKK"])
            P.vec(CP(yv[:, :, 1, :], v3(self.psf[0:nt, 3, :])), r=[("pf", 3)], w=["bKK"])
        y16 = YN[0:nt, :, :].rearrange("p a (c d) -> p (a c) d", d=64)
        q16 = BB[0:nt, :, :].rearrange("p a (c d) -> p (a c) d", d=64)
        P.vec(lambda e: e.tensor_reduce(out=ST[0:nt, 0:16], in_=y16, axis=AX.X, op=ALU.add), r=["bKK"], w=["bST0"])
        P.vec(TS(ST[0:nt, 0:16], ST[0:nt, 0:16], -1.0 / 64, ALU.mult), r=["bST0"], w=["bST0"])
        P.vec(TT(y16, y16, ST[0:nt, 0:16].rearrange("p (a b) -> p a b", b=1).to_broadcast([nt, 16, 64]), ALU.add), r=["bKK", "bST0"], w=["bKK"])
        P.act(ACTF(q16, y16, AF.Square), r=["bKK"], w=["bBB"])
        P.vec(lambda e: e.tensor_reduce(out=ST[0:nt, 16:32], in_=q16, axis=AX.X, op=ALU.add), r=["bBB"], w=["bST1"])
        P.act(ACTF(ST[0:nt, 32:48], ST[0:nt, 16:32], AF.Ln, scale=1.0 / 64, bias=self.epsc[0:nt, 1:2]), r=["bST1", "epsc"], w=["bST2"])
        P.act(ACTF(ST[0:nt, 48:64], ST[0:nt, 32:48], AF.Exp, scale=-0.5), r=["bST2"], w=["bST3"])
        P.vec(TT(YB[0:nt, :, :].rearrange("p a (c d) -> p (a c) d", d=64), y16,
                 ST[0:nt, 48:64].rearrange("p (a b) -> p a b", b=1).to_broadcast([nt, 16, 64]), ALU.mult), r=["bKK", "bST3"], w=["bYB"])
        for hp in range(8):
            pb = ("pb", 1)
            P.pe(TR(self.psb[:, 1, hp * 128:hp * 128 + nt], YB[0:nt, hp, :], self.identb[0:nt, 0:nt]), r=["bYB", "identb"], w=[pb])
            P.vec(TS(T2[:, hp, 0:nt], self.psb[:, 1, hp * 128:hp * 128 + nt], self.vv("lnw", hp), ALU.mult, self.vv("lnb", hp), ALU.add),
                  r=[pb, "vecs"], w=["bT2"])
        P.vec(TT(T2[:, :, 0:nt], T2[:, :, 0:nt], BON[:, :, 0:nt], ALU.add), r=["bT2", "bK"], w=["bT2"])
        P.vec(TT(oFM[:, 0:8, tsl], T2[:, :, 0:nt], G[:, :, 0:nt], ALU.mult), r=["bT2", "bT1"], w=[("oFM", k) for k in range(8)])

    def sincos(self, S, C, X, Rt, Ft, shape_key, n, pool=False):
        P = self.P
        MAGIC = 12582912.0
        (P.pool if pool else P.vec)(TS(Rt, X, MAGIC, ALU.add), r=[shape_key + "X"], w=[shape_key + "R"])
        P.vec(STT(Ft, Rt, -MAGIC, X, ALU.add, ALU.subtract), r=[shape_key + "R", shape_key + "X"], w=[shape_key + "F"])
        P.act(ACTF(S, Ft, AF.Sin, scale=-2.0 * math.pi), r=[shape_key + "F"], w=[shape_key + "S"])
        P.vec(STT(Rt, Ft, -1.0, Ft, ALU.mult, ALU.max), r=[shape_key + "F"], w=[shape_key + "R"])
        P.act(ACTF(C, Rt, AF.Sin, scale=-2.0 * math.pi, bias=self.epsc[0:n, 2:3]), r=[shape_key + "R", "epsc"], w=[shape_key + "C"])

    def s5_setup(self):
        from contextlib import ExitStack
        P = self.P
        T = self.T
        self.LB = self.sb("s5_LB", [128, 2, 8, 128], BF16)
        self.LC = self.sb("s5_LC", [128, 2, 32, 64], BF16)
        self.LBz = self.sb("s5_LBz", [128, 2, 8, 2, 128], BF16)
        self.s5p = self.sb("s5_p", [128, 10, 32])
        self.G0 = self.sb("s5_G0", [128, 2, 32])
        self.GL = self.sb("s5_GL", [128, 2, 32])
        self.Hs5 = self.sb("s5_H", [128, 2, 32])
        self.Hs5T = self.sb("s5_HT", [32, 2, 128])
        with ExitStack() as st:
            a = self.sb("s5s_a", [128, 3, 32], F32, st)
            b = self.sb("s5s_b", [128, 2, 32, 16], F32, st)
            c = self.sb("s5s_c", [128, 2, 32 * 64], F32, st)
            t = self.sb("s5s_t", [128, 24, 32], F32, st)
            bb = self.sb("s5s_bb", [128, 2, 32, 16], F32, st)
            tb = self.sb("s5s_tb", [128, 2, 32, 16], F32, st)
            ZB = self.sb("s5s_ZB", [128, 2, 32, 2, 16], BF16, st)
            P.dma("sync", a[:], self.s5a_d[:, :, :], w=["s_a"])
            P.dma("sync", b[:].rearrange("p a g q -> p a (g q)"), self.s5b_d[:, :, :], w=["s_b"])
            P.dma("sync", c[:], self.s5c_d[:, :, :], w=["s_c"])
            P.vec(CP(self.LC[:].rearrange("p a g q -> p a (g q)"), c[:]), r=["s_c"], w=["LC"])
            lr, li = a[:, 0, :], a[:, 1, :]
            dt = t[:, 0, :]
            P.act(ACTF(dt, a[:, 2, :], AF.Exp), r=["s_a"], w=["s_dt"])
            th = self.s5p[:, 0, :]
            P.vec(STT(th, li, 1.0 / (2.0 * math.pi), dt, ALU.mult, ALU.mult), r=["s_a", "s_dt"], w=["s5p0", "thX"])
            P.vec(TT(t[:, 1, :], lr, dt, ALU.mult), r=["s_a", "s_dt"], w=["s_t1"])
            rho = self.s5p[:, 1, :]
            P.act(ACTF(rho, t[:, 1, :], AF.Exp), r=["s_t1"], w=["s5p1"])
            sn, cs = t[:, 2, :], t[:, 3, :]
            self.sincos(sn, cs, th, t[:, 4, :], t[:, 5, :], "th", 128)
            abr, abi = self.s5p[:, 8, :], self.s5p[:, 9, :]
            P.vec(TT(abr, rho, cs, ALU.mult), r=["s5p1", "thC"], w=["s_abr"])
            P.vec(TT(abi, rho, sn, ALU.mult), r=["s5p1", "thS"], w=["s_abi"])
            den = t[:, 8, :]
            P.vec(TT(den, lr, lr, ALU.mult), r=["s_a"], w=["s_den"])
            P.vec(TT(t[:, 9, :], li, li, ALU.mult), r=["s_a"], w=["s_t9"])
            P.vec(TT(den, den, t[:, 9, :], ALU.add), r=["s_den", "s_t9"], w=["s_den"])
            P.vec(lambda e: e.reciprocal(out=den, in_=den), r=["s_den"], w=["s_den"])
            am1 = t[:, 10, :]
            P.vec(TS(am1, abr, -1.0, ALU.add), r=["s_abr"], w=["s_am1"])
            fre, fim = t[:, 11, :], t[:, 12, :]
            P.vec(TT(fre, am1, lr, ALU.mult), r=["s_am1", "s_a"], w=["s_fre"])
            P.vec(TT(t[:, 13, :], abi, li, ALU.mult), r=["s_abi", "s_a"], w=["s_t13"])
            P.vec(TT(fre, fre, t[:, 13, :], ALU.add), r=["s_fre", "s_t13"], w=["s_fre"])
            P.vec(TT(fre, fre, den, ALU.mult), r=["s_fre", "s_den"], w=["s_fre"])
            P.vec(TT(fim, abi, lr, ALU.mult), r=["s_abi", "s_a"], w=["s_fim"])
            P.vec(TT(t[:, 14, :], am1, li, ALU.mult), r=["s_am1", "s_a"], w=["s_t14"])
            P.vec(TT(fim, fim, t[:, 14, :], ALU.subtract), r=["s_fim", "s_t14"], w=["s_fim"])
            P.vec(TT(fim, fim, den, ALU.mult), r=["s_fim", "s_den"], w=["s_fim"])
            fb = lambda x: x.rearrange("p (g q) -> p g q", q=1).to_broadcast([128, 32, 16])
            P.vec(TT(bb[:, 0], b[:, 0], fb(fre), ALU.mult), r=["s_b", "s_fre"], w=["s_bb0"])
            P.vec(TT(tb[:, 0], b[:, 1], fb(fim), ALU.mult), r=["s_b", "s_fim"], w=["s_tb0"])
            P.vec(TT(bb[:, 0], bb[:, 0], tb[:, 0], ALU.subtract), r=["s_bb0", "s_tb0"], w=["s_bb0"])
            P.vec(TT(bb[:, 1], b[:, 1], fb(fre), ALU.mult), r=["s_b", "s_fre"], w=["s_bb1"])
            P.vec(TT(tb[:, 1], b[:, 0], fb(fim), ALU.mult), r=["s_b", "s_fim"], w=["s_tb1"])
            P.vec(TT(bb[:, 1], bb[:, 1], tb[:, 1], ALU.add), r=["s_bb1", "s_tb1"], w=["s_bb1"])
            P.vec(MS(ZB[:], 0.0), w=["s_ZB"])
            for ri in range(2):
                P.vec(CP(ZB[0:64, ri, :, 0, :], bb[0:64, ri]), r=["s_bb%d" % ri], w=["s_ZB"])
                P.vec(CP(ZB[64:128, ri, :, 1, :], bb[64:128, ri]), r=["s_bb%d" % ri], w=["s_ZB"])
            for ri in range(2):
                for ch in range(8):
                    pb = ("pb", ri)
                    P.pe(TR(self.psb[:, ri, ch * 128:(ch + 1) * 128], ZB[:, ri, 4 * ch:4 * ch + 4, :, :].rearrange("p g a q -> p (g a q)"),
                            self.identb[:]), r=["s_ZB", "identb"], w=[pb])
                P.vec(CP(self.LB[:, ri, :, :], self.psb[:, ri, :].rearrange("p (a b) -> p a b", b=128)),
                      r=[("pb", ri) for ch in range(8)], w=["LB"])
                for ql in range(2):
                    mo, _ = COFF["m%d" % ql]
                    P.vec(TS(self.LBz[:, ri, :, ql, :], self.LB[:, ri, :, :], self.cst[:, mo:mo + 1], ALU.mult), r=["LB", "cst"], w=["LBz"])
            for k, mult, base in ((2, float(T), 15), (4, float(T - 1), 18)):
                X = t[:, base, :]
                kk_ = "ec%d" % k
                P.vec(TS(X, th, mult, ALU.mult), r=["s5p0"], w=[kk_ + "X"])
                self.sincos(self.s5p[:, k + 1, :], self.s5p[:, k, :], X, t[:, base + 1, :], t[:, base + 2, :], kk_, 128)
            P.vec(MS(self.G0[:], 0.0), w=["G0"])
            P.barrier()

    def s5_tile(self, st, oFM, last_tile, sample=None):
        P = self.P
        T = self.T if sample is None else 16
        import os
        if int(os.environ.get("S5STOP", "99")) <= 1:
            return
        f = lambda nm: self.sb("v_" + nm, [128, T], F32, st)
        Ub = self.sb("v_Ub", [128, 8, T], BF16, st)
        DU = self.sb("v_DU", [128, 8, T], F32, st)
        YG = self.sb("v_YG", [128, 8, T], BF16, st)
        Y, Y2 = f("Y"), f("Y2")
        TNAMES = ("X", "R", "F", "SIN", "COS", "A1", "A2", "WR", "WI", "GR", "GI", "A3", "A4", "A5", "A6")
        TSET = [{n: f(n + str(i)) for n in TNAMES} for i in range(2)]
        HRS = [self.sb("v_HR%d" % i, [128, T], BF16, st) for i in range(2)]
        HIS = [self.sb("v_HI%d" % i, [128, T], BF16, st) for i in range(2)]
        for gi in range(13, 17):
            wg, kg = self.wload(self.wa_d[gi], 16, 256)
            for m in range(2):
                ch = 2 * (gi - 13) + m
                b = self.bank()
                for kc in range(16):
                    P.pe(MM(self.psf[:, b, 0:T], wg[:, kc, m * 128:(m + 1) * 128], self.hT[:, kc, 0:T], kc == 0, kc == 15),
                         r=[kg, ("hT", kc)], w=[("pf", b)])
                P.act(ACP(Ub[:, ch, :], self.psf[:, b, 0:T]), r=[("pf", b)], w=[("Ub", ch)])
                P.vec(TS(DU[:, ch, :], self.psf[:, b, 0:T], self.vv("s5d", ch), ALU.mult), r=[("pf", b), "vecs"], w=[("DU", ch)])
        io, _ = COFF["iota"]
        import os
        S5STOP = int(os.environ.get("S5STOP", "99"))
        if S5STOP <= 2:
            return
        if sample is not None:
            self.s5_sample_step(st, Ub, sample)
        for ch in range(8 if S5STOP > 5 else 1):
            by = 4 + (ch % 2)
            def gp_body(q, ch=ch, by=by):
                    if sample is not None:
                        gp = 4 * ch + q
                        hf, ql = q // 2, q % 2
                        ps = slice(64 * hf, 64 * hf + 64)
                        P.pe(MM(self.psf[ps, by, 0:T], self.LC[:, 0, gp, :], self.sHR[:, gp, :], ql == 0, False), r=["LC", "sHR"], w=[("pf", by)])
                        yield
                        P.pe(MM(self.psf[ps, by, 0:T], self.LC[:, 1, gp, :], self.sHI[:, gp, :], False, ql == 1), r=["LC", "sHI"], w=[("pf", by)])
                        yield
                        return
                    gp = 4 * ch + q
                    hf, ql = q // 2, q % 2
                    ps = slice(64 * hf, 64 * hf + 64)
                    pz = gp % 2
                    X, Rt, Ft, SIN, COS, A1, A2, WR, WI, GR, GI, A3, A4, A5, A6 = (TSET[pz][n] for n in TNAMES)
                    HR, HI = HRS[pz], HIS[pz]
                    K_ = lambda nm: nm + str(pz)
                    br, bi = self.bank(), self.bank()
                    P.pe(MM(self.psf[:, br, 0:T], self.LBz[ps, 0, ch, ql, :], Ub[ps, ch, :]), r=["LBz", ("Ub", ch)], w=[("pf", br)])
                    yield
                    P.pe(MM(self.psf[:, bi, 0:T], self.LBz[ps, 1, ch, ql, :], Ub[ps, ch, :]), r=["LBz", ("Ub", ch)], w=[("pf", bi)])
                    yield
                    P.vec(TS(X[:], self.cst[:, io:io + T], self.s5p[:, 0, gp:gp + 1], ALU.mult), r=["cst", "s5p0"], w=[K_("tb") + "X"])
                    yield
                    self.sincos(SIN[:], COS[:], X[:], Rt[:], Ft[:], K_("tb"), 128)
                    yield
                    Br, Bi = self.psf[:, br, 0:T], self.psf[:, bi, 0:T]
                    P.vec(TT(A1[:], Br, COS[:], ALU.mult), r=[("pf", br), K_("tb") + "C"], w=[K_("A1")])
                    yield
                    P.vec(TT(A2[:], Bi, SIN[:], ALU.mult), r=[("pf", bi), K_("tb") + "S"], w=[K_("A2")])
                    yield
                    P.vec(TT(WR[:], A1[:], A2[:], ALU.add), r=[K_("A1"), K_("A2")], w=[K_("WR")])
                    yield
                    P.vec(TT(A1[:], Bi, COS[:], ALU.mult), r=[("pf", bi), K_("tb") + "C"], w=[K_("A1")])
                    yield
                    P.vec(TT(A2[:], Br, SIN[:], ALU.mult), r=[("pf", br), K_("tb") + "S"], w=[K_("A2")])
                    yield
                    P.vec(TT(WI[:], A1[:], A2[:], ALU.subtract), r=[K_("A1"), K_("A2")], w=[K_("WI")])
                    yield
                    rho_bc = self.s5p[:, 1, gp:gp + 1].to_broadcast([128, T])
                    P.vec(lambda e, GR=GR, WR=WR, rho_bc=rho_bc, gp=gp: e.tensor_tensor_scan(out=GR[:], data0=rho_bc, data1=WR[:],
                          initial=self.G0[:, 0, gp:gp + 1], op0=ALU.mult, op1=ALU.add), r=[K_("WR"), "s5p1", "G0"], w=[K_("GR")])
                    P.vec(lambda e, GI=GI, WI=WI, rho_bc=rho_bc, gp=gp: e.tensor_tensor_scan(out=GI[:], data0=rho_bc, data1=WI[:],
                          initial=self.G0[:, 1, gp:gp + 1], op0=ALU.mult, op1=ALU.add), r=[K_("WI"), "s5p1", "G0"], w=[K_("GI")])
                    P.act(ACP(self.GL[:, 0, gp:gp + 1], GR[:, T - 1:T]), r=[K_("GR")], w=["GL"])
                    yield
                    P.act(ACP(self.GL[:, 1, gp:gp + 1], GI[:, T - 1:T]), r=[K_("GI")], w=["GL"])
                    yield
                    P.vec(TT(A3[:], GR[:], COS[:], ALU.mult), r=[K_("GR"), K_("tb") + "C"], w=[K_("A3")])
                    yield
                    P.vec(TT(A4[:], GI[:], SIN[:], ALU.mult), r=[K_("GI"), K_("tb") + "S"], w=[K_("A4")])
                    yield
                    P.vec(TT(A5[:], GR[:], SIN[:], ALU.mult), r=[K_("GR"), K_("tb") + "S"], w=[K_("A5")])
                    yield
                    P.vec(TT(A6[:], GI[:], COS[:], ALU.mult), r=[K_("GI"), K_("tb") + "C"], w=[K_("A6")])
                    yield
                    P.vec(TT(HR[:], A3[:], A4[:], ALU.subtract), r=[K_("A3"), K_("A4")], w=[K_("HR")])
                    yield
                    P.vec(STT(HI[:], A5[:], -1.0, A6[:], ALU.mult, ALU.subtract), r=[K_("A5"), K_("A6")], w=[K_("HI")])
                    yield
                    P.pe(MM(self.psf[ps, by, 0:T], self.LC[:, 0, gp, :], HR[:], ql == 0, False), r=["LC", K_("HR")], w=[("pf", by)])
                    yield
                    P.pe(MM(self.psf[ps, by, 0:T], self.LC[:, 1, gp, :], HI[:], False, ql == 1), r=["LC", K_("HI")], w=[("pf", by)])
                    yield
            if sample is not None:
                for q in range(4):
                    for _ in gp_body(q):
                        pass
            else:
                for pair in ((0, 1), (2, 3)):
                    alive = [gp_body(q) for q in pair]
                    while alive:
                        for g_ in list(alive):
                            try:
                                next(g_)
                            except StopIteration:
                                alive.remove(g_)
            P.vec(TT(Y[:], self.psf[:, by, 0:T], DU[:, ch, :], ALU.add), r=[("pf", by), ("DU", ch)], w=["Y"])
            P.act(ACTF(Y2[:], Y[:], AF.Square), r=["Y"], w=["Y2"])
            P.vec(STT(Y2[:], Y2[:], 0.044715, Y[:], ALU.mult, ALU.mult), r=["Y2", "Y"], w=["Y2"])
            P.vec(TT(Y2[:], Y2[:], Y[:], ALU.add), r=["Y2", "Y"], w=["Y2"])
            P.act(ACTF(Y2[:], Y2[:], AF.Sigmoid, scale=1.5957691216057308), r=["Y2"], w=["Y2"])
            P.vec(TT(YG[:, ch, :], Y[:], Y2[:], ALU.mult), r=["Y", "Y2"], w=[("YG", ch)])
        if S5STOP <= 6:
            return
        for gw in range(2):
            wgl, kgl = self.wload(self.wglu_d[gw], 8, 512)
            for m4 in range(4):
                m = 4 * gw + m4
                b = self.bank()
                for kc in range(8):
                    P.pe(MM(self.psf[:, b, 0:T], wgl[:, kc, m4 * 128:(m4 + 1) * 128], YG[:, kc, :], kc == 0, kc == 7),
                         r=[kgl, ("YG", kc)], w=[("pf", b)])
                P.act(ACTF(Y[:], self.psf[:, b, 0:T], AF.Sigmoid, bias=self.vv("bglu", m)), r=[("pf", b), "vecs"], w=["Y"])
                P.vec(TT(oFM[:, 8 + m, 0:T], YG[:, m, :], Y[:], ALU.mult), r=[("YG", m), "Y"], w=[("oFM", 8 + m)])
        if sample is None:
            self.s5_rot_state(2, self.G0, "G0")
            if last_tile:
                self.s5_state_out(self.o_ps5)

    def s5_rot_state(self, k, dst, dkey):
        P = self.P
        c, s = self.s5p[:, k, :], self.s5p[:, k + 1, :]
        t0, t1 = self.s5p[:, 6, :], self.s5p[:, 7, :]
        glr, gli = self.GL[:, 0, :], self.GL[:, 1, :]
        P.vec(TT(t0, c, glr, ALU.mult), r=["GL"], w=["s5t0"])
        P.vec(TT(t1, s, gli, ALU.mult), r=["GL"], w=["s5t1"])
        P.vec(TT(dst[:, 0, :], t0, t1, ALU.subtract), r=["s5t0", "s5t1"], w=[dkey])
        P.vec(TT(t0, s, glr, ALU.mult), r=["GL", dkey], w=["s5t0"])
        P.vec(TT(t1, c, gli, ALU.mult), r=["GL", dkey], w=["s5t1"])
        P.vec(TT(dst[:, 1, :], t0, t1, ALU.add), r=["s5t0", "s5t1"], w=[dkey])

    def s5_state_out(self, out_ap):
        P = self.P
        self.s5_rot_state(4, self.Hs5, "Hs5")
        for ri in range(2):
            b = self.bank()
            P.pe(TR(self.psf[0:32, b, 0:128], self.Hs5[:, ri, :], self.cv("ident")), r=["Hs5", "cst"], w=[("pf", b)])
            P.act(ACP(self.Hs5T[0:32, ri, :], self.psf[0:32, b, 0:128]), r=[("pf", b)], w=["Hs5T"])
            P.dma("gpsimd", out_ap[ri], self.Hs5T[0:32, ri, :], r=["Hs5T"], w=[("ps5o", ri)])

    def build(self):
        from contextlib import ExitStack
        P = self.P
        nc = self.nc
        T, NSUB = self.T, self.NSUB
        self.declare()
        self.alloc()
        self.carry = self.sb("carry", [128, 26])
        self.Tst = self.sb("Tst", [128, 8, 64])
        self.Tb = self.sb("Tb", [128, 8, 64], BF16)
        P.vec(MS(self.Tb[:], 0.0), w=["Tb"])
        self.csel = self.sb("csel", [128, 255], BF16)
        self.epsc = self.sb("epsc", [128, 4])
        self.ffn_act = self.sb("ffn_act", [128, 2, T], BF16)
        self.ffn_sil = self.sb("ffn_sil", [128, T])
        self.setup()
        P.vec(MS(self.carry[:], 0.0), w=["carry"])
        P.vec(MS(self.Tst[:], 0.0), w=["Tst"])
        P.vec(MS(self.csel[:], 0.0), w=["csel"])
        P.vec(MS(self.csel[:, 127:128], 1.0), w=["csel"])
        P.vec(MS(self.epsc[:, 0:1], EPS), w=["epsc"])
        P.vec(MS(self.epsc[:, 1:2], GN_EPS), w=["epsc"])
        P.vec(MS(self.epsc[:, 2:3], math.pi / 2.0), w=["epsc"])
        P.vec(MS(self.epsc[:, 3:4], 1.0), w=["epsc"])
        self.cself = self.sb("cself", [128, 31])
        P.vec(MS(self.cself[:], 0.0), w=["cself"])
        P.vec(MS(self.cself[:, 15:16], 1.0), w=["cself"])
        if self.on("s5"):
            self.s5_setup()
        P.barrier()
        for ti in range(self.NTILE):
            first, last = ti == 0, ti == self.NTILE - 1
            for s in range(NSUB):
                r0 = (ti * NSUB + s) * 128
                P.dma("sync", self.x[:, s, :], self.xp[r0:r0 + 128, :], w=[("x", s)])
            P.dma("sync", self.rot[:], self.rot_d[:, :, ti * T:(ti + 1) * T], w=["rot"])
            if self.on("rwkv") or self.on("s5"):
                self.norm_to_hT("nm0")
                P.barrier()
                with ExitStack() as st:
                    oFM = self.sb("oFM", [128, 16, T], BF16, st)
                    if not (self.on("rwkv") and self.on("s5")):
                        P.vec(MS(oFM[:], 0.0), w=[("oFM", k) for k in range(16)])
                    if self.on("rwkv"):
                        with ExitStack() as st2:
                            B = self.rwkv_alloc(st2, chunked=CHUNKED)
                            for s in range(NSUB):
                                self.rwkv_subtile(B, s, oFM, chunked=CHUNKED)
                            if last:
                                self.rwkv_state_out(B)
                            P.barrier()
                    if self.on("s5"):
                        with ExitStack() as st2:
                            self.s5_tile(st2, oFM, last)
                            P.barrier()
                    self.outproj_a(oFM)
                    P.barrier()
            if self.on("norm"):
                self.norm_to_hT("nf0")
                self.dump("hT", self.hT[:].rearrange("p a b -> p (a b)"), [128, 16 * T], [("hT", k) for k in range(16)], BF16)
            if self.on("ffn0"):
                self.norm_to_hT("nf0")
                self.ffn(0)
            if self.on("ret"):
                self.norm_to_hT("nm1")
                self.layer1(first, last)
            if self.on("ffn1"):
                self.norm_to_hT("nf1")
                self.ffn(1)
            if self.on("final"):
                self.final_norm(lambda s: self.yp[(ti * NSUB + s) * 128:(ti * NSUB + s + 1) * 128, :])
            else:
                for s in range(NSUB):
                    r0 = (ti * NSUB + s) * 128
                    P.dma("gpsimd", self.yp[r0:r0 + 128, :], self.x[:, s, :], r=[("x", s)], w=["yout"])
            P.barrier()
        if self.with_sample:
            self.sample_phase()
        P.barrier()
        with nc.Block() as block:
            @block.tensor
            def _(e):
                P.replay("tensor", e)

            @block.vector
            def _(e):
                P.replay("vector", e)

            @block.scalar
            def _(e):
                P.replay("scalar", e)

            @block.gpsimd
            def _(e):
                P.replay("gpsimd", e)

            @block.sync
            def _(e):
                P.replay("sync", e)
        return nc

    def sample_phase(self):
        from contextlib import ExitStack
        P = self.P
        smp = self.smp
        P.barrier()
        P.dma("sync", self.x[0:16, 0, :], self.xs_d[:, :], w=[("x", 0)])
        if self.on("rwkv") or self.on("s5"):
            self.norm_to_hT("nm0", ntok=16, nsub=1)
            P.barrier()
            with ExitStack() as st:
                oFM = self.sb("oFMs", [128, 16, 16], BF16, st)
                if not (self.on("rwkv") and self.on("s5")):
                    P.vec(MS(oFM[:], 0.0), w=[("oFM", k) for k in range(16)])
                if self.on("rwkv"):
                    with ExitStack() as st2:
                        self.rwkv_sample(st2, oFM, smp)
                        P.barrier()
                if self.on("s5"):
                    with ExitStack() as st2:
                        self.s5_tile(st2, oFM, False, sample=smp)
                        P.barrier()
                self.outproj_a(oFM, ntok=16, nsub=1)
                P.barrier()
        if self.on("ffn0"):
            self.norm_to_hT("nf0", ntok=16, nsub=1)
            self.ffn(0, ntok=16, nsub=1)
        if self.on("ret"):
            self.norm_to_hT("nm1", ntok=16, nsub=1)
            self.layer1_sample(smp)
        if self.on("ffn1"):
            self.norm_to_hT("nf1", ntok=16, nsub=1)
            self.ffn(1, ntok=16, nsub=1)
        if self.on("final"):
            self.final_norm(lambda s: self.ys[:, :], ntok=16, nsub=1)
        else:
            P.dma("gpsimd", self.ys[:, :], self.x[0:16, 0, :], r=[("x", 0)], w=["ysout"])
        P.barrier()

    def rwkv_state_out(self, B):
        P = self.P
        S1 = B["R"][:].rearrange("p a b -> p (a b)")
        for hp in range(8):
            b = self.bank()
            P.pe(TR(self.psf[0:64, b, 0:128], self.Tst[:, hp, :], self.cv("ident")), r=["Tst", "cst"], w=[("pf", b)])
            P.act(ACP(S1[0:64, 0:128], self.psf[0:64, b, 0:128]), r=[("pf", b)], w=["bS1"])
            P.dma("gpsimd", self.o_prwkv[2 * hp:2 * hp + 2].rearrange("h i j -> i h j"),
                  S1[0:64, 0:128].rearrange("p (h j) -> p h j", j=64), r=["bS1"], w=[("prwkv", hp)])
        b = self.bank()
        P.pe(TR(self.psf[0:26, b, 0:128], self.carry[:, 0:26], self.cv("ident")), r=["carry", "cst"], w=[("pf", b)])
        P.act(ACP(S1[0:26, 128:256], self.psf[0:26, b, 0:128]), r=[("pf", b)], w=["bS1"])
        P.dma("gpsimd", self.o_pshift[:, :], S1[0:26, 128:256], r=["bS1"], w=["pshift"])


def shared_maps(inp, seq):
    m = {}
    cc = make_consts()
    m["consts"] = np.ascontiguousarray(np.concatenate([cc[n].reshape(128, -1) for n, _ in CONST_LAYOUT], axis=1).astype(np.float32))
    vec = {"nm0": inp["norm_mix"][0], "nf0": inp["norm_ffn"][0], "nm1": inp["norm_mix"][1], "nf1": inp["norm_ffn"][1],
           "mu": inp["mu_shift"][0], "w0": inp["rwkv_w0"][0], "a0": inp["rwkv_a0"][0], "kk": inp["rwkv_k_k"][0],
           "ka": inp["rwkv_k_a"][0], "rk": inp["rwkv_r_k"][0], "lnw": inp["rwkv_ln_w"][0], "lnb": inp["rwkv_ln_b"][0],
           "s5d": inp["s5_d"][0], "bglu": inp["s5_b_glu"][0]}
    m["vecs"] = np.ascontiguousarray(np.concatenate([fm(vec[n]) for n, _ in VEC_LAYOUT], axis=1))
    m["rot"] = rot_tables(np.concatenate([np.arange(seq, dtype=np.float32), np.array([PAST], np.float32)]))
    m["nfin"] = np.ascontiguousarray(inp["norm_final"].reshape(1, D))
    m["retgn"] = np.ascontiguousarray(inp["ret_gn"][0].reshape(RH, DV))
    m["w2a2"] = np.ascontiguousarray(np.concatenate([inp["rwkv_w2"][0], inp["rwkv_a2"][0]], axis=0))
    m["g2"] = np.ascontiguousarray(inp["rwkv_g2"][0])
    lay = lambda a: a.reshape(32, 2, 64).transpose(1, 2, 0).reshape(128, 32)
    ld = np.repeat(inp["s5_log_dt"][0].reshape(32, 2).T[:, None, :], 64, axis=1).reshape(128, 32)
    m["s5a"] = np.ascontiguousarray(np.stack([lay(inp["s5_a_re"][0]), lay(inp["s5_a_im"][0]), ld], axis=1))
    layb = lambda b: b.reshape(32, 2, 64, 16).transpose(1, 2, 0, 3).reshape(128, 32 * 16)
    m["s5b"] = np.ascontiguousarray(np.stack([layb(inp["s5_b_re"][0]), layb(inp["s5_b_im"][0])], axis=1))

    def layc(c):
        c4 = c.reshape(32, 2, 16, 64)
        Z = np.zeros((2, 64, 32, 2, 2, 16), np.float32)
        for gl in range(2):
            for gp in range(32):
                Z[gl, :, gp, gp % 2, gl, :] = c4[gp, gl].T
        return Z.reshape(128, 32 * 64)
    m["s5c"] = np.ascontiguousarray(np.stack([layc(inp["s5_c_re"][0]), layc(inp["s5_c_im"][0])], axis=1))
    m["wa"] = tile_w(inp["w_in_a"][0], 256)
    m["wglu"] = tile_w(inp["s5_w_glu"][0], 512)
    m["woa"] = tile_w(inp["w_out_a"][0], 256)
    for i in range(2):
        m["wg%d" % i] = tile_w(inp["ffn_w_gate"][i], 256)
        m["wu%d" % i] = tile_w(inp["ffn_w_up"][i], 256)
        m["wd%d" % i] = tile_rows(inp["ffn_w_down"][i], 2)
    wc = inp["w_in_c"][0]
    hs = []
    for h in range(RH):
        cols = [wc[:, h * 256:(h + 1) * 256], wc[:, D + h * 256:D + (h + 1) * 256]]
        for base in (2 * D, 2 * D + RH * DV):
            for vg in range(2):
                cols.append(wc[:, base + h * 512 + vg * 256:base + h * 512 + (vg + 1) * 256])
        hs.append(np.stack([tile_w(np.ascontiguousarray(c), 256)[0] for c in cols], axis=0))
    m["wc"] = np.ascontiguousarray(np.stack(hs, axis=0))
    m["woc"] = tile_rows(inp["w_out_c"][0], 2).reshape(RH, 2, 128, 2 * D)
    return m


def run(inp, seq, nsub, cores, dbg=(), stages=None, trace=False, with_sample=True):
    bld = Builder(seq, nsub, dbg=dbg, stages=stages, with_sample=with_sample)
    nc = bld.build()
    sh = shared_maps(inp, seq)
    in_maps = []
    for c in cores:
        m = dict(sh)
        m["xp"] = np.ascontiguousarray(inp["x_prompt"][c % 4, :seq])
        rs = slice(16 * c, 16 * c + 16)
        m["xs"] = np.ascontiguousarray(inp["x_sample"][rs, 0, :])
        m["st_rwkv"] = np.ascontiguousarray(inp["state_rwkv"][0, rs])
        m["st_shift"] = np.ascontiguousarray(inp["state_shift"][0, rs])
        m["st_s5"] = np.ascontiguousarray(np.stack([inp["state_s5_re"][0, rs].reshape(16, 4096), inp["state_s5_im"][0, rs].reshape(16, 4096)]))
        m["st_ret"] = np.ascontiguousarray(inp["state_ret"][0, rs])
        in_maps.append({k: v for k, v in m.items() if k in bld.dram_in})
    res = run_bass_kernel_spmd(nc, in_maps, core_ids=list(range(len(cores))), trace=trace)
    return res, bld


def kernel(**inp):
    inp = {k: np.asarray(v) for k, v in inp.items()}
    seq = inp["x_prompt"].shape[1]
    res, bld = run(inp, seq, 2, list(range(8)))
    R = res.results
    B = 4
    y_prompt = np.stack([R[b]["yp"] for b in range(B)])
    p_rwkv = np.stack([R[b]["p_rwkv"] for b in range(B)])[None]
    p_shift = np.stack([R[b]["p_shift"].reshape(PROJ) for b in range(B)])[None]
    p_s5_re = np.stack([R[b]["p_s5"][0].reshape(64, 64) for b in range(B)])[None]
    p_s5_im = np.stack([R[b]["p_s5"][1].reshape(64, 64) for b in range(B)])[None]
    p_ret = np.stack([R[b]["p_ret"].reshape(RH, DK, DV) for b in range(B)])[None]
    cat = lambda k: np.concatenate([R[c][k] for c in range(8)], axis=0)
    y_sample = cat("ys")[:, None, :]
    s_rwkv = cat("s_rwkv")[None]
    s_shift = cat("s_shift")[None]
    s_s5_re = np.concatenate([R[c]["s_s5"][0] for c in range(8)], axis=0).reshape(1, 128, 64, 64)
    s_s5_im = np.concatenate([R[c]["s_s5"][1] for c in range(8)], axis=0).reshape(1, 128, 64, 64)
    s_ret = cat("s_ret")[None]
    return (y_prompt, y_sample, p_rwkv, p_shift, p_s5_re, p_s5_im, p_ret,
            s_rwkv, s_shift, s_s5_re, s_s5_im, s_ret)


def simulate_sync(P):
    val = {}
    pc = {e: 0 for e in ENGS}
    progress = True
    while progress:
        progress = False
        for e in ENGS:
            while pc[e] < len(P.ops[e]):
                waits, fn, inc = P.ops[e][pc[e]]
                if any(val.get(id(sem), 0) < v for sem, v in waits):
                    break
                if inc is not None:
                    val[id(inc[0])] = val.get(id(inc[0]), 0) + inc[1]
                pc[e] += 1
                progress = True
    stuck = {e: (pc[e], len(P.ops[e])) for e in ENGS if pc[e] < len(P.ops[e])}
    return stuck


def _s5_sample_step(self, st, Ub, smp):
    P = self.P
    H0 = self.sb("q_H0", [128, 2, 32, 16], F32, st)
    HN = self.sb("q_HN", [128, 2, 32, 16], F32, st)
    TA = self.sb("q_TA", [128, 32, 16], F32, st)
    TB = self.sb("q_TB", [128, 32, 16], F32, st)
    self.sHR = self.sb("q_sHR", [128, 32, 16], BF16, st)
    self.sHI = self.sb("q_sHI", [128, 32, 16], BF16, st)
    stg = self.sb("q_stg", [16, 2, 1024], F32, st)
    idf = self.cv("ident")
    n = 0
    for ri in range(2):
        for k in range(4):
            buf = stg[:, n % 2, :]
            sk = ("stg", n % 2)
            n += 1
            P.dma("sync", buf, smp["st_s5"][ri, :, k * 1024:(k + 1) * 1024], w=[sk])
            b = self.bank()
            for g8 in range(8):
                P.pe(TR(self.psf[:, b, g8 * 16:(g8 + 1) * 16], buf[:, g8 * 128:(g8 + 1) * 128], idf[0:16, 0:16]), r=[sk, "cst"], w=[("pf", b)])
            P.vec(CP(H0[:, ri, 8 * k:8 * k + 8, :], self.psf[:, b, 0:128].rearrange("p (a b) -> p a b", b=16)), r=[("pf", b)], w=[("H0", ri)])
    for gp in range(32):
        ch, q = gp // 4, gp % 4
        hf, ql = q // 2, q % 2
        ps = slice(64 * hf, 64 * hf + 64)
        for ri in range(2):
            P.pe(MM(self.psf[:, ri, gp * 16:(gp + 1) * 16], self.LBz[ps, ri, ch, ql, :], Ub[ps, ch, 0:16]), r=["LBz", ("Ub", ch)], w=[("pf", ri)])
    abc = lambda k: self.s5p[:, k, :].rearrange("p (g q) -> p g q", q=1).to_broadcast([128, 32, 16])
    pv = lambda ri: self.psf[:, ri, :].rearrange("p (a b) -> p a b", b=16)
    P.vec(TT(TA[:], H0[:, 0], abc(8), ALU.mult), r=[("H0", 0), "s5p"], w=["qTA"])
    P.vec(TT(TB[:], H0[:, 1], abc(9), ALU.mult), r=[("H0", 1), "s5p"], w=["qTB"])
    P.vec(TT(HN[:, 0], TA[:], TB[:], ALU.subtract), r=["qTA", "qTB"], w=[("HN", 0)])
    P.vec(TT(HN[:, 0], HN[:, 0], pv(0), ALU.add), r=[("HN", 0), ("pf", 0)], w=[("HN", 0)])
    P.vec(TT(TA[:], H0[:, 1], abc(8), ALU.mult), r=[("H0", 1), "s5p"], w=["qTA"])
    P.vec(TT(TB[:], H0[:, 0], abc(9), ALU.mult), r=[("H0", 0), "s5p"], w=["qTB"])
    P.vec(TT(HN[:, 1], TA[:], TB[:], ALU.add), r=["qTA", "qTB"], w=[("HN", 1)])
    P.vec(TT(HN[:, 1], HN[:, 1], pv(1), ALU.add), r=[("HN", 1), ("pf", 1)], w=[("HN", 1)])
    P.vec(CP(self.sHR[:], HN[:, 0]), r=[("HN", 0)], w=["sHR"])
    P.vec(TS(self.sHI[:], HN[:, 1], -1.0, ALU.mult), r=[("HN", 1)], w=["sHI"])
    for ri in range(2):
        for k in range(4):
            buf = stg[:, n % 2, :]
            sk = ("stg", n % 2)
            n += 1
            b = self.bank(2, 4)
            b2 = self.bank(2, 4)
            for g8 in range(8):
                bb = b if g8 < 4 else b2
                P.pe(TR(self.psf[0:16, bb, (g8 % 4) * 128:(g8 % 4 + 1) * 128], HN[:, ri, 8 * k + g8, :], idf), r=[("HN", ri), "cst"], w=[("pf", bb)])
            P.vec(CP(buf[:, 0:512], self.psf[0:16, b, :]), r=[("pf", b)], w=[sk])
            P.vec(CP(buf[:, 512:1024], self.psf[0:16, b2, :]), r=[("pf", b2)], w=[sk])
            P.dma("gpsimd", smp["s_s5"][ri, :, k * 1024:(k + 1) * 1024], buf, r=[sk], w=[("s5out", ri, k)])


def _rwkv_sample(self, st, oFM, smp):
    P = self.P
    B = self.rwkv_alloc(st)
    PREV = self.sb("q_PREV", [128, 26, 16], F32, st)
    self.pnew = self.sb("q_pnew", [128, 26, 16], F32, st)
    SH = self.sb("q_SH", [16, PROJ], F32, st)
    Sin = [self.sb("q_Sin%d" % i, [64, 16, 64], F32, st) for i in range(2)]
    Sout = [self.sb("q_Sout%d" % i, [64, 16, 64], F32, st) for i in range(2)]
    Tb = [self.sb("q_Tb%d" % i, [128, 8, 64], F32, st) for i in range(2)]
    idf = self.cv("ident")
    P.dma("sync", SH[:], smp["st_shift"][:, :], w=["SH"])
    for c0 in range(0, 26, 8):
        cn = min(8, 26 - c0)
        b = self.bank()
        for c in range(cn):
            P.pe(TR(self.psf[:, b, c * 16:(c + 1) * 16], SH[:, (c0 + c) * 128:(c0 + c + 1) * 128], idf[0:16, 0:16]), r=["SH", "cst"], w=[("pf", b)])
        P.vec(CP(PREV[:, c0:c0 + cn, :], self.psf[:, b, 0:cn * 16].rearrange("p (a b) -> p a b", b=16)), r=[("pf", b)], w=["prev"])

    def Tstates(t):
        i = t % 2
        P.dma("sync", Sin[i][:], smp["st_rwkv"][t].rearrange("h i j -> i h j"), w=[("Sin", i)])
        for half in range(2):
            b = self.bank(0, 2)
            for q in range(4):
                hp = half * 4 + q
                P.pe(TR(self.psf[:, b, q * 64:(q + 1) * 64], Sin[i][:, 2 * hp:2 * hp + 2, :].rearrange("p a b -> p (a b)"), idf[0:64, 0:64]),
                     r=[("Sin", i), "cst"], w=[("pf", b)])
            P.vec(CP(Tb[i][:, half * 4:half * 4 + 4, :], self.psf[:, b, 0:256].rearrange("p (a b) -> p a b", b=64)), r=[("pf", b)], w=[("Tb", i)])
        return Tb[i], ("Tb", i)

    def step_post(t, Tst, tk):
        i = t % 2
        for half in range(2):
            b = self.bank(0, 2)
            for q in range(4):
                hp = half * 4 + q
                P.pe(TR(self.psf[0:64, b, q * 128:(q + 1) * 128], Tst[:, hp, :], idf), r=[tk, "cst"], w=[("pf", b)])
            P.act(ACP(Sout[i][:, half * 8:half * 8 + 8, :].rearrange("p a b -> p (a b)"), self.psf[0:64, b, :]), r=[("pf", b)], w=[("Sout", i)])
        P.dma("gpsimd", smp["s_rwkv"][t].rearrange("h i j -> i h j"), Sout[i][:], r=[("Sout", i)], w=[("srwkv", t)])

    self.rwkv_subtile(B, 0, oFM, ntok=16, prev=PREV, Tstates=Tstates, step_post=step_post)
    for c0 in range(0, 26, 4):
        cn = min(4, 26 - c0)
        b = self.bank()
        for c in range(cn):
            P.pe(TR(self.psf[0:16, b, c * 128:(c + 1) * 128], self.pnew[:, c0 + c, :], idf), r=["pnew", "cst"], w=[("pf", b)])
        P.vec(CP(SH[:, c0 * 128:(c0 + cn) * 128], self.psf[0:16, b, 0:cn * 128]), r=[("pf", b)], w=["SH"])
    P.dma("gpsimd", smp["s_shift"][:, :], SH[:], r=["SH"], w=["sshift"])


def _layer1_sample(self, smp):
    from contextlib import ExitStack
    P = self.P
    CC = make_consts()
    NT = 16
    P.barrier()
    with ExitStack() as st:
        Qp = self.sb("z_Qp", [128, 2, NT], F32, st)
        Kp = self.sb("z_Kp", [128, 2, NT], F32, st)
        TA = self.sb("z_TA", [128, NT], F32, st)
        TB = self.sb("z_TB", [128, NT], F32, st)
        Qf = self.sb("z_Qf", [128, 2, NT], F32, st)
        Kf = self.sb("z_Kf", [128, 2, NT], F32, st)
        Kr = self.sb("z_Kr", [128, 2, NT], BF16, st)
        QK = self.sb("z_QK", [128, 2, NT], F32, st)
        QM = self.sb("z_QM", [128, 2, NT], F32, st)
        KT = self.sb("z_KT", [NT, 256], BF16, st)
        KTm = self.sb("z_KTm", [NT, 256], BF16, st)
        V = self.sb("z_V", [NT, 512], BF16, st)
        GS = self.sb("z_GS", [NT, 512], BF16, st)
        Sf = [self.sb("z_Sf%d" % i, [128, 2, 512], F32, st) for i in range(3)]
        ATTc = self.sb("z_ATT", [NT, 1], F32, st)
        O = self.sb("z_O", [NT, 512], F32, st)
        TMP = self.sb("z_TMP", [NT, 512], F32, st)
        GA = self.sb("z_GA", [NT, 512], BF16, st)
        GAT = self.sb("z_GAT", [128, 4, NT], BF16, st)
        gnb = self.sb("z_gnb", [NT, 512], F32, st)
        rs = self.sb("z_rot", [128, 4, NT], F32, st)
        r1 = self.sb("z_rot1", [128, 4, 1], F32, st)
        P.dma("sync", r1[:], self.rot_d[:, :, self.SEQ:self.SEQ + 1], w=["r1"], allow_slow_non_contiguous=True)
        P.vec(CP(rs[:], r1[:].to_broadcast([128, 4, NT])), r=["r1"], w=["rs"])
        cos, sin, cos16, sin16 = (rs[:, i, :] for i in range(4))
        nb = 0
        for h in range(RH):
            g1 = float(CC["g1"][h])
            P.dma("sync", gnb[:], self.retgn_d[h, :].partition_broadcast(NT), w=["gnb"])
            for which, dst, (c_, s_), rdst in ((0, Qp, (cos, sin), Qf), (1, Kp, (cos16, sin16), Kf)):
                wq, kq = self.wload(self.wc_d[h, which], 16, 256)
                nm = "QK"[which]
                for dkc in range(2):
                    b = self.bank()
                    for kc in range(16):
                        P.pe(MM(self.psf[:, b, 0:NT], wq[:, kc, dkc * 128:(dkc + 1) * 128], self.hT[:, kc, 0:NT], kc == 0, kc == 15),
                             r=[kq, ("hT", kc)], w=[("pf", b)])
                    P.act(ACP(dst[:, dkc, :], self.psf[:, b, 0:NT]), r=[("pf", b)], w=[(nm + "p", dkc)])
                P.vec(TT(TA[:], dst[:, 0, :], c_, ALU.mult), r=[(nm + "p", 0), "rs"], w=["TA"])
                P.vec(TT(TB[:], dst[:, 1, :], s_, ALU.mult), r=[(nm + "p", 1), "rs"], w=["TB"])
                P.vec(TT(rdst[:, 0, :], TA[:], TB[:], ALU.subtract), r=["TA", "TB"], w=[(nm + "r", 0)])
                P.vec(TT(TA[:], dst[:, 1, :], c_, ALU.mult), r=[(nm + "p", 1), "rs"], w=["TA"])
                P.vec(TT(TB[:], dst[:, 0, :], s_, ALU.mult), r=[(nm + "p", 0), "rs"], w=["TB"])
                P.vec(TT(rdst[:, 1, :], TA[:], TB[:], ALU.add), r=["TA", "TB"], w=[(nm + "r", 1)])
            P.vec(CP(Kr[:], Kf[:]), r=[("Kr", 0), ("Kr", 1)], w=["Krb"])
            P.vec(TT(QK[:], Qf[:], Kf[:], ALU.mult), r=[("Qr", 0), ("Qr", 1), ("Kr", 0), ("Kr", 1)], w=["QKp"])
            ba = self.bank()
            for dkc in range(2):
                P.pe(MM(self.psf[0:NT, ba, 0:2], QK[:, dkc, :], self.epsc[:, 2:4], dkc == 0, dkc == 1), r=["QKp", "epsc"], w=[("pf", ba)])
            P.vec(CP(ATTc[:], self.psf[0:NT, ba, 1:2]), r=[("pf", ba)], w=["ATTc"])
            for dkc in range(2):
                P.pe(TR(self.psb[0:NT, 1, dkc * 128:(dkc + 1) * 128], Kr[:, dkc, :], self.identb[:]), r=["Krb", "identb"], w=[("pb", 1)])
            P.vec(CP(KT[:], self.psb[0:NT, 1, 0:256]), r=[("pb", 1)], w=["KT"])
            for which, dst, nm in ((2, V, "V"), (4, GS, "GS")):
                for vg in range(2):
                    wv, kv = self.wload(self.wc_d[h, which + vg], 16, 256)
                    b = self.bank()
                    for kc in range(16):
                        P.pe(MM(self.psf[0:NT, b, 0:256], self.hT[:, kc, 0:NT], wv[:, kc, :], kc == 0, kc == 15), r=[kv, ("hT", kc)], w=[("pf", b)])
                    if nm == "V":
                        P.act(ACP(dst[:, vg * 256:(vg + 1) * 256], self.psf[0:NT, b, 0:256]), r=[("pf", b)], w=[nm])
                    else:
                        P.act(ACTF(dst[:, vg * 256:(vg + 1) * 256], self.psf[0:NT, b, 0:256], AF.Silu), r=[("pf", b)], w=[nm])
            bcx = 5
            for bb in range(NT):
                S = Sf[nb % 3]
                sk = ("Sf", nb % 3)
                nb += 1
                P.dma("sync", S[:], smp["st_ret"][bb, h].rearrange("(a p) e -> p a e", p=128), w=[sk])
                P.vec(TT(QM[:], Qf[:], self.cself[:, 15 - bb:31 - bb].rearrange("p (a b) -> p a b", a=1).to_broadcast([128, 2, NT]), ALU.mult),
                       r=[("Qr", 0), ("Qr", 1), "cself"], w=["QM"])
                for dkc in range(2):
                    P.pe(MM(self.psf[0:NT, bcx, :], QM[:, dkc, :], S[:, dkc, :], bb == 0 and dkc == 0, bb == NT - 1 and dkc == 1),
                         r=["QM", sk], w=[("pf", bcx)])
                io_, _ = COFF["ident"]
                P.vec(TS(KTm[:], KT[:], self.cst[0:NT, io_ + bb:io_ + bb + 1], ALU.mult), r=["KT", "cst"], w=["KTm"])
                for dkc in range(2):
                    bs = self.bank()
                    P.pe(MM(self.psf[:, bs, :], KTm[:, dkc * 128:(dkc + 1) * 128], V[:, :]), r=["KTm", "V"], w=[("pf", bs)])
                    P.vec(STT(S[:, dkc, :], S[:, dkc, :], g1, self.psf[:, bs, :], ALU.mult, ALU.add), r=[("pf", bs), sk], w=[sk])
                P.dma("gpsimd", smp["s_ret"][bb, h].rearrange("(a p) e -> p a e", p=128), S[:], r=[sk], w=[("sret", bb, h)])
            P.vec(TS(TMP[:], self.psf[0:NT, bcx, :], g1, ALU.mult), r=[("pf", bcx)], w=["TMP"])
            P.vec(STT(O[:], V[:, :], ATTc[:, 0:1], TMP[:], ALU.mult, ALU.add), r=["V", "ATTc", "TMP"], w=["O"])
            P.act(ACTF(self.junk[0:NT, 0:512], O[:], AF.Square, accum_out=self.stat[0:NT, 8:9]), r=["O"], w=["junk", "stat8"])
            P.act(ACTF(self.stat[0:NT, 9:10], self.stat[0:NT, 8:9], AF.Ln, scale=1.0 / DV, bias=self.epsc[0:NT, 0:1]), r=["stat8", "epsc"], w=["stat9"])
            P.act(ACTF(self.stat[0:NT, 10:11], self.stat[0:NT, 9:10], AF.Exp, scale=-0.5), r=["stat9"], w=["stat10"])
            P.vec(STT(TMP[:], O[:], self.stat[0:NT, 10:11], gnb[:], ALU.mult, ALU.mult), r=["O", "stat10", "gnb"], w=["TMP"])
            P.vec(TT(GA[:], TMP[:], GS[:, :], ALU.mult), r=["TMP", "GS"], w=["GA"])
            for ec in range(4):
                P.pe(TR(self.psb[:, 0, ec * 128:ec * 128 + NT], GA[:, ec * 128:(ec + 1) * 128], self.identb[0:NT, 0:NT]), r=["GA", "identb"], w=[("pb", 0)])
            for ec in range(4):
                P.vec(CP(GAT[:, ec, :], self.psb[:, 0, ec * 128:ec * 128 + NT]), r=[("pb", 0)], w=[("GAT", ec)])
            wo0, k0 = self.wload(self.woc_d[h, 0], 2, D)
            wo1, k1 = self.wload(self.woc_d[h, 1], 2, D)
            self.tm_accum(lambda kc, s: GAT[:, kc, 0:NT], 4,
                          lambda kc, cb: (wo0 if kc < 2 else wo1)[:, kc % 2, cb * 512:(cb + 1) * 512],
                          [k0, k1], [("GAT", e) for e in range(4)], ntok=NT, nsub=1)
        P.barrier()


Builder.s5_sample_step = _s5_sample_step
Builder.rwkv_sample = _rwkv_sample
Builder.layer1_sample = _layer1_sample


def _rwkv_chunk_core(self, B):
    P = self.P
    R, K, V, A, KK, BB, KM, T1, T2 = (B[n] for n in ("R", "K", "V", "A", "KK", "BB", "KM", "T1", "T2"))
    KR, BT, KTl, BH, KH, BHT, KHT, GC = (B[n] for n in ("KR", "BT", "KTl", "BH", "KH", "BHT", "KHT", "GC"))
    VT = B["VT"]
    LW, CUM, E1 = A, T2, V
    c4 = lambda X: X[:].rearrange("p a (c t) -> p a c t", t=64)
    cv = self.cv
    for hp in range(8):
        P.vec(lambda e, hp=hp: e.tensor_tensor_scan(out=CUM[:, hp, :], data0=cv("rmask"), data1=LW[:, hp, :], initial=0.0,
                                                    op0=ALU.mult, op1=ALU.add), r=["bA", "cst"], w=["bT2"])
    P.act(ACTF(E1[:], CUM[:], AF.Exp), r=["bT2", "bVb"], w=["bV"])
    P.vec(TT(KR[:, :, :, 1, :], c4(R), c4(E1), ALU.mult), r=["bR", "bV"], w=["KR"])
    P.vec(TT(E1[:], CUM[:], LW[:], ALU.subtract), r=["bT2", "bA", "KR"], w=["bV"])
    P.act(ACTF(E1[:], E1[:], AF.Exp), r=["bV"], w=["bV"])
    P.vec(TT(KR[:, :, :, 0, :], c4(KK), c4(E1), ALU.mult), r=["bKK", "bV"], w=["KR"])
    P.act(ACTF(E1[:], CUM[:], AF.Exp, scale=-1.0), r=["bT2", "KR"], w=["bV"])
    P.vec(TT(BT[:], BB[:], E1[:], ALU.mult), r=["bBB", "bV"], w=["BT"])
    P.vec(TT(KTl[:], KM[:], E1[:], ALU.mult), r=["bKM", "bV"], w=["KTl"])
    cumc = c4(CUM)[:, :, :, 63:64]
    P.vec(TT(c4(E1), cumc.to_broadcast([128, 8, 2, 64]), c4(CUM), ALU.subtract), r=["bT2", "BT", "KTl"], w=["bV"])
    P.act(ACTF(E1[:], E1[:], AF.Exp), r=["bV"], w=["bV"])
    P.vec(TT(BH[:], BB[:], E1[:], ALU.mult), r=["bBB", "bV"], w=["BH"])
    P.vec(TT(KH[:], KM[:], E1[:], ALU.mult), r=["bKM", "bV"], w=["KH"])
    P.act(ACTF(GC[:], cumc.rearrange("p a c o -> p a (c o)"), AF.Exp), r=["bT2"], w=["GC"])
    for src, dst, nm, bk in ((BH, BHT, "BH", 1), (KH, KHT, "KH", 0)):
        for hp in range(8):
            P.pe(TR(self.psb[:, bk, hp * 128:(hp + 1) * 128], src[:, hp, :], self.identb[:]), r=[nm, "identb"], w=[("pb", bk)])
        P.vec(CP(dst[:], self.psb[:, bk, :].rearrange("p (a b) -> p a b", b=128)), r=[("pb", bk)], w=[nm + "T"])
    P.barrier()
    import os
    CHSTOP = int(os.environ.get("CHSTOP", "99"))
    if CHSTOP <= 1:
        return
    bv = lambda X: X[:].bitcast(BF16).rearrange("p a (h t) -> p (a h) t", t=64)
    XA, XB = bv(R)[:, 0:16, :], bv(R)[:, 16:32, :]
    XtA, XtB = bv(KK)[:, 0:16, :], bv(KK)[:, 16:32, :]
    PA, PB = bv(BB)[:, 0:16, :], bv(BB)[:, 16:32, :]
    AkT, RbT = bv(KM)[:, 0:16, :], bv(KM)[:, 16:32, :]
    RkT, NW = bv(A)[:, 0:16, :], bv(A)[:, 16:32, :]
    Ub = bv(T2)[:, 0:16, :]
    Ttmp = V[:, 0:4, :].rearrange("p a (b c) -> p (a b) c", c=64)
    mb = lambda nm, n: cv(nm).rearrange("p (o t) -> p o t", o=1).to_broadcast([128, n, 64])
    for qd in range(4):
        b1, b2, b3 = (0, 1, 2) if qd % 2 == 0 else (3, 4, 5)
        for par in range(2):
            pj = slice(64 * par, 64 * par + 64)
            for hpp in range(2):
                hp = 2 * qd + hpp
                hh = hpp * 2 + par
                for c in range(2):
                    pc = slice(64 * c, 64 * c + 64)
                    krc = KR[pj, hp, c, :, :].rearrange("p a t -> p (a t)")
                    P.pe(MM(self.psf[pc, b1, hh * 128:(hh + 1) * 128], BT[pj, hp, pc], krc), r=["BT", "KR"], w=[("pf", b1)], rg=("j", par))
                    P.pe(MM(self.psf[pc, b2, hh * 128:(hh + 1) * 128], KTl[pj, hp, pc], krc), r=["KTl", "KR"], w=[("pf", b2)], rg=("j", par))
                    P.pe(MM(self.psf[pc, b3, hh * 64:(hh + 1) * 64], KR[pj, hp, c, 0, :], BT[pj, hp, pc]), r=["BT", "KR"], w=[("pf", b3)], rg=("j", par))
        hs = slice(4 * qd, 4 * qd + 4)
        if os.environ.get("CHM") == "1":
            continue
        m1 = self.psf[:, b1, :].rearrange("p (h a t) -> p h a t", a=2, t=64)
        m2 = self.psf[:, b2, :].rearrange("p (h a t) -> p h a t", a=2, t=64)
        m3 = self.psf[:, b3, 0:256].rearrange("p (h t) -> p h t", t=64)
        P.vec(TT(XA[:, hs, :], m1[:, :, 0, :], mb("nsu", 4), ALU.mult), r=[("pf", b1), "cst"], w=["XA"])
        P.vec(TT(RbT[:, hs, :], m1[:, :, 1, :], mb("iu", 4), ALU.mult), r=[("pf", b1), "cst"], w=["RbT"])
        P.vec(TT(AkT[:, hs, :], m2[:, :, 0, :], mb("su", 4), ALU.mult), r=[("pf", b2), "cst"], w=["AkT"])
        P.vec(TT(RkT[:, hs, :], m2[:, :, 1, :], mb("iu", 4), ALU.mult), r=[("pf", b2), "cst"], w=["RkT"])
        P.vec(TT(XtA[:, hs, :], m3, mb("nsl", 4), ALU.mult), r=[("pf", b3), "cst"], w=["XtA"])
    if CHSTOP <= 2:
        P.barrier()
        return
    P.pool(TT(PA, XA, mb("i64", 16), ALU.add), r=["XA", "cst"], w=["PA"])
    Xc, Xtc, Xn, Xtn = (XA, "XA"), (XtA, "XtA"), (XB, "XB"), (XtB, "XtB")
    Pc, Pn = (PA, "PA"), (PB, "PB")
    bview = lambda b: self.psf[:, b, :].rearrange("p (h t) -> p h t", t=64)
    for lvl in range(5):
        last = lvl == 4
        for c in range(2):
            pc = slice(64 * c, 64 * c + 64)
            for h in range(16):
                col = slice((h % 8) * 64, (h % 8) * 64 + 64)
                if not last:
                    P.pe(MM(self.psf[pc, 0 + h // 8, col], Xtc[0][pc, h, :], Xc[0][pc, h, :]), r=[Xtc[1], Xc[1]], w=[("pf", 0 + h // 8)], rg=("t", c))
                P.pe(MM(self.psf[pc, 2 + h // 8, col], Xc[0][pc, h, :], Xtc[0][pc, h, :]), r=[Xtc[1], Xc[1]], w=[("pf", 2 + h // 8)], rg=("t", c))
        for hb in range(2):
            hs = slice(8 * hb, 8 * hb + 8)
            if not last:
                P.act(ACP(Xn[0][:, hs, :], bview(0 + hb)), r=[("pf", 0 + hb)], w=[Xn[1]])
            P.vec(CP(Xtn[0][:, hs, :], bview(2 + hb)), r=[("pf", 2 + hb)], w=[Xtn[1]])
        for c in range(2):
            pc = slice(64 * c, 64 * c + 64)
            for h in range(16):
                col = slice((h % 8) * 64, (h % 8) * 64 + 64)
                P.pe(MM(self.psf[pc, 4 + h // 8, col], Xtn[0][pc, h, :], Pc[0][pc, h, :]), r=[Xtn[1], Pc[1]], w=[("pf", 4 + h // 8)], rg=("t", c))
        for hb in range(2):
            hs = slice(8 * hb, 8 * hb + 8)
            P.vec(TT(Pn[0][:, hs, :], bview(4 + hb), Pc[0][:, hs, :], ALU.add), r=[("pf", 4 + hb), Pc[1]], w=[Pn[1]])
        Xc, Xn = Xn, Xc
        Xtc, Xtn = Xtn, Xtc
        Pc, Pn = Pn, Pc
    MT = Pc
    if CHSTOP <= 3:
        P.barrier()
        return
    Tst, Tb = self.Tst, self.Tb
    for c in range(2):
        pc = slice(64 * c, 64 * c + 64)
        hcol = lambda h: slice((h % 8) * 64, (h % 8) * 64 + 64)
        first = {0: True, 1: True}
        for par in range(2):
            pj = slice(64 * par, 64 * par + 64)
            for hp in range(8):
                h = 2 * hp + par
                P.pe(MM(self.psf[pc, h // 8, hcol(h)], KR[pj, hp, c, 0, :], Tb[pj, hp, :], first[h // 8], False, sgc=True),
                     r=["KR", "Tb"], w=[("pf", h // 8)], rg=("j", par))
                first[h // 8] = False
        for h in range(16):
            hp, par = h // 2, h % 2
            P.pe(MM(self.psf[pc, h // 8, hcol(h)], AkT[pc, h, :], VT[pc, hp, 64 * par:64 * par + 64], False, True, sgc=True),
                 r=["AkT", "bVT"], w=[("pf", h // 8)], rg=("t", c))
        for hb in range(2):
            hs = slice(8 * hb, 8 * hb + 8)
            P.act(ACTF(NW[pc, hs, :], bview(hb)[pc], AF.Copy, scale=-1.0), r=[("pf", hb)], w=["NW"])
        for h in range(16):
            P.pe(MM(self.psf[pc, 2 + h // 8, hcol(h)], MT[0][pc, h, :], NW[pc, h, :]), r=[MT[1], "NW"], w=[("pf", 2 + h // 8)], rg=("t", c))
        for hb in range(2):
            hs = slice(8 * hb, 8 * hb + 8)
            P.vec(CP(Ub[pc, hs, :], bview(2 + hb)[pc]), r=[("pf", 2 + hb)], w=["Ub"])
        first = {0: True, 1: True}
        for par in range(2):
            pj = slice(64 * par, 64 * par + 64)
            for hp in range(8):
                h = 2 * hp + par
                P.pe(MM(self.psf[pc, 4 + h // 8, hcol(h)], KR[pj, hp, c, 1, :], Tb[pj, hp, :], first[h // 8], False, sgc=True),
                     r=["KR", "Tb"], w=[("pf", 4 + h // 8)], rg=("j", par))
                first[h // 8] = False
        for h in range(16):
            hp, par = h // 2, h % 2
            vt = VT[pc, hp, 64 * par:64 * par + 64]
            P.pe(MM(self.psf[pc, 4 + h // 8, hcol(h)], RbT[pc, h, :], Ub[pc, h, :], False, False, sgc=True), r=["RbT", "Ub"], w=[("pf", 4 + h // 8)], rg=("t", c))
            P.pe(MM(self.psf[pc, 4 + h // 8, hcol(h)], RkT[pc, h, :], vt, False, True, sgc=True), r=["RkT", "bVT"], w=[("pf", 4 + h // 8)], rg=("t", c))
        for h in range(16):
            hp, par = h // 2, h % 2
            pj = slice(64 * par, 64 * par + 64)
            vt = VT[pc, hp, 64 * par:64 * par + 64]
            P.pe(MM(self.psf[pj, 0, hp * 64:(hp + 1) * 64], BHT[pc, hp, pj], Ub[pc, h, :], True, False), r=["BHT", "Ub"], w=[("pf", 0)], rg=("t", c))
            P.pe(MM(self.psf[pj, 0, hp * 64:(hp + 1) * 64], KHT[pc, hp, pj], vt, False, True), r=["KHT", "bVT"], w=[("pf", 0)], rg=("t", c))
        gcb = GC[:, :, c:c + 1].to_broadcast([128, 8, 64])
        P.pool(TT(Ttmp, Tst[:], gcb, ALU.mult), r=["Tst", "GC"], w=["Ttmp"])
        P.vec(TT(Tst[:], Ttmp, bview(0), ALU.add), r=["Ttmp", ("pf", 0)], w=["Tst"])
        P.act(ACP(Tb[:], Tst[:]), r=["Tst"], w=["Tb"])
    P.barrier()


Builder.rwkv_chunk_core = _rwkv_chunk_core
```

```python
import math
import numpy as np
import concourse.bass as bass
import concourse.mybir as mybir
from concourse.alu_op_type import AluOpType as ALU
from concourse.bass_utils import run_bass_kernel_spmd

F32 = mybir.dt.float32
BF16 = mybir.dt.bfloat16
I32 = mybir.dt.int32
AF = mybir.ActivationFunctionType
AX = mybir.AxisListType

D = 2048
W = 1024
HR = 16
PROJ = 3328
INA = 4352
DFF = 5632
RH = 8
DK = 256
DV = 512
INC = 12288
PAST = 16384
EPS = 1e-6
GN_EPS = 64e-5

ENGS = ("tensor", "vector", "scalar", "gpsimd", "sync")
CHUNKED = True
USE_SCRATCH = True


class Prog:
    def __init__(self, nc, stack):
        self.nc = nc
        self.ops = {e: [] for e in ENGS}
        self.seq = {e: 0 for e in ENGS}
        self.seen = {e: {} for e in ENGS}
        self.lastw = {}
        self.readers = {}
        self.esem = {e: stack.enter_context(nc.semaphore("es_" + e)) for e in ENGS}
        self.ndsem = 24
        self.dsem = [stack.enter_context(nc.semaphore("ds%d" % i)) for i in range(self.ndsem)]
        self.dcnt = [0] * self.ndsem
        self.drr = 0
        self.pending_dma = []

    def _need(self, eng, ev, waits, force=False):
        key, sem, val, src = ev
        if self.seen[eng].get(key, 0) >= val:
            return
        self.seen[eng][key] = val
        waits.append((sem, val))

    def op(self, eng, fn, r=(), w=(), dma=False, rg=None):
        waits = []
        if eng == "tensor":
            pfrg = self.__dict__.setdefault("pfrg", {})
            for k in w:
                last = pfrg.get(k)
                if last is not None and last[0] != rg:
                    self._need(eng, last[1], waits)
        for k in r:
            ev = self.lastw.get(k)
            if ev is not None:
                if ev[3] == eng and not dma and eng == "tensor":
                    pass
                else:
                    self._need(eng, ev, waits)
        for k in r:
            if isinstance(k, tuple) and k[0] in ("pf", "pb"):
                for ev2 in self.readers.get(k, ()):
                    if ev2[3] != eng:
                        self._need(eng, ev2, waits)
        for k in w:
            ev = self.lastw.get(k)
            if ev is not None and (dma or ev[3] != eng):
                self._need(eng, ev, waits)
            for ev2 in self.readers.get(k, ()):
                if dma or ev2[3] != eng:
                    self._need(eng, ev2, waits)
        if dma:
            i = self.drr
            self.drr = (self.drr + 1) % self.ndsem
            if self.dcnt[i] > 0:
                self._need(eng, (("D", i), self.dsem[i], self.dcnt[i], None), waits)
            self.dcnt[i] += 16
            ev = (("D", i), self.dsem[i], self.dcnt[i], None)
            inc = (self.dsem[i], 16)
            self.pending_dma.append(ev)
        else:
            self.seq[eng] += 1
            ev = (("E", eng), self.esem[eng], self.seq[eng], eng)
            inc = (self.esem[eng], 1)
            self.seen[eng][("E", eng)] = max(self.seen[eng].get(("E", eng), 0), 0)
        for k in r:
            self.readers.setdefault(k, []).append(ev)
        for k in w:
            self.lastw[k] = ev
            self.readers[k] = []
            if eng == "tensor":
                self.pfrg[k] = (rg, ev)
        self.nrec = getattr(self, "nrec", 0) + 1
        import os
        if self.nrec > int(os.environ.get("KLIMIT", "100000000")):
            fn = lambda e: e.nop()
        self.ops[eng].append((waits, fn, inc))
        return ev

    def pe(self, fn, r=(), w=(), rg=None):
        return self.op("tensor", fn, r, w, rg=rg)

    def vec(self, fn, r=(), w=()):
        return self.op("vector", fn, r, w)

    def act(self, fn, r=(), w=()):
        return self.op("scalar", fn, r, w)

    def pool(self, fn, r=(), w=()):
        return self.op("gpsimd", fn, r, w)

    def dma(self, q, out, in_, r=(), w=(), **kw):
        return self.op(q, lambda e: e.dma_start(out=out, in_=in_, **kw), r, w, dma=True)

    def barrier(self):
        evs = [(("E", e), self.esem[e], self.seq[e], e) for e in ENGS if self.seq[e] > 0]
        evs += [(("D", i), self.dsem[i], self.dcnt[i], None) for i in range(self.ndsem) if self.dcnt[i] > 0]
        for e in ENGS:
            waits = []
            for ev in evs:
                if ev[3] == e:
                    continue
                self._need(e, ev, waits)
            if waits:
                self.ops[e].append((waits, None, None))
        self.lastw = {}
        self.readers = {}
        self.pfrg = {}

    def replay(self, eng, e):
        for waits, fn, inc in self.ops[eng]:
            for sem, val in waits:
                e.wait_ge(sem, val)
            if fn is not None:
                ins = fn(e)
                ins.then_inc(inc[0], inc[1])


def TT(out, in0, in1, op):
    return lambda e: e.tensor_tensor(out=out, in0=in0, in1=in1, op=op)


def TS(out, in0, s1, op0, s2=None, op1=None):
    if op1 is None:
        return lambda e: e.tensor_scalar(out=out, in0=in0, scalar1=s1, scalar2=None, op0=op0)
    return lambda e: e.tensor_scalar(out=out, in0=in0, scalar1=s1, scalar2=s2, op0=op0, op1=op1)


def STT(out, in0, scalar, in1, op0, op1):
    return lambda e: e.scalar_tensor_tensor(out=out, in0=in0, scalar=scalar, in1=in1, op0=op0, op1=op1)


def ACTF(out, in_, func, bias=None, scale=None, accum_out=None):
    kw = {}
    if bias is not None:
        kw["bias"] = bias
    if scale is not None:
        kw["scale"] = scale
    if accum_out is not None:
        kw["accum_out"] = accum_out
    return lambda e: e.activation(out=out, in_=in_, func=func, **kw)


def ACP(out, in_):
    return lambda e: e.activation(out=out, in_=in_, func=AF.Copy)


def CP(out, in_):
    return lambda e: e.tensor_copy(out=out, in_=in_)


def MM(out, lhsT, rhs, start=True, stop=True, sgc=False):
    if sgc:
        return lambda e: e.matmul(out, lhsT=lhsT, rhs=rhs, start=start, stop=stop, skip_group_check=True)
    return lambda e: e.matmul(out, lhsT=lhsT, rhs=rhs, start=start, stop=stop)


def TR(out, in_, ident):
    return lambda e: e.transpose(out, in_, ident)


def MS(ap, val):
    return lambda e: e.memset(ap, val)


def fm(v):
    v = np.asarray(v, np.float32).reshape(-1, 128)
    return np.ascontiguousarray(v.T)


def tile_w(w, gw):
    K, N = w.shape
    kc = K // 128
    g = N // gw
    t = w.reshape(kc, 128, g, gw).transpose(2, 1, 0, 3)
    return np.ascontiguousarray(t).reshape(g, 128, kc * gw)


def tile_rows(w, rk):
    K, N = w.shape
    g = K // (128 * rk)
    t = w.reshape(g, rk, 128, N).transpose(0, 2, 1, 3)
    return np.ascontiguousarray(t).reshape(g, 128, rk * N)


def make_consts():
    c = {}
    c["ident"] = np.eye(128, dtype=np.float32)
    bo = np.zeros((128, 128), np.float32)
    bo[:64, :64] = 1.0
    bo[64:, 64:] = 1.0
    c["bones"] = bo
    c["iota"] = np.broadcast_to(np.arange(512, dtype=np.float32)[None, :], (128, 512)).copy()
    log_g = np.log(1.0 - np.exp2(-5.0 - np.arange(RH, dtype=np.float64)))
    idx = np.arange(128, dtype=np.float64)
    dist = idx[None, :] - idx[:, None]
    intra = np.where(dist >= 0, np.exp(log_g[:, None, None] * np.maximum(dist, 0.0)), 0.0)
    c["intraT"] = np.ascontiguousarray(intra.transpose(1, 0, 2)).astype(np.float32).reshape(128, RH * 128)
    c["qs"] = np.exp(log_g[None, :] * (idx[:, None] + 1.0)).astype(np.float32)
    c["ks"] = np.exp(log_g[None, :] * (127.0 - idx[:, None])).astype(np.float32)
    pp = np.arange(128)
    c["m0"] = ((pp // 32) % 2 == 0).astype(np.float32).reshape(128, 1)
    c["m1"] = ((pp // 32) % 2 == 1).astype(np.float32).reshape(128, 1)
    r64 = (pp % 64)[:, None]
    t64 = np.arange(64)[None, :]
    c["nsu"] = -(t64 > r64).astype(np.float32)
    c["su"] = (t64 > r64).astype(np.float32)
    c["nsl"] = -(t64 < r64).astype(np.float32)
    c["iu"] = (t64 >= r64).astype(np.float32)
    c["i64"] = (t64 == r64).astype(np.float32)
    c["rmask"] = np.broadcast_to((np.arange(128) % 64 != 0).astype(np.float32)[None, :], (128, 128)).copy()
    c["cd"] = np.exp(log_g * 128.0)
    c["g1"] = np.exp(log_g)
    return c


def rot_tables(pos):
    half = 128
    freq = (1.0 / (10000.0 ** np.linspace(0.0, 1.0, half, dtype=np.float32))).astype(np.float32)
    ang = (np.asarray(pos, np.float32)[None, :] * freq[:, None]).astype(np.float32)
    cs = np.cos(ang).astype(np.float32)
    sn = np.sin(ang).astype(np.float32)
    return np.ascontiguousarray(np.stack([cs, sn, cs / 16.0, sn / 16.0], axis=1).astype(np.float32))


CONST_LAYOUT = [("ident", 128), ("bones", 128), ("iota", 512), ("intraT", RH * 128), ("qs", RH), ("ks", RH), ("m0", 1), ("m1", 1), ("nsu", 64), ("su", 64), ("nsl", 64), ("iu", 64), ("i64", 64), ("rmask", 128)]
VEC_LAYOUT = [("nm0", 16), ("nf0", 16), ("nm1", 16), ("nf1", 16), ("mu", 26), ("w0", 8), ("a0", 8), ("kk", 8),
              ("ka", 8), ("rk", 8), ("lnw", 8), ("lnb", 8), ("s5d", 8), ("bglu", 8)]


def _offsets(layout):
    o = {}
    p = 0
    for n, k in layout:
        o[n] = (p, k)
        p += k
    return o, p


COFF, NCONST = _offsets(CONST_LAYOUT)
VOFF, NVEC = _offsets(VEC_LAYOUT)


class Builder:
    def __init__(self, seq, nsub, dbg=(), stages=None, with_sample=True):
        from contextlib import ExitStack
        self.SEQ = seq
        self.NSUB = nsub
        self.T = nsub * 128
        self.NTILE = seq // self.T
        self.dbg = set(dbg)
        self.stages = stages
        self.with_sample = with_sample
        self.stack = ExitStack()
        self.nc = bass.Bass("TRN2", target_bir_lowering=False)
        self.P = Prog(self.nc, self.stack)
        self.dram_in = {}
        self.dram_out = {}
        self.wslot = 0
        self.gbank = 0
        self.scr = {}
        self.scr_done = {}
        self.dbg_shapes = {}

    def din(self, name, shape, dt=F32):
        t = self.nc.dram_tensor(name, list(shape), dt, kind="ExternalInput").ap()
        self.dram_in[name] = t
        return t

    def dout(self, name, shape, dt=F32):
        t = self.nc.dram_tensor(name, list(shape), dt, kind="ExternalOutput").ap()
        self.dram_out[name] = t
        return t

    def sb(self, name, shape, dt=F32, stack=None):
        self._uid = getattr(self, "_uid", 0) + 1
        return (stack or self.stack).enter_context(self.nc.sbuf_tensor("sb%d_%s" % (self._uid, name), list(shape), dt))

    def on(self, st):
        return self.stages is None or st in self.stages

    def dump(self, name, ap, shape, r, dt=F32):
        if name not in self.dbg:
            return
        o = self.dout("dbg_" + name, shape, dt)
        self.P.dma("sync", o, ap, r=r, w=["dbg_" + name])

    def wload(self, src_ap, kc, gw, part=None):
        s = self.wslot
        self.wslot = (self.wslot + 1) % 4
        key = "wb%d" % s
        view = self.wbuf[:, s, 0:kc * gw]
        name = src_ap.tensor.name
        gidx = src_ap.offset // (128 * 4096)
        if not USE_SCRATCH:
            self.P.dma("gpsimd", view, src_ap, w=[key])
            return view.rearrange("p (k g) -> p k g", g=gw), key
        if name not in self.scr:
            ng = 1
            for d in src_ap.tensor.shape:
                ng *= d
            ng //= 128 * 4096
            self.scr[name] = self.nc.dram_tensor("scr_" + name, [ng, 128, 4096], BF16, kind="Internal").ap()
            self.scr_done[name] = set()
        sk = ("scr", name, gidx)
        if gidx not in self.scr_done[name]:
            self.scr_done[name].add(gidx)
            self.P.dma("gpsimd", view, src_ap, w=[key])
            self.P.dma("sync", self.scr[name][gidx], view, r=[key], w=[sk])
        else:
            self.P.dma("sync", view, self.scr[name][gidx], r=[sk], w=[key])
        return view.rearrange("p (k g) -> p k g", g=gw), key

    def bank(self, lo=0, hi=4):
        b = lo + (self.gbank % (hi - lo))
        self.gbank += 1
        return b

    def declare(self):
        SEQ = self.SEQ
        self.xp = self.din("xp", [SEQ, D])
        self.consts_d = self.din("consts", [128, NCONST])
        self.vecs_d = self.din("vecs", [128, NVEC])
        self.rot_d = self.din("rot", [128, 4, SEQ + 1])
        self.nfin_d = self.din("nfin", [1, D])
        self.retgn_d = self.din("retgn", [RH, DV])
        self.w2a2_d = self.din("w2a2", [128, W])
        self.g2_d = self.din("g2", [128, W])
        self.s5a_d = self.din("s5a", [128, 3, 32])
        self.s5b_d = self.din("s5b", [128, 2, 32 * 16])
        self.s5c_d = self.din("s5c", [128, 2, 32 * 64])
        if self.on("rwkv") or self.on("s5"):
            self.wa_d = self.din("wa", [17, 128, 16 * 256])
            self.wglu_d = self.din("wglu", [2, 128, 8 * 512])
            self.woa_d = self.din("woa", [8, 128, 16 * 256])
        self.wg_d, self.wu_d, self.wd_d = {}, {}, {}
        for i in range(2):
            if self.on("ffn%d" % i):
                self.wg_d[i] = self.din("wg%d" % i, [22, 128, 16 * 256])
                self.wu_d[i] = self.din("wu%d" % i, [22, 128, 16 * 256])
                self.wd_d[i] = self.din("wd%d" % i, [22, 128, 2 * D])
        if self.on("ret"):
            self.wc_d = self.din("wc", [RH, 6, 128, 16 * 256])
            self.woc_d = self.din("woc", [RH, 2, 128, 2 * D])
        if self.with_sample:
            self.xs_d = self.din("xs", [16, D])
            self.smp = {"st_rwkv": self.din("st_rwkv", [16, HR, 64, 64]), "st_shift": self.din("st_shift", [16, PROJ]),
                        "st_s5": self.din("st_s5", [2, 16, 4096]), "st_ret": self.din("st_ret", [16, RH, DK, DV]),
                        "s_rwkv": self.dout("s_rwkv", [16, HR, 64, 64]), "s_shift": self.dout("s_shift", [16, PROJ]),
                        "s_s5": self.dout("s_s5", [2, 16, 4096]), "s_ret": self.dout("s_ret", [16, RH, DK, DV])}
            self.ys = self.dout("ys", [16, D])
        self.yp = self.dout("yp", [SEQ, D])
        self.o_prwkv = self.dout("p_rwkv", [HR, 64, 64])
        self.o_pshift = self.dout("p_shift", [26, 128])
        self.o_ps5 = self.dout("p_s5", [2, 32, 128])
        self.o_pret = self.dout("p_ret", [RH, 2, 128, DV])

    def alloc(self):
        T, NSUB = self.T, self.NSUB
        nc = self.nc
        self.x = self.sb("x_tm", [128, NSUB, D])
        self.hT = self.sb("hT", [128, 16, T], BF16)
        self.wbuf = self.sb("wbuf", [128, 4, 4096], BF16)
        self.cst = self.sb("cst", [128, NCONST])
        self.vecs = self.sb("vecs", [128, NVEC])
        self.identb = self.sb("identb", [128, 128], BF16)
        self.nfin = self.sb("nfin_bc", [128, D])
        self.w2a2 = self.sb("w2a2", [128, W], BF16)
        self.g2 = self.sb("g2", [128, W], BF16)
        self.rot = self.sb("rot", [128, 4, T])
        self.junk = self.sb("junk", [128, D], BF16)
        self.stat = self.sb("stat", [128, 16])
        self.omm = self.sb("omm", [128, 26])
        self.psf = self.stack.enter_context(nc.psum_tensor("psf", [128, 6, 512], F32))
        self.psb = self.stack.enter_context(nc.psum_tensor("psb", [128, 2, 1024], BF16))

    def cv(self, name):
        o, k = COFF[name]
        return self.cst[:, o:o + k]

    def vv(self, name, c=None):
        o, k = VOFF[name]
        if c is None:
            return self.vecs[:, o:o + k]
        return self.vecs[:, o + c:o + c + 1]

    def setup(self):
        P = self.P
        P.dma("sync", self.cst[:], self.consts_d[:, :], w=["cst"])
        P.dma("sync", self.vecs[:], self.vecs_d[:, :], w=["vecs"])
        P.dma("sync", self.nfin[:], self.nfin_d[0, :].partition_broadcast(128), w=["nfin"])
        P.dma("gpsimd", self.w2a2[:], self.w2a2_d[:, :], w=["w2a2"])
        P.dma("gpsimd", self.g2[:], self.g2_d[:, :], w=["g2"])
        P.vec(CP(self.identb[:], self.cv("ident")), r=["cst"], w=["identb"])
        mo, mk = VOFF["mu"]
        P.vec(TS(self.omm[:], self.vecs[:, mo:mo + mk], -1.0, ALU.mult, 1.0, ALU.add), r=["vecs"], w=["omm"])

    def norm_to_hT(self, gname, ntok=128, nsub=None, x=None, hT=None):
        P = self.P
        nsub = self.NSUB if nsub is None else nsub
        x = self.x if x is None else x
        hT = self.hT if hT is None else hT
        for s in range(nsub):
            xs = x[0:ntok, s, :]
            ss = self.stat[0:ntok, 0:1]
            P.act(ACTF(self.junk[0:ntok, :], xs, AF.Square, accum_out=ss), r=[("x", s)], w=["junk", "stat0"])
            P.act(ACTF(self.stat[0:ntok, 1:2], ss, AF.Ln, scale=1.0 / D, bias=self.epsc[0:ntok, 0:1]), r=["stat0", "epsc"], w=["stat1"])
            P.act(ACTF(self.stat[0:ntok, 2:3], self.stat[0:ntok, 1:2], AF.Exp, scale=-0.5), r=["stat1"], w=["stat2"])
            P.vec(TS(self.junk[0:ntok, :], xs, self.stat[0:ntok, 2:3], ALU.mult), r=[("x", s), "stat2"], w=["junk"])
            for half in range(2):
                pb = ("pb", half)
                for k8 in range(8):
                    kc = half * 8 + k8
                    P.pe(TR(self.psb[:, half, k8 * 128:k8 * 128 + ntok], self.junk[0:ntok, kc * 128:(kc + 1) * 128],
                            self.identb[0:ntok, 0:ntok]), r=["junk", "identb"], w=[pb])
                for k8 in range(8):
                    kc = half * 8 + k8
                    src = self.psb[:, half, k8 * 128:k8 * 128 + ntok]
                    dst = hT[:, kc, s * 128:s * 128 + ntok]
                    g = self.vv(gname, kc)
                    if False:
                        P.act(ACTF(dst, src, AF.Identity, scale=g), r=[pb, "vecs"], w=[("hT", kc)])
                    else:
                        P.vec(TS(dst, src, g, ALU.mult), r=[pb, "vecs"], w=[("hT", kc)])

    def tm_accum(self, lhs_fn, kcn, w_fn, w_keys, lhs_keys, ntok=128, nsub=None, x=None, xkey="x", banks=(0, 4)):
        P = self.P
        nsub = self.NSUB if nsub is None else nsub
        x = self.x if x is None else x
        for s in range(nsub):
            for cb in range(4):
                b = self.bank(*banks)
                pk = ("pf", b)
                for kc in range(kcn):
                    P.pe(MM(self.psf[0:ntok, b, :], lhs_fn(kc, s), w_fn(kc, cb), start=(kc == 0), stop=(kc == kcn - 1)),
                         r=list(lhs_keys) + list(w_keys), w=[pk])
                xs = x[0:ntok, s, cb * 512:(cb + 1) * 512]
                P.vec(TT(xs, xs, self.psf[0:ntok, b, :], ALU.add), r=[pk, (xkey, s)], w=[(xkey, s)])

    def ffn(self, li, ntok=128, nsub=None, x=None, hT=None, stack=None):
        P = self.P
        nsub = self.NSUB if nsub is None else nsub
        T = nsub * ntok if ntok == 128 else ntok
        hT = self.hT if hT is None else hT
        actv = self.ffn_act
        sil = self.ffn_sil
        for g in range(22):
            wg, kg = self.wload(self.wg_d[li][g], 16, 256)
            wu, ku = self.wload(self.wu_d[li][g], 16, 256)
            wd, kd = self.wload(self.wd_d[li][g], 2, D)
            for m in range(2):
                bg = self.bank(0, 6)
                bu = self.bank(0, 6)
                for kc in range(16):
                    P.pe(MM(self.psf[:, bg, 0:T], wg[:, kc, m * 128:(m + 1) * 128], hT[:, kc, 0:T], kc == 0, kc == 15),
                         r=[kg, ("hT", kc)], w=[("pf", bg)])
                for kc in range(16):
                    P.pe(MM(self.psf[:, bu, 0:T], wu[:, kc, m * 128:(m + 1) * 128], hT[:, kc, 0:T], kc == 0, kc == 15),
                         r=[ku, ("hT", kc)], w=[("pf", bu)])
                P.act(ACTF(sil[:, 0:T], self.psf[:, bg, 0:T], AF.Silu), r=[("pf", bg)], w=["sil"])
                P.vec(TT(actv[:, m, 0:T], sil[:, 0:T], self.psf[:, bu, 0:T], ALU.mult), r=["sil", ("pf", bu)], w=[("actv", m)])
            self.tm_accum(lambda kc, s: actv[:, kc, s * 128:s * 128 + ntok], 2,
                          lambda kc, cb: wd[:, kc, cb * 512:(cb + 1) * 512], [kd], [("actv", 0), ("actv", 1)],
                          ntok=ntok, nsub=nsub, x=x, banks=(0, 6))

    def outproj_a(self, oFM, ntok=128, nsub=None, x=None):
        for cb2 in range(8):
            wo, ko = self.wload(self.woa_d[cb2], 16, 256)
            P = self.P
            nsub_ = self.NSUB if nsub is None else nsub
            x_ = self.x if x is None else x
            for s in range(nsub_):
                b = self.bank(0, 6)
                pk = ("pf", b)
                for kc in range(16):
                    P.pe(MM(self.psf[0:ntok, b, 0:256], oFM[:, kc, s * 128:s * 128 + ntok], wo[:, kc, :], kc == 0, kc == 15),
                         r=[("oFM", kc), ko], w=[pk])
                xs = x_[0:ntok, s, cb2 * 256:(cb2 + 1) * 256]
                P.vec(TT(xs, xs, self.psf[0:ntok, b, 0:256], ALU.add), r=[pk, ("x", s)], w=[("x", s)])

    def final_norm(self, out_ap_fn, ntok=128, nsub=None, x=None):
        P = self.P
        nsub = self.NSUB if nsub is None else nsub
        x = self.x if x is None else x
        for s in range(nsub):
            xs = x[0:ntok, s, :]
            ss = self.stat[0:ntok, 4:5]
            P.act(ACTF(self.junk[0:ntok, :], xs, AF.Square, accum_out=ss), r=[("x", s)], w=["junk", "stat4"])
            P.act(ACTF(self.stat[0:ntok, 5:6], ss, AF.Ln, scale=1.0 / D, bias=self.epsc[0:ntok, 0:1]), r=["stat4", "epsc"], w=["stat5"])
            P.act(ACTF(self.stat[0:ntok, 6:7], self.stat[0:ntok, 5:6], AF.Exp, scale=-0.5), r=["stat5"], w=["stat6"])
            P.vec(STT(xs, xs, self.stat[0:ntok, 6:7], self.nfin[0:ntok, :], ALU.mult, ALU.mult), r=[("x", s), "stat6", "nfin"], w=[("x", s)])
            P.dma("gpsimd", out_ap_fn(s), xs, r=[("x", s)], w=["yout"])

    def layer1(self, first_tile, last_tile):
        from contextlib import ExitStack
        P = self.P
        T, NSUB = self.T, self.NSUB
        CC = make_consts()
        P.barrier()
        with ExitStack() as st:
            Qp = self.sb("r_Qp", [128, 2, T], F32, st)
            Kp = self.sb("r_Kp", [128, 2, T], F32, st)
            TA = self.sb("r_TA", [128, T], F32, st)
            TB = self.sb("r_TB", [128, T], F32, st)
            Qr2 = [self.sb("r_Qr%d" % i, [128, 2, T], BF16, st) for i in range(2)]
            Kr2 = [self.sb("r_Kr%d" % i, [128, 2, T], BF16, st) for i in range(2)]
            KT2 = [self.sb("r_KT%d" % i, [128, NSUB, 256], BF16, st) for i in range(2)]
            V2 = [self.sb("r_V%d" % i, [128, NSUB, 512], BF16, st) for i in range(2)]
            GS2 = [self.sb("r_GS%d" % i, [128, NSUB, 512], BF16, st) for i in range(2)]
            Sf = self.sb("r_Sf", [128, 2, 512], F32, st)
            Sb = self.sb("r_Sb", [128, 2, 512], BF16, st)
            ATT = self.sb("r_ATT", [128, 128], BF16, st)
            CR = self.sb("r_CR", [128, 512], F32, st)
            O = self.sb("r_O", [128, 512], F32, st)
            TMP = self.sb("r_TMP", [128, 512], F32, st)
            GA = self.sb("r_GA", [128, 512], BF16, st)
            GAT = self.sb("r_GAT", [128, 4, T], BF16, st)
            gnb = self.sb("r_gnb", [128, 512], F32, st)
            cos, sin, cos16, sin16 = (self.rot[:, i, :] for i in range(4))

            def proj(h):
                i = h % 2
                Qr, Kr, KT, V, GS = Qr2[i], Kr2[i], KT2[i], V2[i], GS2[i]
                for which, dst, (c_, s_), rdst in ((0, Qp, (cos, sin), Qr), (1, Kp, (cos16, sin16), Kr)):
                    wq, kq = self.wload(self.wc_d[h, which], 16, 256)
                    nm = "QK"[which]
                    for dkc in range(2):
                        b = self.bank(0, 3)
                        for kc in range(16):
                            P.pe(MM(self.psf[:, b, 0:T], wq[:, kc, dkc * 128:(dkc + 1) * 128], self.hT[:, kc, 0:T], kc == 0, kc == 15),
                                 r=[kq, ("hT", kc)], w=[("pf", b)])
                        P.act(ACP(dst[:, dkc, :], self.psf[:, b, 0:T]), r=[("pf", b)], w=[(nm + "p", dkc)])
                        yield
                    P.vec(TT(TA[:], dst[:, 0, :], c_, ALU.mult), r=[(nm + "p", 0), "rot"], w=["TA"])
                    P.vec(TT(TB[:], dst[:, 1, :], s_, ALU.mult), r=[(nm + "p", 1), "rot"], w=["TB"])
                    P.vec(TT(rdst[:, 0, :], TA[:], TB[:], ALU.subtract), r=["TA", "TB"], w=[(nm + "r", i, 0)])
                    P.vec(TT(TA[:], dst[:, 1, :], c_, ALU.mult), r=[(nm + "p", 1), "rot"], w=["TA"])
                    P.vec(TT(TB[:], dst[:, 0, :], s_, ALU.mult), r=[(nm + "p", 0), "rot"], w=["TB"])
                    P.vec(TT(rdst[:, 1, :], TA[:], TB[:], ALU.add), r=["TA", "TB"], w=[(nm + "r", i, 1)])
                    yield
                for s in range(NSUB):
                    for dkc in range(2):
                        slot = (s * 2 + dkc) % 8
                        pb = ("pb", 1)
                        P.pe(TR(self.psb[:, 1, slot * 128:(slot + 1) * 128], Kr[:, dkc, s * 128:(s + 1) * 128], self.identb[:]),
                             r=[("Kr", i, dkc), "identb"], w=[pb])
                    o_, k_ = COFF["ks"]
                    for dkc in range(2):
                        slot = (s * 2 + dkc) % 8
                        P.vec(TS(KT[:, s, dkc * 128:(dkc + 1) * 128], self.psb[:, 1, slot * 128:(slot + 1) * 128],
                                 self.cst[:, o_ + h:o_ + h + 1], ALU.mult), r=[("pb", 1), "cst"], w=[("KT", i, s)])
                    yield
                for which, dst, nm in ((2, V, "V"), (4, GS, "GS")):
                    for vg in range(2):
                        wv, kv = self.wload(self.wc_d[h, which + vg], 16, 256)
                        for s in range(NSUB):
                            b = self.bank(0, 3)
                            for kc in range(16):
                                P.pe(MM(self.psf[:, b, 0:256], self.hT[:, kc, s * 128:(s + 1) * 128], wv[:, kc, :], kc == 0, kc == 15),
                                     r=[kv, ("hT", kc)], w=[("pf", b)])
                            if nm == "V":
                                P.act(ACP(dst[:, s, vg * 256:(vg + 1) * 256], self.psf[:, b, 0:256]), r=[("pf", b)], w=[(nm, i, s)])
                            else:
                                P.act(ACTF(dst[:, s, vg * 256:(vg + 1) * 256], self.psf[:, b, 0:256], AF.Silu), r=[("pf", b)], w=[(nm, i, s)])
                            yield

            def drain(g, n):
                if g is None:
                    return
                for _ in range(n):
                    try:
                        next(g)
                    except StopIteration:
                        return

            for _ in proj(0):
                pass
            for h in range(RH):
                i = h % 2
                Qr, Kr, KT, V, GS = Qr2[i], Kr2[i], KT2[i], V2[i], GS2[i]
                nxt = proj(h + 1) if h + 1 < RH else None
                P.dma("sync", gnb[:], self.retgn_d[h, :].partition_broadcast(128), w=["gnb"])
                if first_tile:
                    P.vec(MS(Sf[:], 0.0), w=["Sf0", "Sf1"])
                else:
                    P.dma("sync", Sf[:], self.o_pret[h].rearrange("a p e -> p a e"), w=["Sf0", "Sf1"], r=[("scr", h)])
                P.act(ACP(Sb[:], Sf[:]), r=["Sf0", "Sf1"], w=["Sb"])
                qo, _ = COFF["qs"]
                io, _ = COFF["intraT"]
                for s in range(NSUB):
                    sl = slice(s * 128, (s + 1) * 128)
                    b = 3
                    for dkc in range(2):
                        P.pe(MM(self.psf[:, b, 0:128], Kr[:, dkc, sl], Qr[:, dkc, sl], dkc == 0, dkc == 1),
                             r=[("Kr", i, dkc), ("Qr", i, dkc)], w=[("pf", b)])
                    bc = 4
                    for dkc in range(2):
                        P.pe(MM(self.psf[:, bc, :], Qr[:, dkc, sl], Sb[:, dkc, :], dkc == 0, dkc == 1),
                             r=[("Qr", i, dkc), "Sb"], w=[("pf", bc)])
                    P.vec(TT(ATT[:], self.psf[:, b, 0:128], self.cst[:, io + h * 128:io + (h + 1) * 128], ALU.mult),
                          r=[("pf", b), "cst"], w=["ATT"])
                    P.vec(TS(CR[:], self.psf[:, bc, :], self.cst[:, qo + h:qo + h + 1], ALU.mult), r=[("pf", bc), "cst"], w=["CR"])
                    for dkc in range(2):
                        bs = 3 + dkc if False else (5 if dkc == 0 else 3)
                        P.pe(MM(self.psf[:, bs, :], KT[:, s, dkc * 128:(dkc + 1) * 128], V[:, s, :]), r=[("KT", i, s), ("V", i, s)], w=[("pf", bs)])
                        if dkc == 0:
                            drain(nxt, 1)
                    bi = 4
                    P.pe(MM(self.psf[:, bi, :], ATT[:], V[:, s, :]), r=["ATT", ("V", i, s)], w=[("pf", bi)])
                    drain(nxt, 1)
                    for dkc in range(2):
                        bs = 5 if dkc == 0 else 3
                        P.vec(STT(Sf[:, dkc, :], Sf[:, dkc, :], float(CC["cd"][h]), self.psf[:, bs, :], ALU.mult, ALU.add),
                              r=[("pf", bs), "Sf%d" % dkc], w=["Sf%d" % dkc])
                    P.act(ACP(Sb[:], Sf[:]), r=["Sf0", "Sf1"], w=["Sb"])
                    P.vec(TT(O[:], self.psf[:, bi, :], CR[:], ALU.add), r=[("pf", bi), "CR"], w=["O"])
                    drain(nxt, 1)
                    P.act(ACTF(self.junk[:, 0:512], O[:], AF.Square, accum_out=self.stat[:, 8:9]), r=["O"], w=["junk", "stat8"])
                    P.act(ACTF(self.stat[:, 9:10], self.stat[:, 8:9], AF.Ln, scale=1.0 / DV, bias=self.epsc[:, 0:1]), r=["stat8", "epsc"], w=["stat9"])
                    P.act(ACTF(self.stat[:, 10:11], self.stat[:, 9:10], AF.Exp, scale=-0.5), r=["stat9"], w=["stat10"])
                    P.vec(STT(TMP[:], O[:], self.stat[:, 10:11], gnb[:], ALU.mult, ALU.mult), r=["O", "stat10", "gnb"], w=["TMP"])
                    P.vec(TT(GA[:], TMP[:], GS[:, s, :], ALU.mult), r=["TMP", ("GS", i, s)], w=["GA"])
                    drain(nxt, 1)
                    for ec in range(4):
                        P.pe(TR(self.psb[:, 0, ec * 128:(ec + 1) * 128], GA[:, ec * 128:(ec + 1) * 128], self.identb[:]),
                             r=["GA", "identb"], w=[("pb", 0)])
                    for ec in range(4):
                        P.act(ACP(GAT[:, ec, sl], self.psb[:, 0, ec * 128:(ec + 1) * 128]), r=[("pb", 0)], w=[("GAT", ec)])
                    drain(nxt, 2)
                P.dma("sync" if first_tile else "gpsimd", self.o_pret[h].rearrange("a p e -> p a e"), Sf[:], r=["Sf0", "Sf1"], w=[("scr", h)])
                drain(nxt, 100)
                wo0, k0 = self.wload(self.woc_d[h, 0], 2, D)
                wo1, k1 = self.wload(self.woc_d[h, 1], 2, D)
                self.tm_accum(lambda kc, s: GAT[:, kc, s * 128:(s + 1) * 128], 4,
                              lambda kc, cb: (wo0 if kc < 2 else wo1)[:, kc % 2, cb * 512:(cb + 1) * 512],
                              [k0, k1], [("GAT", e) for e in range(4)])
            P.barrier()

    def bc(self, name, c0, n, ntok):
        o, _ = VOFF[name]
        return self.vecs[:, o + c0:o + c0 + n].rearrange("p (a b) -> p a b", b=1).to_broadcast([128, n, ntok])

    def rwkv_alloc(self, st, chunked=False):
        B = {}
        if chunked:
            B["KR"] = self.sb("c_KR", [128, 8, 2, 2, 64], BF16, st)
            for nm in ("BT", "KTl", "BH", "KH", "BHT", "KHT"):
                B[nm] = self.sb("c_" + nm, [128, 8, 128], BF16, st)
            B["GC"] = self.sb("c_GC", [128, 8, 2], F32, st)
        for nm in ("R", "K", "V", "A", "KK", "BB", "KM", "T1", "T2"):
            B[nm] = self.sb("w_" + nm, [128, 8, 128], F32, st)
        B["P4"] = self.sb("w_P4", [128, 2, 129], F32, st)
        B["PA"] = self.sb("w_PA", [128, 2, 128], F32, st)
        B["PB"] = self.sb("w_PB", [128, 2, 128], F32, st)
        B["XL"] = self.sb("w_XL", [128, 2, 128], F32, st)
        B["TXW"] = self.sb("w_TXW", [128, 128], BF16, st)
        B["SXG"] = self.sb("w_SXG", [128, 128], BF16, st)
        B["Vb"] = self.sb("w_Vb", [128, 8, 128], BF16, st)
        B["VT"] = self.sb("w_VT", [128, 8, 128], BF16, st)
        if not chunked:
            B["S1"] = self.sb("w_S1", [128, 512], F32, st)
            B["S2"] = self.sb("w_S2", [128, 512], F32, st)
            B["S3"] = self.sb("w_S3", [128, 512], F32, st)
            B["S4"] = self.sb("w_S4", [128, 512], F32, st)
            B["S5"] = self.sb("w_S5", [128, 512], BF16, st)
        B["YB"] = self.sb("w_YB", [128, 8, 128], BF16, st)
        B["ST"] = self.sb("w_ST", [128, 64], F32, st)
        return B

    def rwkv_subtile(self, B, s, oFM, ntok=128, prev=None, hT=None, Tstates=None, step_post=None, chunked=False):
        P = self.P
        hT = self.hT if hT is None else hT
        R, K, V, A, KK, BB, KM, T1, T2 = (B[n] for n in ("R", "K", "V", "A", "KK", "BB", "KM", "T1", "T2"))
        P4, PA, PB, XL = B["P4"], B["PA"], B["PB"], B["XL"]
        nt = ntok
        tsl = slice(s * 128, s * 128 + nt)
        for gi in range(13):
            wg, kg = self.wload(self.wa_d[gi], 16, 256)
            b = self.bank(0, 6)
            for m in range(2):
                for kc in range(16):
                    P.pe(MM(self.psf[:, b, m * 128:m * 128 + nt], wg[:, kc, m * 128:(m + 1) * 128], hT[:, kc, tsl], kc == 0, kc == 15),
                         r=[kg, ("hT", kc)], w=[("pf", b)])
            c0 = 2 * gi
            if prev is None:
                P.vec(CP(P4[:, :, 0:1], self.carry[:, c0:c0 + 2].rearrange("p (a b) -> p a b", b=1)), r=["carry"], w=["bP4"])
                P.act(ACP(P4[:, :, 1:1 + nt], self.psf[:, b, 0:256].rearrange("p (a b) -> p a b", b=128)[:, :, 0:nt]), r=[("pf", b)], w=["bP4"])
                pprev = P4[:, :, 0:nt]
                pcur = P4[:, :, 1:1 + nt]
                P.vec(CP(self.carry[:, c0:c0 + 2].rearrange("p (a b) -> p a b", b=1), P4[:, :, nt:nt + 1]), r=["bP4"], w=["carry"])
                pk = ["bP4"]
            else:
                P.act(ACP(P4[:, :, 0:nt], self.psf[:, b, 0:256].rearrange("p (a b) -> p a b", b=128)[:, :, 0:nt]), r=[("pf", b)], w=["bP4"])
                pprev = prev[:, c0:c0 + 2, 0:nt]
                pcur = P4[:, :, 0:nt]
                P.vec(CP(self.pnew[:, c0:c0 + 2, 0:nt], pcur), r=["bP4"], w=["pnew"])
                pk = ["bP4", "prev"]
            P.vec(TT(PA[:, :, 0:nt], pprev, self.bc("mu", c0, 2, nt), ALU.mult), r=pk + ["vecs"], w=["bPA"])
            P.vec(TT(PB[:, :, 0:nt], pcur, self.omm[:, c0:c0 + 2].rearrange("p (a b) -> p a b", b=1).to_broadcast([128, 2, nt]), ALU.mult),
                  r=pk + ["omm"], w=["bPB"])
            if gi < 4:
                dst, dk = R[:, c0:c0 + 2, 0:nt], "bR"
            elif gi < 8:
                dst, dk = K[:, c0 - 8:c0 - 6, 0:nt], "bK"
            elif gi < 12:
                dst, dk = V[:, c0 - 16:c0 - 14, 0:nt], "bV"
            else:
                dst, dk = XL[:, :, 0:nt], "bXL"
            P.vec(TT(dst, PA[:, :, 0:nt], PB[:, :, 0:nt], ALU.add), r=["bPA", "bPB"], w=[dk])
        TXW, SXG = B["TXW"], B["SXG"]
        P.act(ACTF(TXW[0:64, 0:nt], XL[0:64, 0, 0:nt], AF.Tanh), r=["bXL"], w=["bTXW"])
        P.vec(CP(TXW[64:128, 0:nt], XL[64:128, 0, 0:nt]), r=["bXL"], w=["bTXW"])
        P.act(ACTF(SXG[:, 0:nt], XL[:, 1, 0:nt], AF.Sigmoid), r=["bXL"], w=["bSXG"])
        WD = A
        for what in ("w", "a", "g"):
            for half in range(2):
                b = self.bank(0, 6)
                for q in range(4):
                    hp = half * 4 + q
                    cs = slice(hp * 128, (hp + 1) * 128)
                    if what == "w":
                        P.pe(MM(self.psf[:, b, q * 128:q * 128 + nt], self.w2a2[0:64, cs], TXW[0:64, 0:nt]), r=["w2a2", "bTXW"], w=[("pf", b)])
                    elif what == "a":
                        P.pe(MM(self.psf[:, b, q * 128:q * 128 + nt], self.w2a2[64:128, cs], TXW[64:128, 0:nt]), r=["w2a2", "bTXW"], w=[("pf", b)])
                    else:
                        P.pe(MM(self.psf[:, b, q * 128:q * 128 + nt], self.g2[:, cs], SXG[:, 0:nt]), r=["g2", "bSXG"], w=[("pf", b)])
                for q in range(4):
                    hp = half * 4 + q
                    src = self.psf[:, b, q * 128:q * 128 + nt]
                    if what == "w":
                        P.act(ACTF(T2[:, hp, 0:nt], src, AF.Sigmoid, bias=self.vv("w0", hp)), r=[("pf", b), "vecs"], w=["bT2"])
                    elif what == "a":
                        P.act(ACTF(A[:, hp, 0:nt], src, AF.Sigmoid, bias=self.vv("a0", hp)), r=[("pf", b), "vecs"], w=["bA"])
                    else:
                        P.act(ACP(T1[:, hp, 0:nt], src), r=[("pf", b)], w=["bT1"])
        G = T1
        P.vec(TT(KK[:, :, 0:nt], K[:, :, 0:nt], self.bc("kk", 0, 8, nt), ALU.mult), r=["bK", "vecs"], w=["bKK"])
        P.act(ACTF(BB[:, :, 0:nt], KK[:, :, 0:nt], AF.Square), r=["bKK"], w=["bBB"])
        for half in range(2):
            b = self.bank()
            hs = slice(half * 4, half * 4 + 4)
            P.pe(MM(self.psf[:, b, 0:4 * nt], self.cv("bones"), BB[:, hs, 0:nt]), r=["cst", "bBB"], w=[("pf", b)])
            P.act(ACTF(KM[:, hs, 0:nt], self.psf[:, b, 0:4 * nt].rearrange("p (a b) -> p a b", b=nt), AF.Sqrt), r=[("pf", b)], w=["bKM"])
        P.vec(TS(KM[:, :, 0:nt], KM[:, :, 0:nt], 1e-12, ALU.max), r=["bKM"], w=["bKM"])
        P.vec(lambda e: e.reciprocal(out=KM[:, :, 0:nt], in_=KM[:, :, 0:nt]), r=["bKM"], w=["bKM"])
        P.vec(TT(KK[:, :, 0:nt], KK[:, :, 0:nt], KM[:, :, 0:nt], ALU.mult), r=["bKK", "bKM"], w=["bKK"])
        P.vec(TT(BB[:, :, 0:nt], KK[:, :, 0:nt], A[:, :, 0:nt], ALU.mult), r=["bKK", "bA"], w=["bBB"])
        P.vec(STT(KM[:, :, 0:nt], A[:, :, 0:nt], -1.0, self.bc("ka", 0, 8, nt), ALU.add, ALU.mult), r=["bA", "vecs"], w=["bKM"])
        P.vec(STT(KM[:, :, 0:nt], KM[:, :, 0:nt], 1.0, K[:, :, 0:nt], ALU.add, ALU.mult), r=["bKM", "bK"], w=["bKM"])
        if chunked:
            P.vec(TS(A[:], T2[:], -math.exp(-0.5), ALU.mult), r=["bT2", "bA"], w=["bA"])
        else:
            P.act(ACTF(WD[:, :, 0:nt], T2[:, :, 0:nt], AF.Exp, scale=-math.exp(-0.5)), r=["bT2", "bA"], w=["bA"])
        BON = K
        P.vec(TT(T2[:, :, 0:nt], R[:, :, 0:nt], KM[:, :, 0:nt], ALU.mult), r=["bR", "bKM"], w=["bT2"])
        P.vec(TT(T2[:, :, 0:nt], T2[:, :, 0:nt], self.bc("rk", 0, 8, nt), ALU.mult), r=["bT2", "vecs"], w=["bT2"])
        for half in range(2):
            b = self.bank()
            hs = slice(half * 4, half * 4 + 4)
            P.pe(MM(self.psf[:, b, 0:4 * nt], self.cv("bones"), T2[:, hs, 0:nt]), r=["cst", "bT2"], w=[("pf", b)])
            P.vec(TT(BON[:, hs, 0:nt], self.psf[:, b, 0:4 * nt].rearrange("p (a b) -> p a b", b=nt), V[:, hs, 0:nt], ALU.mult),
                  r=[("pf", b), "bV", "bKM"], w=["bK"])
        Vb, VT = B["Vb"], B["VT"]
        P.act(ACP(Vb[:, :, 0:nt], V[:, :, 0:nt]), r=["bV"], w=["bVb"])
        for hp in range(8):
            pb = ("pb", 0)
            P.pe(TR(self.psb[0:nt, 0, hp * 128:(hp + 1) * 128], Vb[:, hp, 0:nt], self.identb[:]), r=["bVb", "identb"], w=[pb])
        P.vec(CP(VT[0:nt, :, :], self.psb[0:nt, 0, :].rearrange("p (a b) -> p a b", b=128)), r=[("pb", 0) for hp in range(8)], w=["bVT"])
        v3 = lambda ap: ap.rearrange("p (a b) -> p a b", b=64)
        if chunked:
            self.rwkv_chunk_core(B)
        else:
            S1, S2, S3, S4, S5 = (B[n] for n in ("S1", "S2", "S3", "S4", "S5"))
        for t in range(0 if chunked else nt):
            if Tstates is None:
                Tst, tk = self.Tst, "Tst"
            else:
                Tst, tk = Tstates(t)
            col = lambda X: X[:, :, t:t + 1].to_broadcast([128, 8, 64])
            P.vec(TT(v3(S1[:]), Tst[:], col(KK), ALU.mult), r=[tk, "bKK"], w=["bS1"])
            P.pe(MM(self.psf[:, 4, :], self.cv("bones"), S1[:]), r=["cst", "bS1"], w=[("pf", 4)])
            P.pool(TT(v3(S3[:]), Tst[:], col(WD), ALU.mult), r=[tk, "bA"], w=["bS3"])
            P.pe(MM(self.psf[0:64, 5, :], self.identb[0:nt, t:t + 1].to_broadcast([nt, 64]), VT[0:nt, :, 0:64]), r=["identb", "bVT"], w=[("pf", 5)])
            P.pe(MM(self.psf[64:128, 5, :], self.identb[0:nt, t:t + 1].to_broadcast([nt, 64]), VT[0:nt, :, 64:128]), r=["identb", "bVT"], w=[("pf", 5)])
            P.vec(TT(v3(S4[:]), v3(self.psf[:, 5, :]), col(KM), ALU.mult), r=[("pf", 5), "bKM"], w=["bS4"])
            P.vec(TT(v3(S2[:]), v3(self.psf[:, 4, :]), col(BB), ALU.mult), r=[("pf", 4), "bBB"], w=["bS2"])
            P.vec(TT(S3[:], S3[:], S2[:], ALU.subtract), r=["bS3", "bS2"], w=["bS3"])
            P.vec(TT(Tst[:], v3(S3[:]), v3(S4[:]), ALU.add), r=["bS3", "bS4"], w=[tk])
            P.pool(TT(v3(S5[:]), Tst[:], col(R), ALU.mult), r=[tk, "bR"], w=["bS5"])
            o_, _ = COFF["iota"]
            P.pe(MM(self.psf[0:nt, 2, :], self.csel[0:64, 127 - t:127 - t + nt], S5[0:64, :], t == 0, t == nt - 1), r=["csel", "bS5"], w=[("pf", 2)])
            P.pe(MM(self.psf[0:nt, 3, :], self.csel[64:128, 127 - t:127 - t + nt], S5[64:128, :], t == 0, t == nt - 1), r=["csel", "bS5"], w=[("pf", 3)])
            if step_post is not None:
                step_post(t, Tst, tk)
        YN = KK
        YB, ST = B["YB"], B["ST"]
        yv = YN[0:nt, :, :].rearrange("p a (c d) -> p a c d", d=64)
        if chunked:
            P.act(ACP(YN[:, 0:4, :], self.psf[:, 4, :].rearrange("p (a b) -> p a b", b=128)), r=[("pf", 4)], w=["bKK"])
            P.vec(CP(YN[:, 4:8, :], self.psf[:, 5, :].rearrange("p (a b) -> p a b", b=128)), r=[("pf", 5)], w=["bKK"])
        else:
            P.act(ACP(yv[:, :, 0, :], v3(self.psf[0:nt, 2, :])), r=[("pf", 2)], w=["bKK"])
            P.vec(CP(yv[:, :, 1, :], v3(self.psf[0:nt, 3, :])), r=[("pf", 3)], w=["bKK"])
        y16 = YN[0:nt, :, :].rearrange("p a (c d) -> p (a c) d", d=64)
        q16 = BB[0:nt, :, :].rearrange("p a (c d) -> p (a c) d", d=64)
        P.vec(lambda e: e.tensor_reduce(out=ST[0:nt, 0:16], in_=y16, axis=AX.X, op=ALU.add), r=["bKK"], w=["bST0"])
        P.vec(TS(ST[0:nt, 0:16], ST[0:nt, 0:16], -1.0 / 64, ALU.mult), r=["bST0"], w=["bST0"])
        P.vec(TT(y16, y16, ST[0:nt, 0:16].rearrange("p (a b) -> p a b", b=1).to_broadcast([nt, 16, 64]), ALU.add), r=["bKK", "bST0"], w=["bKK"])
        P.act(ACTF(q16, y16, AF.Square), r=["bKK"], w=["bBB"])
        P.vec(lambda e: e.tensor_reduce(out=ST[0:nt, 16:32], in_=q16, axis=AX.X, op=ALU.add), r=["bBB"], w=["bST1"])
        P.act(ACTF(ST[0:nt, 32:48], ST[0:nt, 16:32], AF.Ln, scale=1.0 / 64, bias=self.epsc[0:nt, 1:2]), r=["bST1", "epsc"], w=["bST2"])
        P.act(ACTF(ST[0:nt, 48:64], ST[0:nt, 32:48], AF.Exp, scale=-0.5), r=["bST2"], w=["bST3"])
        P.vec(TT(YB[0:nt, :, :].rearrange("p a (c d) -> p (a c) d", d=64), y16,
                 ST[0:nt, 48:64].rearrange("p (a b) -> p a b", b=1).to_broadcast([nt, 16, 64]), ALU.mult), r=["bKK", "bST3"], w=["bYB"])
        for hp in range(8):
            pb = ("pb", 1)
            P.pe(TR(self.psb[:, 1, hp * 128:hp * 128 + nt], YB[0:nt, hp, :], self.identb[0:nt, 0:nt]), r=["bYB", "identb"], w=[pb])
            P.vec(TS(T2[:, hp, 0:nt], self.psb[:, 1, hp * 128:hp * 128 + nt], self.vv("lnw", hp), ALU.mult, self.vv("lnb", hp), ALU.add),
                  r=[pb, "vecs"], w=["bT2"])
        P.vec(TT(T2[:, :, 0:nt], T2[:, :, 0:nt], BON[:, :, 0:nt], ALU.add), r=["bT2", "bK"], w=["bT2"])
        P.vec(TT(oFM[:, 0:8, tsl], T2[:, :, 0:nt], G[:, :, 0:nt], ALU.mult), r=["bT2", "bT1"], w=[("oFM", k) for k in range(8)])

    def sincos(self, S, C, X, Rt, Ft, shape_key, n, pool=False):
        P = self.P
        MAGIC = 12582912.0
        (P.pool if pool else P.vec)(TS(Rt, X, MAGIC, ALU.add), r=[shape_key + "X"], w=[shape_key + "R"])
        P.vec(STT(Ft, Rt, -MAGIC, X, ALU.add, ALU.subtract), r=[shape_key + "R", shape_key + "X"], w=[shape_key + "F"])
        P.act(ACTF(S, Ft, AF.Sin, scale=-2.0 * math.pi), r=[shape_key + "F"], w=[shape_key + "S"])
        P.vec(STT(Rt, Ft, -1.0, Ft, ALU.mult, ALU.max), r=[shape_key + "F"], w=[shape_key + "R"])
        P.act(ACTF(C, Rt, AF.Sin, scale=-2.0 * math.pi, bias=self.epsc[0:n, 2:3]), r=[shape_key + "R", "epsc"], w=[shape_key + "C"])

    def s5_setup(self):
        from contextlib import ExitStack
        P = self.P
        T = self.T
        self.LB = self.sb("s5_LB", [128, 2, 8, 128], BF16)
        self.LC = self.sb("s5_LC", [128, 2, 32, 64], BF16)
        self.LBz = self.sb("s5_LBz", [128, 2, 8, 2, 128], BF16)
        self.s5p = self.sb("s5_p", [128, 10, 32])
        self.G0 = self.sb("s5_G0", [128, 2, 32])
        self.GL = self.sb("s5_GL", [128, 2, 32])
        self.Hs5 = self.sb("s5_H", [128, 2, 32])
        self.Hs5T = self.sb("s5_HT", [32, 2, 128])
        with ExitStack() as st:
            a = self.sb("s5s_a", [128, 3, 32], F32, st)
            b = self.sb("s5s_b", [128, 2, 32, 16], F32, st)
            c = self.sb("s5s_c", [128, 2, 32 * 64], F32, st)
            t = self.sb("s5s_t", [128, 24, 32], F32, st)
            bb = self.sb("s5s_bb", [128, 2, 32, 16], F32, st)
            tb = self.sb("s5s_tb", [128, 2, 32, 16], F32, st)
            ZB = self.sb("s5s_ZB", [128, 2, 32, 2, 16], BF16, st)
            P.dma("sync", a[:], self.s5a_d[:, :, :], w=["s_a"])
            P.dma("sync", b[:].rearrange("p a g q -> p a (g q)"), self.s5b_d[:, :, :], w=["s_b"])
            P.dma("sync", c[:], self.s5c_d[:, :, :], w=["s_c"])
            P.vec(CP(self.LC[:].rearrange("p a g q -> p a (g q)"), c[:]), r=["s_c"], w=["LC"])
            lr, li = a[:, 0, :], a[:, 1, :]
            dt = t[:, 0, :]
            P.act(ACTF(dt, a[:, 2, :], AF.Exp), r=["s_a"], w=["s_dt"])
            th = self.s5p[:, 0, :]
            P.vec(STT(th, li, 1.0 / (2.0 * math.pi), dt, ALU.mult, ALU.mult), r=["s_a", "s_dt"], w=["s5p0", "thX"])
            P.vec(TT(t[:, 1, :], lr, dt, ALU.mult), r=["s_a", "s_dt"], w=["s_t1"])
            rho = self.s5p[:, 1, :]
            P.act(ACTF(rho, t[:, 1, :], AF.Exp), r=["s_t1"], w=["s5p1"])
            sn, cs = t[:, 2, :], t[:, 3, :]
            self.sincos(sn, cs, th, t[:, 4, :], t[:, 5, :], "th", 128)
            abr, abi = self.s5p[:, 8, :], self.s5p[:, 9, :]
            P.vec(TT(abr, rho, cs, ALU.mult), r=["s5p1", "thC"], w=["s_abr"])
            P.vec(TT(abi, rho, sn, ALU.mult), r=["s5p1", "thS"], w=["s_abi"])
            den = t[:, 8, :]
            P.vec(TT(den, lr, lr, ALU.mult), r=["s_a"], w=["s_den"])
            P.vec(TT(t[:, 9, :], li, li, ALU.mult), r=["s_a"], w=["s_t9"])
            P.vec(TT(den, den, t[:, 9, :], ALU.add), r=["s_den", "s_t9"], w=["s_den"])
            P.vec(lambda e: e.reciprocal(out=den, in_=den), r=["s_den"], w=["s_den"])
            am1 = t[:, 10, :]
            P.vec(TS(am1, abr, -1.0, ALU.add), r=["s_abr"], w=["s_am1"])
            fre, fim = t[:, 11, :], t[:, 12, :]
            P.vec(TT(fre, am1, lr, ALU.mult), r=["s_am1", "s_a"], w=["s_fre"])
            P.vec(TT(t[:, 13, :], abi, li, ALU.mult), r=["s_abi", "s_a"], w=["s_t13"])
            P.vec(TT(fre, fre, t[:, 13, :], ALU.add), r=["s_fre", "s_t13"], w=["s_fre"])
            P.vec(TT(fre, fre, den, ALU.mult), r=["s_fre", "s_den"], w=["s_fre"])
            P.vec(TT(fim, abi, lr, ALU.mult), r=["s_abi", "s_a"], w=["s_fim"])
            P.vec(TT(t[:, 14, :], am1, li, ALU.mult), r=["s_am1", "s_a"], w=["s_t14"])
            P.vec(TT(fim, fim, t[:, 14, :], ALU.subtract), r=["s_fim", "s_t14"], w=["s_fim"])
            P.vec(TT(fim, fim, den, ALU.mult), r=["s_fim", "s_den"], w=["s_fim"])
            fb = lambda x: x.rearrange("p (g q) -> p g q", q=1).to_broadcast([128, 32, 16])
            P.vec(TT(bb[:, 0], b[:, 0], fb(fre), ALU.mult), r=["s_b", "s_fre"], w=["s_bb0"])
            P.vec(TT(tb[:, 0], b[:, 1], fb(fim), ALU.mult), r=["s_b", "s_fim"], w=["s_tb0"])
            P.vec(TT(bb[:, 0], bb[:, 0], tb[:, 0], ALU.subtract), r=["s_bb0", "s_tb0"], w=["s_bb0"])
            P.vec(TT(bb[:, 1], b[:, 1], fb(fre), ALU.mult), r=["s_b", "s_fre"], w=["s_bb1"])
            P.vec(TT(tb[:, 1], b[:, 0], fb(fim), ALU.mult), r=["s_b", "s_fim"], w=["s_tb1"])
            P.vec(TT(bb[:, 1], bb[:, 1], tb[:, 1], ALU.add), r=["s_bb1", "s_tb1"], w=["s_bb1"])
            P.vec(MS(ZB[:], 0.0), w=["s_ZB"])
            for ri in range(2):
                P.vec(CP(ZB[0:64, ri, :, 0, :], bb[0:64, ri]), r=["s_bb%d" % ri], w=["s_ZB"])
                P.vec(CP(ZB[64:128, ri, :, 1, :], bb[64:128, ri]), r=["s_bb%d" % ri], w=["s_ZB"])
            for ri in range(2):
                for ch in range(8):
                    pb = ("pb", ri)
                    P.pe(TR(self.psb[:, ri, ch * 128:(ch + 1) * 128], ZB[:, ri, 4 * ch:4 * ch + 4, :, :].rearrange("p g a q -> p (g a q)"),
                            self.identb[:]), r=["s_ZB", "identb"], w=[pb])
                P.vec(CP(self.LB[:, ri, :, :], self.psb[:, ri, :].rearrange("p (a b) -> p a b", b=128)),
                      r=[("pb", ri) for ch in range(8)], w=["LB"])
                for ql in range(2):
                    mo, _ = COFF["m%d" % ql]
                    P.vec(TS(self.LBz[:, ri, :, ql, :], self.LB[:, ri, :, :], self.cst[:, mo:mo + 1], ALU.mult), r=["LB", "cst"], w=["LBz"])
            for k, mult, base in ((2, float(T), 15), (4, float(T - 1), 18)):
                X = t[:, base, :]
                kk_ = "ec%d" % k
                P.vec(TS(X, th, mult, ALU.mult), r=["s5p0"], w=[kk_ + "X"])
                self.sincos(self.s5p[:, k + 1, :], self.s5p[:, k, :], X, t[:, base + 1, :], t[:, base + 2, :], kk_, 128)
            P.vec(MS(self.G0[:], 0.0), w=["G0"])
            P.barrier()

    def s5_tile(self, st, oFM, last_tile, sample=None):
        P = self.P
        T = self.T if sample is None else 16
        import os
        if int(os.environ.get("S5STOP", "99")) <= 1:
            return
        f = lambda nm: self.sb("v_" + nm, [128, T], F32, st)
        Ub = self.sb("v_Ub", [128, 8, T], BF16, st)
        DU = self.sb("v_DU", [128, 8, T], F32, st)
        YG = self.sb("v_YG", [128, 8, T], BF16, st)
        Y, Y2 = f("Y"), f("Y2")
        TNAMES = ("X", "R", "F", "SIN", "COS", "A1", "A2", "WR", "WI", "GR", "GI", "A3", "A4", "A5", "A6")
        TSET = [{n: f(n + str(i)) for n in TNAMES} for i in range(2)]
        HRS = [self.sb("v_HR%d" % i, [128, T], BF16, st) for i in range(2)]
        HIS = [self.sb("v_HI%d" % i, [128, T], BF16, st) for i in range(2)]
        for gi in range(13, 17):
            wg, kg = self.wload(self.wa_d[gi], 16, 256)
            for m in range(2):
                ch = 2 * (gi - 13) + m
                b = self.bank()
                for kc in range(16):
                    P.pe(MM(self.psf[:, b, 0:T], wg[:, kc, m * 128:(m + 1) * 128], self.hT[:, kc, 0:T], kc == 0, kc == 15),
                         r=[kg, ("hT", kc)], w=[("pf", b)])
                P.act(ACP(Ub[:, ch, :], self.psf[:, b, 0:T]), r=[("pf", b)], w=[("Ub", ch)])
                P.vec(TS(DU[:, ch, :], self.psf[:, b, 0:T], self.vv("s5d", ch), ALU.mult), r=[("pf", b), "vecs"], w=[("DU", ch)])
        io, _ = COFF["iota"]
        import os
        S5STOP = int(os.environ.get("S5STOP", "99"))
        if S5STOP <= 2:
            return
        if sample is not None:
            self.s5_sample_step(st, Ub, sample)
        for ch in range(8 if S5STOP > 5 else 1):
            by = 4 + (ch % 2)
            def gp_body(q, ch=ch, by=by):
                    if sample is not None:
                        gp = 4 * ch + q
                        hf, ql = q // 2, q % 2
                        ps = slice(64 * hf, 64 * hf + 64)
                        P.pe(MM(self.psf[ps, by, 0:T], self.LC[:, 0, gp, :], self.sHR[:, gp, :], ql == 0, False), r=["LC", "sHR"], w=[("pf", by)])
                        yield
                        P.pe(MM(self.psf[ps, by, 0:T], self.LC[:, 1, gp, :], self.sHI[:, gp, :], False, ql == 1), r=["LC", "sHI"], w=[("pf", by)])
                        yield
                        return
                    gp = 4 * ch + q
                    hf, ql = q // 2, q % 2
                    ps = slice(64 * hf, 64 * hf + 64)
                    pz = gp % 2
                    X, Rt, Ft, SIN, COS, A1, A2, WR, WI, GR, GI, A3, A4, A5, A6 = (TSET[pz][n] for n in TNAMES)
                    HR, HI = HRS[pz], HIS[pz]
                    K_ = lambda nm: nm + str(pz)
                    br, bi = self.bank(), self.bank()
                    P.pe(MM(self.psf[:, br, 0:T], self.LBz[ps, 0, ch, ql, :], Ub[ps, ch, :]), r=["LBz", ("Ub", ch)], w=[("pf", br)])
                    yield
                    P.pe(MM(self.psf[:, bi, 0:T], self.LBz[ps, 1, ch, ql, :], Ub[ps, ch, :]), r=["LBz", ("Ub", ch)], w=[("pf", bi)])
                    yield
                    P.vec(TS(X[:], self.cst[:, io:io + T], self.s5p[:, 0, gp:gp + 1], ALU.mult), r=["cst", "s5p0"], w=[K_("tb") + "X"])
                    yield
                    self.sincos(SIN[:], COS[:], X[:], Rt[:], Ft[:], K_("tb"), 128)
                    yield
                    Br, Bi = self.psf[:, br, 0:T], self.psf[:, bi, 0:T]
                    P.vec(TT(A1[:], Br, COS[:], ALU.mult), r=[("pf", br), K_("tb") + "C"], w=[K_("A1")])
                    yield
                    P.vec(TT(A2[:], Bi, SIN[:], ALU.mult), r=[("pf", bi), K_("tb") + "S"], w=[K_("A2")])
                    yield
                    P.vec(TT(WR[:], A1[:], A2[:], ALU.add), r=[K_("A1"), K_("A2")], w=[K_("WR")])
                    yield
                    P.vec(TT(A1[:], Bi, COS[:], ALU.mult), r=[("pf", bi), K_("tb") + "C"], w=[K_("A1")])
                    yield
                    P.vec(TT(A2[:], Br, SIN[:], ALU.mult), r=[("pf", br), K_("tb") + "S"], w=[K_("A2")])
                    yield
                    P.vec(TT(WI[:], A1[:], A2[:], ALU.subtract), r=[K_("A1"), K_("A2")], w=[K_("WI")])
                    yield
                    rho_bc = self.s5p[:, 1, gp:gp + 1].to_broadcast([128, T])
                    P.vec(lambda e, GR=GR, WR=WR, rho_bc=rho_bc, gp=gp: e.tensor_tensor_scan(out=GR[:], data0=rho_bc, data1=WR[:],
                          initial=self.G0[:, 0, gp:gp + 1], op0=ALU.mult, op1=ALU.add), r=[K_("WR"), "s5p1", "G0"], w=[K_("GR")])
                    P.vec(lambda e, GI=GI, WI=WI, rho_bc=rho_bc, gp=gp: e.tensor_tensor_scan(out=GI[:], data0=rho_bc, data1=WI[:],
                          initial=self.G0[:, 1, gp:gp + 1], op0=ALU.mult, op1=ALU.add), r=[K_("WI"), "s5p1", "G0"], w=[K_("GI")])
                    P.act(ACP(self.GL[:, 0, gp:gp + 1], GR[:, T - 1:T]), r=[K_("GR")], w=["GL"])
                    yield
                    P.act(ACP(self.GL[:, 1, gp:gp + 1], GI[:, T - 1:T]), r=[K_("GI")], w=["GL"])
                    yield
                    P.vec(TT(A3[:], GR[:], COS[:], ALU.mult), r=[K_("GR"), K_("tb") + "C"], w=[K_("A3")])
                    yield
                    P.vec(TT(A4[:], GI[:], SIN[:], ALU.mult), r=[K_("GI"), K_("tb") + "S"], w=[K_("A4")])
                    yield
                    P.vec(TT(A5[:], GR[:], SIN[:], ALU.mult), r=[K_("GR"), K_("tb") + "S"], w=[K_("A5")])
                    yield
                    P.vec(TT(A6[:], GI[:], COS[:], ALU.mult), r=[K_("GI"), K_("tb") + "C"], w=[K_("A6")])
                    yield
                    P.vec(TT(HR[:], A3[:], A4[:], ALU.subtract), r=[K_("A3"), K_("A4")], w=[K_("HR")])
                    yield
                    P.vec(STT(HI[:], A5[:], -1.0, A6[:], ALU.mult, ALU.subtract), r=[K_("A5"), K_("A6")], w=[K_("HI")])
                    yield
                    P.pe(MM(self.psf[ps, by, 0:T], self.LC[:, 0, gp, :], HR[:], ql == 0, False), r=["LC", K_("HR")], w=[("pf", by)])
                    yield
                    P.pe(MM(self.psf[ps, by, 0:T], self.LC[:, 1, gp, :], HI[:], False, ql == 1), r=["LC", K_("HI")], w=[("pf", by)])
                    yield
            if sample is not None:
                for q in range(4):
                    for _ in gp_body(q):
                        pass
            else:
                for pair in ((0, 1), (2, 3)):
                    alive = [gp_body(q) for q in pair]
                    while alive:
                        for g_ in list(alive):
                            try:
                                next(g_)
                            except StopIteration:
                                alive.remove(g_)
            P.vec(TT(Y[:], self.psf[:, by, 0:T], DU[:, ch, :], ALU.add), r=[("pf", by), ("DU", ch)], w=["Y"])
            P.act(ACTF(Y2[:], Y[:], AF.Square), r=["Y"], w=["Y2"])
            P.vec(STT(Y2[:], Y2[:], 0.044715, Y[:], ALU.mult, ALU.mult), r=["Y2", "Y"], w=["Y2"])
            P.vec(TT(Y2[:], Y2[:], Y[:], ALU.add), r=["Y2", "Y"], w=["Y2"])
            P.act(ACTF(Y2[:], Y2[:], AF.Sigmoid, scale=1.5957691216057308), r=["Y2"], w=["Y2"])
            P.vec(TT(YG[:, ch, :], Y[:], Y2[:], ALU.mult), r=["Y", "Y2"], w=[("YG", ch)])
        if S5STOP <= 6:
            return
        for gw in range(2):
            wgl, kgl = self.wload(self.wglu_d[gw], 8, 512)
            for m4 in range(4):
                m = 4 * gw + m4
                b = self.bank()
                for kc in range(8):
                    P.pe(MM(self.psf[:, b, 0:T], wgl[:, kc, m4 * 128:(m4 + 1) * 128], YG[:, kc, :], kc == 0, kc == 7),
                         r=[kgl, ("YG", kc)], w=[("pf", b)])
                P.act(ACTF(Y[:], self.psf[:, b, 0:T], AF.Sigmoid, bias=self.vv("bglu", m)), r=[("pf", b), "vecs"], w=["Y"])
                P.vec(TT(oFM[:, 8 + m, 0:T], YG[:, m, :], Y[:], ALU.mult), r=[("YG", m), "Y"], w=[("oFM", 8 + m)])
        if sample is None:
            self.s5_rot_state(2, self.G0, "G0")
            if last_tile:
                self.s5_state_out(self.o_ps5)

    def s5_rot_state(self, k, dst, dkey):
        P = self.P
        c, s = self.s5p[:, k, :], self.s5p[:, k + 1, :]
        t0, t1 = self.s5p[:, 6, :], self.s5p[:, 7, :]
        glr, gli = self.GL[:, 0, :], self.GL[:, 1, :]
        P.vec(TT(t0, c, glr, ALU.mult), r=["GL"], w=["s5t0"])
        P.vec(TT(t1, s, gli, ALU.mult), r=["GL"], w=["s5t1"])
        P.vec(TT(dst[:, 0, :], t0, t1, ALU.subtract), r=["s5t0", "s5t1"], w=[dkey])
        P.vec(TT(t0, s, glr, ALU.mult), r=["GL", dkey], w=["s5t0"])
        P.vec(TT(t1, c, gli, ALU.mult), r=["GL", dkey], w=["s5t1"])
        P.vec(TT(dst[:, 1, :], t0, t1, ALU.add), r=["s5t0", "s5t1"], w=[dkey])

    def s5_state_out(self, out_ap):
        P = self.P
        self.s5_rot_state(4, self.Hs5, "Hs5")
        for ri in range(2):
            b = self.bank()
            P.pe(TR(self.psf[0:32, b, 0:128], self.Hs5[:, ri, :], self.cv("ident")), r=["Hs5", "cst"], w=[("pf", b)])
            P.act(ACP(self.Hs5T[0:32, ri, :], self.psf[0:32, b, 0:128]), r=[("pf", b)], w=["Hs5T"])
            P.dma("gpsimd", out_ap[ri], self.Hs5T[0:32, ri, :], r=["Hs5T"], w=[("ps5o", ri)])

    def build(self):
        from contextlib import ExitStack
        P = self.P
        nc = self.nc
        T, NSUB = self.T, self.NSUB
        self.declare()
        self.alloc()
        self.carry = self.sb("carry", [128, 26])
        self.Tst = self.sb("Tst", [128, 8, 64])
        self.Tb = self.sb("Tb", [128, 8, 64], BF16)
        P.vec(MS(self.Tb[:], 0.0), w=["Tb"])
        self.csel = self.sb("csel", [128, 255], BF16)
        self.epsc = self.sb("epsc", [128, 4])
        self.ffn_act = self.sb("ffn_act", [128, 2, T], BF16)
        self.ffn_sil = self.sb("ffn_sil", [128, T])
        self.setup()
        P.vec(MS(self.carry[:], 0.0), w=["carry"])
        P.vec(MS(self.Tst[:], 0.0), w=["Tst"])
        P.vec(MS(self.csel[:], 0.0), w=["csel"])
        P.vec(MS(self.csel[:, 127:128], 1.0), w=["csel"])
        P.vec(MS(self.epsc[:, 0:1], EPS), w=["epsc"])
        P.vec(MS(self.epsc[:, 1:2], GN_EPS), w=["epsc"])
        P.vec(MS(self.epsc[:, 2:3], math.pi / 2.0), w=["epsc"])
        P.vec(MS(self.epsc[:, 3:4], 1.0), w=["epsc"])
        self.cself = self.sb("cself", [128, 31])
        P.vec(MS(self.cself[:], 0.0), w=["cself"])
        P.vec(MS(self.cself[:, 15:16], 1.0), w=["cself"])
        if self.on("s5"):
            self.s5_setup()
        P.barrier()
        for ti in range(self.NTILE):
            first, last = ti == 0, ti == self.NTILE - 1
            for s in range(NSUB):
                r0 = (ti * NSUB + s) * 128
                P.dma("sync", self.x[:, s, :], self.xp[r0:r0 + 128, :], w=[("x", s)])
            P.dma("sync", self.rot[:], self.rot_d[:, :, ti * T:(ti + 1) * T], w=["rot"])
            if self.on("rwkv") or self.on("s5"):
                self.norm_to_hT("nm0")
                P.barrier()
                with ExitStack() as st:
                    oFM = self.sb("oFM", [128, 16, T], BF16, st)
                    if not (self.on("rwkv") and self.on("s5")):
                        P.vec(MS(oFM[:], 0.0), w=[("oFM", k) for k in range(16)])
                    if self.on("rwkv"):
                        with ExitStack() as st2:
                            B = self.rwkv_alloc(st2, chunked=CHUNKED)
                            for s in range(NSUB):
                                self.rwkv_subtile(B, s, oFM, chunked=CHUNKED)
                            if last:
                                self.rwkv_state_out(B)
                            P.barrier()
                    if self.on("s5"):
                        with ExitStack() as st2:
                            self.s5_tile(st2, oFM, last)
                            P.barrier()
                    self.outproj_a(oFM)
                    P.barrier()
            if self.on("norm"):
                self.norm_to_hT("nf0")
                self.dump("hT", self.hT[:].rearrange("p a b -> p (a b)"), [128, 16 * T], [("hT", k) for k in range(16)], BF16)
            if self.on("ffn0"):
                self.norm_to_hT("nf0")
                self.ffn(0)
            if self.on("ret"):
                self.norm_to_hT("nm1")
                self.layer1(first, last)
            if self.on("ffn1"):
                self.norm_to_hT("nf1")
                self.ffn(1)
            if self.on("final"):
                self.final_norm(lambda s: self.yp[(ti * NSUB + s) * 128:(ti * NSUB + s + 1) * 128, :])
            else:
                for s in range(NSUB):
                    r0 = (ti * NSUB + s) * 128
                    P.dma("gpsimd", self.yp[r0:r0 + 128, :], self.x[:, s, :], r=[("x", s)], w=["yout"])
            P.barrier()
        if self.with_sample:
            self.sample_phase()
        P.barrier()
        with nc.Block() as block:
            @block.tensor
            def _(e):
                P.replay("tensor", e)

            @block.vector
            def _(e):
                P.replay("vector", e)

            @block.scalar
            def _(e):
                P.replay("scalar", e)

            @block.gpsimd
            def _(e):
                P.replay("gpsimd", e)

            @block.sync
            def _(e):
                P.replay("sync", e)
        return nc

    def sample_phase(self):
        from contextlib import ExitStack
        P = self.P
        smp = self.smp
        P.barrier()
        P.dma("sync", self.x[0:16, 0, :], self.xs_d[:, :], w=[("x", 0)])
        if self.on("rwkv") or self.on("s5"):
            self.norm_to_hT("nm0", ntok=16, nsub=1)
            P.barrier()
            with ExitStack() as st:
                oFM = self.sb("oFMs", [128, 16, 16], BF16, st)
                if not (self.on("rwkv") and self.on("s5")):
                    P.vec(MS(oFM[:], 0.0), w=[("oFM", k) for k in range(16)])
                if self.on("rwkv"):
                    with ExitStack() as st2:
                        self.rwkv_sample(st2, oFM, smp)
                        P.barrier()
                if self.on("s5"):
                    with ExitStack() as st2:
                        self.s5_tile(st2, oFM, False, sample=smp)
                        P.barrier()
                self.outproj_a(oFM, ntok=16, nsub=1)
                P.barrier()
        if self.on("ffn0"):
            self.norm_to_hT("nf0", ntok=16, nsub=1)
            self.ffn(0, ntok=16, nsub=1)
        if self.on("ret"):
            self.norm_to_hT("nm1", ntok=16, nsub=1)
            self.layer1_sample(smp)
        if self.on("ffn1"):
            self.norm_to_hT("nf1", ntok=16, nsub=1)
            self.ffn(1, ntok=16, nsub=1)
        if self.on("final"):
            self.final_norm(lambda s: self.ys[:, :], ntok=16, nsub=1)
        else:
            P.dma("gpsimd", self.ys[:, :], self.x[0:16, 0, :], r=[("x", 0)], w=["ysout"])
        P.barrier()

    def rwkv_state_out(self, B):
        P = self.P
        S1 = B["R"][:].rearrange("p a b -> p (a b)")
        for hp in range(8):
            b = self.bank()
            P.pe(TR(self.psf[0:64, b, 0:128], self.Tst[:, hp, :], self.cv("ident")), r=["Tst", "cst"], w=[("pf", b)])
            P.act(ACP(S1[0:64, 0:128], self.psf[0:64, b, 0:128]), r=[("pf", b)], w=["bS1"])
            P.dma("gpsimd", self.o_prwkv[2 * hp:2 * hp + 2].rearrange("h i j -> i h j"),
                  S1[0:64, 0:128].rearrange("p (h j) -> p h j", j=64), r=["bS1"], w=[("prwkv", hp)])
        b = self.bank()
        P.pe(TR(self.psf[0:26, b, 0:128], self.carry[:, 0:26], self.cv("ident")), r=["carry", "cst"], w=[("pf", b)])
        P.act(ACP(S1[0:26, 128:256], self.psf[0:26, b, 0:128]), r=[("pf", b)], w=["bS1"])
        P.dma("gpsimd", self.o_pshift[:, :], S1[0:26, 128:256], r=["bS1"], w=["pshift"])


def shared_maps(inp, seq):
    m = {}
    cc = make_consts()
    m["consts"] = np.ascontiguousarray(np.concatenate([cc[n].reshape(128, -1) for n, _ in CONST_LAYOUT], axis=1).astype(np.float32))
    vec = {"nm0": inp["norm_mix"][0], "nf0": inp["norm_ffn"][0], "nm1": inp["norm_mix"][1], "nf1": inp["norm_ffn"][1],
           "mu": inp["mu_shift"][0], "w0": inp["rwkv_w0"][0], "a0": inp["rwkv_a0"][0], "kk": inp["rwkv_k_k"][0],
           "ka": inp["rwkv_k_a"][0], "rk": inp["rwkv_r_k"][0], "lnw": inp["rwkv_ln_w"][0], "lnb": inp["rwkv_ln_b"][0],
           "s5d": inp["s5_d"][0], "bglu": inp["s5_b_glu"][0]}
    m["vecs"] = np.ascontiguousarray(np.concatenate([fm(vec[n]) for n, _ in VEC_LAYOUT], axis=1))
    m["rot"] = rot_tables(np.concatenate([np.arange(seq, dtype=np.float32), np.array([PAST], np.float32)]))
    m["nfin"] = np.ascontiguousarray(inp["norm_final"].reshape(1, D))
    m["retgn"] = np.ascontiguousarray(inp["ret_gn"][0].reshape(RH, DV))
    m["w2a2"] = np.ascontiguousarray(np.concatenate([inp["rwkv_w2"][0], inp["rwkv_a2"][0]], axis=0))
    m["g2"] = np.ascontiguousarray(inp["rwkv_g2"][0])
    lay = lambda a: a.reshape(32, 2, 64).transpose(1, 2, 0).reshape(128, 32)
    ld = np.repeat(inp["s5_log_dt"][0].reshape(32, 2).T[:, None, :], 64, axis=1).reshape(128, 32)
    m["s5a"] = np.ascontiguousarray(np.stack([lay(inp["s5_a_re"][0]), lay(inp["s5_a_im"][0]), ld], axis=1))
    layb = lambda b: b.reshape(32, 2, 64, 16).transpose(1, 2, 0, 3).reshape(128, 32 * 16)
    m["s5b"] = np.ascontiguousarray(np.stack([layb(inp["s5_b_re"][0]), layb(inp["s5_b_im"][0])], axis=1))

    def layc(c):
        c4 = c.reshape(32, 2, 16, 64)
        Z = np.zeros((2, 64, 32, 2, 2, 16), np.float32)
        for gl in range(2):
            for gp in range(32):
                Z[gl, :, gp, gp % 2, gl, :] = c4[gp, gl].T
        return Z.reshape(128, 32 * 64)
    m["s5c"] = np.ascontiguousarray(np.stack([layc(inp["s5_c_re"][0]), layc(inp["s5_c_im"][0])], axis=1))
    m["wa"] = tile_w(inp["w_in_a"][0], 256)
    m["wglu"] = tile_w(inp["s5_w_glu"][0], 512)
    m["woa"] = tile_w(inp["w_out_a"][0], 256)
    for i in range(2):
        m["wg%d" % i] = tile_w(inp["ffn_w_gate"][i], 256)
        m["wu%d" % i] = tile_w(inp["ffn_w_up"][i], 256)
        m["wd%d" % i] = tile_rows(inp["ffn_w_down"][i], 2)
    wc = inp["w_in_c"][0]
    hs = []
    for h in range(RH):
        cols = [wc[:, h * 256:(h + 1) * 256], wc[:, D + h * 256:D + (h + 1) * 256]]
        for base in (2 * D, 2 * D + RH * DV):
            for vg in range(2):
                cols.append(wc[:, base + h * 512 + vg * 256:base + h * 512 + (vg + 1) * 256])
        hs.append(np.stack([tile_w(np.ascontiguousarray(c), 256)[0] for c in cols], axis=0))
    m["wc"] = np.ascontiguousarray(np.stack(hs, axis=0))
    m["woc"] = tile_rows(inp["w_out_c"][0], 2).reshape(RH, 2, 128, 2 * D)
    return m


def run(inp, seq, nsub, cores, dbg=(), stages=None, trace=False, with_sample=True):
    bld = Builder(seq, nsub, dbg=dbg, stages=stages, with_sample=with_sample)
    nc = bld.build()
    sh = shared_maps(inp, seq)
    in_maps = []
    for c in cores:
        m = dict(sh)
        m["xp"] = np.ascontiguousarray(inp["x_prompt"][c % 4, :seq])
        rs = slice(16 * c, 16 * c + 16)
        m["xs"] = np.ascontiguousarray(inp["x_sample"][rs, 0, :])
        m["st_rwkv"] = np.ascontiguousarray(inp["state_rwkv"][0, rs])
        m["st_shift"] = np.ascontiguousarray(inp["state_shift"][0, rs])
        m["st_s5"] = np.ascontiguousarray(np.stack([inp["state_s5_re"][0, rs].reshape(16, 4096), inp["state_s5_im"][0, rs].reshape(16, 4096)]))
        m["st_ret"] = np.ascontiguousarray(inp["state_ret"][0, rs])
        in_maps.append({k: v for k, v in m.items() if k in bld.dram_in})
    res = run_bass_kernel_spmd(nc, in_maps, core_ids=list(range(len(cores))), trace=trace)
    return res, bld


def kernel(**inp):
    inp = {k: np.asarray(v) for k, v in inp.items()}
    seq = inp["x_prompt"].shape[1]
    res, bld = run(inp, seq, 2, list(range(8)))
    R = res.results
    B = 4
    y_prompt = np.stack([R[b]["yp"] for b in range(B)])
    p_rwkv = np.stack([R[b]["p_rwkv"] for b in range(B)])[None]
    p_shift = np.stack([R[b]["p_shift"].reshape(PROJ) for b in range(B)])[None]
    p_s5_re = np.stack([R[b]["p_s5"][0].reshape(64, 64) for b in range(B)])[None]
    p_s5_im = np.stack([R[b]["p_s5"][1].reshape(64, 64) for b in range(B)])[None]
    p_ret = np.stack([R[b]["p_ret"].reshape(RH, DK, DV) for b in range(B)])[None]
    cat = lambda k: np.concatenate([R[c][k] for c in range(8)], axis=0)
    y_sample = cat("ys")[:, None, :]
    s_rwkv = cat("s_rwkv")[None]
    s_shift = cat("s_shift")[None]
    s_s5_re = np.concatenate([R[c]["s_s5"][0] for c in range(8)], axis=0).reshape(1, 128, 64, 64)
    s_s5_im = np.concatenate([R[c]["s_s5"][1] for c in range(8)], axis=0).reshape(1, 128, 64, 64)
    s_ret = cat("s_ret")[None]
    return (y_prompt, y_sample, p_rwkv, p_shift, p_s5_re, p_s5_im, p_ret,
            s_rwkv, s_shift, s_s5_re, s_s5_im, s_ret)


def simulate_sync(P):
    val = {}
    pc = {e: 0 for e in ENGS}
    progress = True
    while progress:
        progress = False
        for e in ENGS:
            while pc[e] < len(P.ops[e]):
                waits, fn, inc = P.ops[e][pc[e]]
                if any(val.get(id(sem), 0) < v for sem, v in waits):
                    break
                if inc is not None:
                    val[id(inc[0])] = val.get(id(inc[0]), 0) + inc[1]
                pc[e] += 1
                progress = True
    stuck = {e: (pc[e], len(P.ops[e])) for e in ENGS if pc[e] < len(P.ops[e])}
    return stuck


def _s5_sample_step(self, st, Ub, smp):
    P = self.P
    H0 = self.sb("q_H0", [128, 2, 32, 16], F32, st)
    HN = self.sb("q_HN", [128, 2, 32, 16], F32, st)
    TA = self.sb("q_TA", [128, 32, 16], F32, st)
    TB = self.sb("q_TB", [128, 32, 16], F32, st)
    self.sHR = self.sb("q_sHR", [128, 32, 16], BF16, st)
    self.sHI = self.sb("q_sHI", [128, 32, 16], BF16, st)
    stg = self.sb("q_stg", [16, 2, 1024], F32, st)
    idf = self.cv("ident")
    n = 0
    for ri in range(2):
        for k in range(4):
            buf = stg[:, n % 2, :]
            sk = ("stg", n % 2)
            n += 1
            P.dma("sync", buf, smp["st_s5"][ri, :, k * 1024:(k + 1) * 1024], w=[sk])
            b = self.bank()
            for g8 in range(8):
                P.pe(TR(self.psf[:, b, g8 * 16:(g8 + 1) * 16], buf[:, g8 * 128:(g8 + 1) * 128], idf[0:16, 0:16]), r=[sk, "cst"], w=[("pf", b)])
            P.vec(CP(H0[:, ri, 8 * k:8 * k + 8, :], self.psf[:, b, 0:128].rearrange("p (a b) -> p a b", b=16)), r=[("pf", b)], w=[("H0", ri)])
    for gp in range(32):
        ch, q = gp // 4, gp % 4
        hf, ql = q // 2, q % 2
        ps = slice(64 * hf, 64 * hf + 64)
        for ri in range(2):
            P.pe(MM(self.psf[:, ri, gp * 16:(gp + 1) * 16], self.LBz[ps, ri, ch, ql, :], Ub[ps, ch, 0:16]), r=["LBz", ("Ub", ch)], w=[("pf", ri)])
    abc = lambda k: self.s5p[:, k, :].rearrange("p (g q) -> p g q", q=1).to_broadcast([128, 32, 16])
    pv = lambda ri: self.psf[:, ri, :].rearrange("p (a b) -> p a b", b=16)
    P.vec(TT(TA[:], H0[:, 0], abc(8), ALU.mult), r=[("H0", 0), "s5p"], w=["qTA"])
    P.vec(TT(TB[:], H0[:, 1], abc(9), ALU.mult), r=[("H0", 1), "s5p"], w=["qTB"])
    P.vec(TT(HN[:, 0], TA[:], TB[:], ALU.subtract), r=["qTA", "qTB"], w=[("HN", 0)])
    P.vec(TT(HN[:, 0], HN[:, 0], pv(0), ALU.add), r=[("HN", 0), ("pf", 0)], w=[("HN", 0)])
    P.vec(TT(TA[:], H0[:, 1], abc(8), ALU.mult), r=[("H0", 1), "s5p"], w=["qTA"])
    P.vec(TT(TB[:], H0[:, 0], abc(9), ALU.mult), r=[("H0", 0), "s5p"], w=["qTB"])
    P.vec(TT(HN[:, 1], TA[:], TB[:], ALU.add), r=["qTA", "qTB"], w=[("HN", 1)])
    P.vec(TT(HN[:, 1], HN[:, 1], pv(1), ALU.add), r=[("HN", 1), ("pf", 1)], w=[("HN", 1)])
    P.vec(CP(self.sHR[:], HN[:, 0]), r=[("HN", 0)], w=["sHR"])
    P.vec(TS(self.sHI[:], HN[:, 1], -1.0, ALU.mult), r=[("HN", 1)], w=["sHI"])
    for ri in range(2):
        for k in range(4):
            buf = stg[:, n % 2, :]
            sk = ("stg", n % 2)
            n += 1
            b = self.bank(2, 4)
            b2 = self.bank(2, 4)
            for g8 in range(8):
                bb = b if g8 < 4 else b2
                P.pe(TR(self.psf[0:16, bb, (g8 % 4) * 128:(g8 % 4 + 1) * 128], HN[:, ri, 8 * k + g8, :], idf), r=[("HN", ri), "cst"], w=[("pf", bb)])
            P.vec(CP(buf[:, 0:512], self.psf[0:16, b, :]), r=[("pf", b)], w=[sk])
            P.vec(CP(buf[:, 512:1024], self.psf[0:16, b2, :]), r=[("pf", b2)], w=[sk])
            P.dma("gpsimd", smp["s_s5"][ri, :, k * 1024:(k + 1) * 1024], buf, r=[sk], w=[("s5out", ri, k)])


def _rwkv_sample(self, st, oFM, smp):
    P = self.P
    B = self.rwkv_alloc(st)
    PREV = self.sb("q_PREV", [128, 26, 16], F32, st)
    self.pnew = self.sb("q_pnew", [128, 26, 16], F32, st)
    SH = self.sb("q_SH", [16, PROJ], F32, st)
    Sin = [self.sb("q_Sin%d" % i, [64, 16, 64], F32, st) for i in range(2)]
    Sout = [self.sb("q_Sout%d" % i, [64, 16, 64], F32, st) for i in range(2)]
    Tb = [self.sb("q_Tb%d" % i, [128, 8, 64], F32, st) for i in range(2)]
    idf = self.cv("ident")
    P.dma("sync", SH[:], smp["st_shift"][:, :], w=["SH"])
    for c0 in range(0, 26, 8):
        cn = min(8, 26 - c0)
        b = self.bank()
        for c in range(cn):
            P.pe(TR(self.psf[:, b, c * 16:(c + 1) * 16], SH[:, (c0 + c) * 128:(c0 + c + 1) * 128], idf[0:16, 0:16]), r=["SH", "cst"], w=[("pf", b)])
        P.vec(CP(PREV[:, c0:c0 + cn, :], self.psf[:, b, 0:cn * 16].rearrange("p (a b) -> p a b", b=16)), r=[("pf", b)], w=["prev"])

    def Tstates(t):
        i = t % 2
        P.dma("sync", Sin[i][:], smp["st_rwkv"][t].rearrange("h i j -> i h j"), w=[("Sin", i)])
        for half in range(2):
            b = self.bank(0, 2)
            for q in range(4):
                hp = half * 4 + q
                P.pe(TR(self.psf[:, b, q * 64:(q + 1) * 64], Sin[i][:, 2 * hp:2 * hp + 2, :].rearrange("p a b -> p (a b)"), idf[0:64, 0:64]),
                     r=[("Sin", i), "cst"], w=[("pf", b)])
            P.vec(CP(Tb[i][:, half * 4:half * 4 + 4, :], self.psf[:, b, 0:256].rearrange("p (a b) -> p a b", b=64)), r=[("pf", b)], w=[("Tb", i)])
        return Tb[i], ("Tb", i)

    def step_post(t, Tst, tk):
        i = t % 2
        for half in range(2):
            b = self.bank(0, 2)
            for q in range(4):
                hp = half * 4 + q
                P.pe(TR(self.psf[0:64, b, q * 128:(q + 1) * 128], Tst[:, hp, :], idf), r=[tk, "cst"], w=[("pf", b)])
            P.act(ACP(Sout[i][:, half * 8:half * 8 + 8, :].rearrange("p a b -> p (a b)"), self.psf[0:64, b, :]), r=[("pf", b)], w=[("Sout", i)])
        P.dma("gpsimd", smp["s_rwkv"][t].rearrange("h i j -> i h j"), Sout[i][:], r=[("Sout", i)], w=[("srwkv", t)])

    self.rwkv_subtile(B, 0, oFM, ntok=16, prev=PREV, Tstates=Tstates, step_post=step_post)
    for c0 in range(0, 26, 4):
        cn = min(4, 26 - c0)
        b = self.bank()
        for c in range(cn):
            P.pe(TR(self.psf[0:16, b, c * 128:(c + 1) * 128], self.pnew[:, c0 + c, :], idf), r=["pnew", "cst"], w=[("pf", b)])
        P.vec(CP(SH[:, c0 * 128:(c0 + cn) * 128], self.psf[0:16, b, 0:cn * 128]), r=[("pf", b)], w=["SH"])
    P.dma("gpsimd", smp["s_shift"][:, :], SH[:], r=["SH"], w=["sshift"])


def _layer1_sample(self, smp):
    from contextlib import ExitStack
    P = self.P
    CC = make_consts()
    NT = 16
    P.barrier()
    with ExitStack() as st:
        Qp = self.sb("z_Qp", [128, 2, NT], F32, st)
        Kp = self.sb("z_Kp", [128, 2, NT], F32, st)
        TA = self.sb("z_TA", [128, NT], F32, st)
        TB = self.sb("z_TB", [128, NT], F32, st)
        Qf = self.sb("z_Qf", [128, 2, NT], F32, st)
        Kf = self.sb("z_Kf", [128, 2, NT], F32, st)
        Kr = self.sb("z_Kr", [128, 2, NT], BF16, st)
        QK = self.sb("z_QK", [128, 2, NT], F32, st)
        QM = self.sb("z_QM", [128, 2, NT], F32, st)
        KT = self.sb("z_KT", [NT, 256], BF16, st)
        KTm = self.sb("z_KTm", [NT, 256], BF16, st)
        V = self.sb("z_V", [NT, 512], BF16, st)
        GS = self.sb("z_GS", [NT, 512], BF16, st)
        Sf = [self.sb("z_Sf%d" % i, [128, 2, 512], F32, st) for i in range(3)]
        ATTc = self.sb("z_ATT", [NT, 1], F32, st)
        O = self.sb("z_O", [NT, 512], F32, st)
        TMP = self.sb("z_TMP", [NT, 512], F32, st)
        GA = self.sb("z_GA", [NT, 512], BF16, st)
        GAT = self.sb("z_GAT", [128, 4, NT], BF16, st)
        gnb = self.sb("z_gnb", [NT, 512], F32, st)
        rs = self.sb("z_rot", [128, 4, NT], F32, st)
        r1 = self.sb("z_rot1", [128, 4, 1], F32, st)
        P.dma("sync", r1[:], self.rot_d[:, :, self.SEQ:self.SEQ + 1], w=["r1"], allow_slow_non_contiguous=True)
        P.vec(CP(rs[:], r1[:].to_broadcast([128, 4, NT])), r=["r1"], w=["rs"])
        cos, sin, cos16, sin16 = (rs[:, i, :] for i in range(4))
        nb = 0
        for h in range(RH):
            g1 = float(CC["g1"][h])
            P.dma("sync", gnb[:], self.retgn_d[h, :].partition_broadcast(NT), w=["gnb"])
            for which, dst, (c_, s_), rdst in ((0, Qp, (cos, sin), Qf), (1, Kp, (cos16, sin16), Kf)):
                wq, kq = self.wload(self.wc_d[h, which], 16, 256)
                nm = "QK"[which]
                for dkc in range(2):
                    b = self.bank()
                    for kc in range(16):
                        P.pe(MM(self.psf[:, b, 0:NT], wq[:, kc, dkc * 128:(dkc + 1) * 128], self.hT[:, kc, 0:NT], kc == 0, kc == 15),
                             r=[kq, ("hT", kc)], w=[("pf", b)])
                    P.act(ACP(dst[:, dkc, :], self.psf[:, b, 0:NT]), r=[("pf", b)], w=[(nm + "p", dkc)])
                P.vec(TT(TA[:], dst[:, 0, :], c_, ALU.mult), r=[(nm + "p", 0), "rs"], w=["TA"])
                P.vec(TT(TB[:], dst[:, 1, :], s_, ALU.mult), r=[(nm + "p", 1), "rs"], w=["TB"])
                P.vec(TT(rdst[:, 0, :], TA[:], TB[:], ALU.subtract), r=["TA", "TB"], w=[(nm + "r", 0)])
                P.vec(TT(TA[:], dst[:, 1, :], c_, ALU.mult), r=[(nm + "p", 1), "rs"], w=["TA"])
                P.vec(TT(TB[:], dst[:, 0, :], s_, ALU.mult), r=[(nm + "p", 0), "rs"], w=["TB"])
                P.vec(TT(rdst[:, 1, :], TA[:], TB[:], ALU.add), r=["TA", "TB"], w=[(nm + "r", 1)])
            P.vec(CP(Kr[:], Kf[:]), r=[("Kr", 0), ("Kr", 1)], w=["Krb"])
            P.vec(TT(QK[:], Qf[:], Kf[:], ALU.mult), r=[("Qr", 0), ("Qr", 1), ("Kr", 0), ("Kr", 1)], w=["QKp"])
            ba = self.bank()
            for dkc in range(2):
                P.pe(MM(self.psf[0:NT, ba, 0:2], QK[:, dkc, :], self.epsc[:, 2:4], dkc == 0, dkc == 1), r=["QKp", "epsc"], w=[("pf", ba)])
            P.vec(CP(ATTc[:], self.psf[0:NT, ba, 1:2]), r=[("pf", ba)], w=["ATTc"])
            for dkc in range(2):
                P.pe(TR(self.psb[0:NT, 1, dkc * 128:(dkc + 1) * 128], Kr[:, dkc, :], self.identb[:]), r=["Krb", "identb"], w=[("pb", 1)])
            P.vec(CP(KT[:], self.psb[0:NT, 1, 0:256]), r=[("pb", 1)], w=["KT"])
            for which, dst, nm in ((2, V, "V"), (4, GS, "GS")):
                for vg in range(2):
                    wv, kv = self.wload(self.wc_d[h, which + vg], 16, 256)
                    b = self.bank()
                    for kc in range(16):
                        P.pe(MM(self.psf[0:NT, b, 0:256], self.hT[:, kc, 0:NT], wv[:, kc, :], kc == 0, kc == 15), r=[kv, ("hT", kc)], w=[("pf", b)])
                    if nm == "V":
                        P.act(ACP(dst[:, vg * 256:(vg + 1) * 256], self.psf[0:NT, b, 0:256]), r=[("pf", b)], w=[nm])
                    else:
                        P.act(ACTF(dst[:, vg * 256:(vg + 1) * 256], self.psf[0:NT, b, 0:256], AF.Silu), r=[("pf", b)], w=[nm])
            bcx = 5
            for bb in range(NT):
                S = Sf[nb % 3]
                sk = ("Sf", nb % 3)
                nb += 1
                P.dma("sync", S[:], smp["st_ret"][bb, h].rearrange("(a p) e -> p a e", p=128), w=[sk])
                P.vec(TT(QM[:], Qf[:], self.cself[:, 15 - bb:31 - bb].rearrange("p (a b) -> p a b", a=1).to_broadcast([128, 2, NT]), ALU.mult),
                       r=[("Qr", 0), ("Qr", 1), "cself"], w=["QM"])
                for dkc in range(2):
                    P.pe(MM(self.psf[0:NT, bcx, :], QM[:, dkc, :], S[:, dkc, :], bb == 0 and dkc == 0, bb == NT - 1 and dkc == 1),
                         r=["QM", sk], w=[("pf", bcx)])
                io_, _ = COFF["ident"]
                P.vec(TS(KTm[:], KT[:], self.cst[0:NT, io_ + bb:io_ + bb + 1], ALU.mult), r=["KT", "cst"], w=["KTm"])
                for dkc in range(2):
                    bs = self.bank()
                    P.pe(MM(self.psf[:, bs, :], KTm[:, dkc * 128:(dkc + 1) * 128], V[:, :]), r=["KTm", "V"], w=[("pf", bs)])
                    P.vec(STT(S[:, dkc, :], S[:, dkc, :], g1, self.psf[:, bs, :], ALU.mult, ALU.add), r=[("pf", bs), sk], w=[sk])
                P.dma("gpsimd", smp["s_ret"][bb, h].rearrange("(a p) e -> p a e", p=128), S[:], r=[sk], w=[("sret", bb, h)])
            P.vec(TS(TMP[:], self.psf[0:NT, bcx, :], g1, ALU.mult), r=[("pf", bcx)], w=["TMP"])
            P.vec(STT(O[:], V[:, :], ATTc[:, 0:1], TMP[:], ALU.mult, ALU.add), r=["V", "ATTc", "TMP"], w=["O"])
            P.act(ACTF(self.junk[0:NT, 0:512], O[:], AF.Square, accum_out=self.stat[0:NT, 8:9]), r=["O"], w=["junk", "stat8"])
            P.act(ACTF(self.stat[0:NT, 9:10], self.stat[0:NT, 8:9], AF.Ln, scale=1.0 / DV, bias=self.epsc[0:NT, 0:1]), r=["stat8", "epsc"], w=["stat9"])
            P.act(ACTF(self.stat[0:NT, 10:11], self.stat[0:NT, 9:10], AF.Exp, scale=-0.5), r=["stat9"], w=["stat10"])
            P.vec(STT(TMP[:], O[:], self.stat[0:NT, 10:11], gnb[:], ALU.mult, ALU.mult), r=["O", "stat10", "gnb"], w=["TMP"])
            P.vec(TT(GA[:], TMP[:], GS[:, :], ALU.mult), r=["TMP", "GS"], w=["GA"])
            for ec in range(4):
                P.pe(TR(self.psb[:, 0, ec * 128:ec * 128 + NT], GA[:, ec * 128:(ec + 1) * 128], self.identb[0:NT, 0:NT]), r=["GA", "identb"], w=[("pb", 0)])
            for ec in range(4):
                P.vec(CP(GAT[:, ec, :], self.psb[:, 0, ec * 128:ec * 128 + NT]), r=[("pb", 0)], w=[("GAT", ec)])
            wo0, k0 = self.wload(self.woc_d[h, 0], 2, D)
            wo1, k1 = self.wload(self.woc_d[h, 1], 2, D)
            self.tm_accum(lambda kc, s: GAT[:, kc, 0:NT], 4,
                          lambda kc, cb: (wo0 if kc < 2 else wo1)[:, kc % 2, cb * 512:(cb + 1) * 512],
                          [k0, k1], [("GAT", e) for e in range(4)], ntok=NT, nsub=1)
        P.barrier()


Builder.s5_sample_step = _s5_sample_step
Builder.rwkv_sample = _rwkv_sample
Builder.layer1_sample = _layer1_sample


def _rwkv_chunk_core(self, B):
    P = self.P
    R, K, V, A, KK, BB, KM, T1, T2 = (B[n] for n in ("R", "K", "V", "A", "KK", "BB", "KM", "T1", "T2"))
    KR, BT, KTl, BH, KH, BHT, KHT, GC = (B[n] for n in ("KR", "BT", "KTl", "BH", "KH", "BHT", "KHT", "GC"))
    VT = B["VT"]
    LW, CUM, E1 = A, T2, V
    c4 = lambda X: X[:].rearrange("p a (c t) -> p a c t", t=64)
    cv = self.cv
    for hp in range(8):
        P.vec(lambda e, hp=hp: e.tensor_tensor_scan(out=CUM[:, hp, :], data0=cv("rmask"), data1=LW[:, hp, :], initial=0.0,
                                                    op0=ALU.mult, op1=ALU.add), r=["bA", "cst"], w=["bT2"])
    P.act(ACTF(E1[:], CUM[:], AF.Exp), r=["bT2", "bVb"], w=["bV"])
    P.vec(TT(KR[:, :, :, 1, :], c4(R), c4(E1), ALU.mult), r=["bR", "bV"], w=["KR"])
    P.vec(TT(E1[:], CUM[:], LW[:], ALU.subtract), r=["bT2", "bA", "KR"], w=["bV"])
    P.act(ACTF(E1[:], E1[:], AF.Exp), r=["bV"], w=["bV"])
    P.vec(TT(KR[:, :, :, 0, :], c4(KK), c4(E1), ALU.mult), r=["bKK", "bV"], w=["KR"])
    P.act(ACTF(E1[:], CUM[:], AF.Exp, scale=-1.0), r=["bT2", "KR"], w=["bV"])
    P.vec(TT(BT[:], BB[:], E1[:], ALU.mult), r=["bBB", "bV"], w=["BT"])
    P.vec(TT(KTl[:], KM[:], E1[:], ALU.mult), r=["bKM", "bV"], w=["KTl"])
    cumc = c4(CUM)[:, :, :, 63:64]
    P.vec(TT(c4(E1), cumc.to_broadcast([128, 8, 2, 64]), c4(CUM), ALU.subtract), r=["bT2", "BT", "KTl"], w=["bV"])
    P.act(ACTF(E1[:], E1[:], AF.Exp), r=["bV"], w=["bV"])
    P.vec(TT(BH[:], BB[:], E1[:], ALU.mult), r=["bBB", "bV"], w=["BH"])
    P.vec(TT(KH[:], KM[:], E1[:], ALU.mult), r=["bKM", "bV"], w=["KH"])
    P.act(ACTF(GC[:], cumc.rearrange("p a c o -> p a (c o)"), AF.Exp), r=["bT2"], w=["GC"])
    for src, dst, nm, bk in ((BH, BHT, "BH", 1), (KH, KHT, "KH", 0)):
        for hp in range(8):
            P.pe(TR(self.psb[:, bk, hp * 128:(hp + 1) * 128], src[:, hp, :], self.identb[:]), r=[nm, "identb"], w=[("pb", bk)])
        P.vec(CP(dst[:], self.psb[:, bk, :].rearrange("p (a b) -> p a b", b=128)), r=[("pb", bk)], w=[nm + "T"])
    P.barrier()
    import os
    CHSTOP = int(os.environ.get("CHSTOP", "99"))
    if CHSTOP <= 1:
        return
    bv = lambda X: X[:].bitcast(BF16).rearrange("p a (h t) -> p (a h) t", t=64)
    XA, XB = bv(R)[:, 0:16, :], bv(R)[:, 16:32, :]
    XtA, XtB = bv(KK)[:, 0:16, :], bv(KK)[:, 16:32, :]
    PA, PB = bv(BB)[:, 0:16, :], bv(BB)[:, 16:32, :]
    AkT, RbT = bv(KM)[:, 0:16, :], bv(KM)[:, 16:32, :]
    RkT, NW = bv(A)[:, 0:16, :], bv(A)[:, 16:32, :]
    Ub = bv(T2)[:, 0:16, :]
    Ttmp = V[:, 0:4, :].rearrange("p a (b c) -> p (a b) c", c=64)
    mb = lambda nm, n: cv(nm).rearrange("p (o t) -> p o t", o=1).to_broadcast([128, n, 64])
    for qd in range(4):
        b1, b2, b3 = (0, 1, 2) if qd % 2 == 0 else (3, 4, 5)
        for par in range(2):
            pj = slice(64 * par, 64 * par + 64)
            for hpp in range(2):
                hp = 2 * qd + hpp
                hh = hpp * 2 + par
                for c in range(2):
                    pc = slice(64 * c, 64 * c + 64)
                    krc = KR[pj, hp, c, :, :].rearrange("p a t -> p (a t)")
                    P.pe(MM(self.psf[pc, b1, hh * 128:(hh + 1) * 128], BT[pj, hp, pc], krc), r=["BT", "KR"], w=[("pf", b1)], rg=("j", par))
                    P.pe(MM(self.psf[pc, b2, hh * 128:(hh + 1) * 128], KTl[pj, hp, pc], krc), r=["KTl", "KR"], w=[("pf", b2)], rg=("j", par))
                    P.pe(MM(self.psf[pc, b3, hh * 64:(hh + 1) * 64], KR[pj, hp, c, 0, :], BT[pj, hp, pc]), r=["BT", "KR"], w=[("pf", b3)], rg=("j", par))
        hs = slice(4 * qd, 4 * qd + 4)
        if os.environ.get("CHM") == "1":
            continue
        m1 = self.psf[:, b1, :].rearrange("p (h a t) -> p h a t", a=2, t=64)
        m2 = self.psf[:, b2, :].rearrange("p (h a t) -> p h a t", a=2, t=64)
        m3 = self.psf[:, b3, 0:256].rearrange("p (h t) -> p h t", t=64)
        P.vec(TT(XA[:, hs, :], m1[:, :, 0, :], mb("nsu", 4), ALU.mult), r=[("pf", b1), "cst"], w=["XA"])
        P.vec(TT(RbT[:, hs, :], m1[:, :, 1, :], mb("iu", 4), ALU.mult), r=[("pf", b1), "cst"], w=["RbT"])
        P.vec(TT(AkT[:, hs, :], m2[:, :, 0, :], mb("su", 4), ALU.mult), r=[("pf", b2), "cst"], w=["AkT"])
        P.vec(TT(RkT[:, hs, :], m2[:, :, 1, :], mb("iu", 4), ALU.mult), r=[("pf", b2), "cst"], w=["RkT"])
        P.vec(TT(XtA[:, hs, :], m3, mb("nsl", 4), ALU.mult), r=[("pf", b3), "cst"], w=["XtA"])
    if CHSTOP <= 2:
        P.barrier()
        return
    P.pool(TT(PA, XA, mb("i64", 16), ALU.add), r=["XA", "cst"], w=["PA"])
    Xc, Xtc, Xn, Xtn = (XA, "XA"), (XtA, "XtA"), (XB, "XB"), (XtB, "XtB")
    Pc, Pn = (PA, "PA"), (PB, "PB")
    bview = lambda b: self.psf[:, b, :].rearrange("p (h t) -> p h t", t=64)
    for lvl in range(5):
        last = lvl == 4
        for c in range(2):
            pc = slice(64 * c, 64 * c + 64)
            for h in range(16):
                col = slice((h % 8) * 64, (h % 8) * 64 + 64)
                if not last:
                    P.pe(MM(self.psf[pc, 0 + h // 8, col], Xtc[0][pc, h, :], Xc[0][pc, h, :]), r=[Xtc[1], Xc[1]], w=[("pf", 0 + h // 8)], rg=("t", c))
                P.pe(MM(self.psf[pc, 2 + h // 8, col], Xc[0][pc, h, :], Xtc[0][pc, h, :]), r=[Xtc[1], Xc[1]], w=[("pf", 2 + h // 8)], rg=("t", c))
        for hb in range(2):
            hs = slice(8 * hb, 8 * hb + 8)
            if not last:
                P.act(ACP(Xn[0][:, hs, :], bview(0 + hb)), r=[("pf", 0 + hb)], w=[Xn[1]])
            P.vec(CP(Xtn[0][:, hs, :], bview(2 + hb)), r=[("pf", 2 + hb)], w=[Xtn[1]])
        for c in range(2):
            pc = slice(64 * c, 64 * c + 64)
            for h in range(16):
                col = slice((h % 8) * 64, (h % 8) * 64 + 64)
                P.pe(MM(self.psf[pc, 4 + h // 8, col], Xtn[0][pc, h, :], Pc[0][pc, h, :]), r=[Xtn[1], Pc[1]], w=[("pf", 4 + h // 8)], rg=("t", c))
        for hb in range(2):
            hs = slice(8 * hb, 8 * hb + 8)
            P.vec(TT(Pn[0][:, hs, :], bview(4 + hb), Pc[0][:, hs, :], ALU.add), r=[("pf", 4 + hb), Pc[1]], w=[Pn[1]])
        Xc, Xn = Xn, Xc
        Xtc, Xtn = Xtn, Xtc
        Pc, Pn = Pn, Pc
    MT = Pc
    if CHSTOP <= 3:
        P.barrier()
        return
    Tst, Tb = self.Tst, self.Tb
    for c in range(2):
        pc = slice(64 * c, 64 * c + 64)
        hcol = lambda h: slice((h % 8) * 64, (h % 8) * 64 + 64)
        first = {0: True, 1: True}
        for par in range(2):
            pj = slice(64 * par, 64 * par + 64)
            for hp in range(8):
                h = 2 * hp + par
                P.pe(MM(self.psf[pc, h // 8, hcol(h)], KR[pj, hp, c, 0, :], Tb[pj, hp, :], first[h // 8], False, sgc=True),
                     r=["KR", "Tb"], w=[("pf", h // 8)], rg=("j", par))
                first[h // 8] = False
        for h in range(16):
            hp, par = h // 2, h % 2
            P.pe(MM(self.psf[pc, h // 8, hcol(h)], AkT[pc, h, :], VT[pc, hp, 64 * par:64 * par + 64], False, True, sgc=True),
                 r=["AkT", "bVT"], w=[("pf", h // 8)], rg=("t", c))
        for hb in range(2):
            hs = slice(8 * hb, 8 * hb + 8)
            P.act(ACTF(NW[pc, hs, :], bview(hb)[pc], AF.Copy, scale=-1.0), r=[("pf", hb)], w=["NW"])
        for h in range(16):
            P.pe(MM(self.psf[pc, 2 + h // 8, hcol(h)], MT[0][pc, h, :], NW[pc, h, :]), r=[MT[1], "NW"], w=[("pf", 2 + h // 8)], rg=("t", c))
        for hb in range(2):
            hs = slice(8 * hb, 8 * hb + 8)
            P.vec(CP(Ub[pc, hs, :], bview(2 + hb)[pc]), r=[("pf", 2 + hb)], w=["Ub"])
        first = {0: True, 1: True}
        for par in range(2):
            pj = slice(64 * par, 64 * par + 64)
            for hp in range(8):
                h = 2 * hp + par
                P.pe(MM(self.psf[pc, 4 + h // 8, hcol(h)], KR[pj, hp, c, 1, :], Tb[pj, hp, :], first[h // 8], False, sgc=True),
                     r=["KR", "Tb"], w=[("pf", 4 + h // 8)], rg=("j", par))
                first[h // 8] = False
        for h in range(16):
            hp, par = h // 2, h % 2
            vt = VT[pc, hp, 64 * par:64 * par + 64]
            P.pe(MM(self.psf[pc, 4 + h // 8, hcol(h)], RbT[pc, h, :], Ub[pc, h, :], False, False, sgc=True), r=["RbT", "Ub"], w=[("pf", 4 + h // 8)], rg=("t", c))
            P.pe(MM(self.psf[pc, 4 + h // 8, hcol(h)], RkT[pc, h, :], vt, False, True, sgc=True), r=["RkT", "bVT"], w=[("pf", 4 + h // 8)], rg=("t", c))
        for h in range(16):
            hp, par = h // 2, h % 2
            pj = slice(64 * par, 64 * par + 64)
            vt = VT[pc, hp, 64 * par:64 * par + 64]
            P.pe(MM(self.psf[pj, 0, hp * 64:(hp + 1) * 64], BHT[pc, hp, pj], Ub[pc, h, :], True, False), r=["BHT", "Ub"], w=[("pf", 0)], rg=("t", c))
            P.pe(MM(self.psf[pj, 0, hp * 64:(hp + 1) * 64], KHT[pc, hp, pj], vt, False, True), r=["KHT", "bVT"], w=[("pf", 0)], rg=("t", c))
        gcb = GC[:, :, c:c + 1].to_broadcast([128, 8, 64])
        P.pool(TT(Ttmp, Tst[:], gcb, ALU.mult), r=["Tst", "GC"], w=["Ttmp"])
        P.vec(TT(Tst[:], Ttmp, bview(0), ALU.add), r=["Ttmp", ("pf", 0)], w=["Tst"])
        P.act(ACP(Tb[:], Tst[:]), r=["Tst"], w=["Tb"])
    P.barrier()


Builder.rwkv_chunk_core = _rwkv_chunk_core
```
